# Optimizing a Trainium2 kernel written in Bass

```python
import jax, jax.numpy as jnp
from jax import lax
import numpy as np

D_MODEL = 1024
BATCH = 8
SEQ = 2048
DEPTH = 2

GRID_W = 64
CTX_LEN = 256
MLA_HEADS = 8
MLA_NOPE = 64
MLA_ROPE = 32
MLA_V = 64
MLA_Q_RANK = 256
MLA_KV_RANK = 128
GQA_HEADS = 8
GQA_KV_HEADS = 2
GQA_GROUP = GQA_HEADS // GQA_KV_HEADS
GQA_HEAD_DIM = 64
IN_SPLITS = (MLA_Q_RANK, MLA_KV_RANK, MLA_ROPE, GQA_HEADS * GQA_HEAD_DIM,
             GQA_KV_HEADS * GQA_HEAD_DIM, GQA_KV_HEADS * GQA_HEAD_DIM)
IN_WIDTH = sum(IN_SPLITS)
MIX_WIDTH = MLA_HEADS * MLA_V + GQA_HEADS * GQA_HEAD_DIM
DENSE_FF = 2816
N_EXPERTS = 8
TOP_K = 2
EXPERT_FF = 2816
Q_BLOCK = 128
ROPE_THETA = 10000.0
NORM_EPS = 1e-6
MLA_SCALE = (MLA_NOPE + MLA_ROPE) ** -0.5
GQA_SCALE = GQA_HEAD_DIM ** -0.5

kernel_name = "hybrid_mla_gqa_moe_dit_block"


def _rmsnorm(x, g):
    xf = x.astype(jnp.float32)
    y = xf * lax.rsqrt(jnp.mean(xf * xf, axis=-1, keepdims=True) + NORM_EPS)
    return (y * g.astype(jnp.float32)).astype(x.dtype)


def _modulate(h, shift, scale):
    return h * (1 + scale) + shift


def _rope_1d(x, pos):
    half = x.shape[-1] // 2
    freqs = ROPE_THETA ** (-jnp.arange(half, dtype=jnp.float32) / half)
    ang = pos[:, None] * freqs[None, :]
    cos, sin = jnp.cos(ang), jnp.sin(ang)
    x1 = x[..., :half].astype(jnp.float32)
    x2 = x[..., half:].astype(jnp.float32)
    return jnp.concatenate([x1 * cos - x2 * sin, x2 * cos + x1 * sin], axis=-1).astype(x.dtype)


def _rope_2d(x, row, col):
    d = x.shape[-1] // 2
    return jnp.concatenate([_rope_1d(x[..., :d], row), _rope_1d(x[..., d:], col)], axis=-1)


def _rope_tail(x, n_rot, pos):
    if pos is None:
        return x
    d = x.shape[-1]
    return jnp.concatenate([x[..., :d - n_rot], _rope_2d(x[..., d - n_rot:], pos[0], pos[1])], axis=-1)


def _split_in(h, w_in):
    p = h @ w_in
    offs = [int(o) for o in np.cumsum(IN_SPLITS)[:-1]]
    return jnp.split(p, offs, axis=-1)


def _mla_q(c_q, p, pos):
    B, L, _ = c_q.shape
    q = (_rmsnorm(c_q, p["q_lat_norm"]) @ p["w_uq"]).reshape(B, L, MLA_HEADS, MLA_NOPE + MLA_ROPE)
    q = _rmsnorm(q.transpose(0, 2, 1, 3), p["mla_q_gain"])
    return _rope_tail(q, MLA_ROPE, pos)[:, :, None]


def _mla_kv(c_kv, k_rope, p, pos):
    B, L, _ = c_kv.shape
    kv = (_rmsnorm(c_kv, p["kv_lat_norm"]) @ p["w_ukv"]).reshape(B, L, MLA_HEADS, MLA_NOPE + MLA_V)
    kv = kv.transpose(0, 2, 1, 3)
    k_nope, v = kv[..., :MLA_NOPE], kv[..., MLA_NOPE:]
    k_r = jnp.broadcast_to(k_rope[:, None], (B, MLA_HEADS, L, MLA_ROPE))
    k = _rmsnorm(jnp.concatenate([k_nope, k_r], axis=-1), p["mla_k_gain"])
    return _rope_tail(k, MLA_ROPE, pos), v


def _gqa_q(qb, p, pos):
    B, L, _ = qb.shape
    q = qb.reshape(B, L, GQA_KV_HEADS, GQA_GROUP, GQA_HEAD_DIM).transpose(0, 2, 3, 1, 4)
    q = _rmsnorm(q, p["gqa_q_gain"])
    return _rope_tail(q, GQA_HEAD_DIM, pos)


def _gqa_kv(kb, vb, p, pos):
    B, L, _ = kb.shape
    k = kb.reshape(B, L, GQA_KV_HEADS, GQA_HEAD_DIM).transpose(0, 2, 1, 3)
    v = vb.reshape(B, L, GQA_KV_HEADS, GQA_HEAD_DIM).transpose(0, 2, 1, 3)
    k = _rmsnorm(k, p["gqa_k_gain"])
    return _rope_tail(k, GQA_HEAD_DIM, pos), v


def _block_attention(q, k, v, scale):
    B, Hk, G, Sq, D = q.shape
    nb = Sq // Q_BLOCK
    qb = q.reshape(B, Hk, G, nb, Q_BLOCK, D).transpose(3, 0, 1, 2, 4, 5)

    def one_block(qi):
        s = jnp.einsum("bhgqd,bhkd->bhgqk", qi, k).astype(jnp.float32) * scale
        pr = jax.nn.softmax(s, axis=-1).astype(v.dtype)
        return jnp.einsum("bhgqk,bhkd->bhgqd", pr, v)

    o = lax.map(one_block, qb)
    return o.transpose(1, 2, 3, 0, 4, 5).reshape(B, Hk, G, Sq, v.shape[-1])


def _merge(oa, ob, w_out):
    B, L = oa.shape[0], oa.shape[3]
    a = oa[:, :, 0].transpose(0, 2, 1, 3).reshape(B, L, MLA_HEADS * MLA_V)
    b = ob.transpose(0, 3, 1, 2, 4).reshape(B, L, GQA_HEADS * GQA_HEAD_DIM)
    return jnp.concatenate([a, b], axis=-1) @ w_out


def _swiglu(h, wg, wu, wd):
    return (jax.nn.silu(h @ wg) * (h @ wu)) @ wd


def _moe(h, router, wg, wu, wd):
    probs = jax.nn.softmax((h @ router).astype(jnp.float32), axis=-1)
    top_v, top_i = lax.top_k(probs, TOP_K)
    top_v = top_v / jnp.sum(top_v, axis=-1, keepdims=True)
    gates = jnp.sum(jax.nn.one_hot(top_i, N_EXPERTS, dtype=jnp.float32) * top_v[..., None], axis=-2)
    gates = gates.astype(h.dtype)
    out = jnp.zeros_like(h)
    for e in range(N_EXPERTS):
        out = out + gates[..., e:e + 1] * _swiglu(h, wg[e], wu[e], wd[e])
    return out


def _ffn(h, p, moe):
    if moe:
        return _moe(h, *p["ffn"])
    return _swiglu(h, *p["ffn"])


def _layer(x, hc, c, c_ctx, p, pos, moe, last):
    mod_x = (jax.nn.silu(c) @ p["w_mod"] + p["b_mod"])[:, None, :]
    mod_c = (jax.nn.silu(c_ctx) @ p["w_mod"] + p["b_mod"])[None, None, :]
    sh1x, sc1x, g1x, sh2x, sc2x, g2x = jnp.split(mod_x, 6, axis=-1)
    sh1c, sc1c, g1c, sh2c, sc2c, g2c = jnp.split(mod_c, 6, axis=-1)

    hx = _modulate(_rmsnorm(x, p["norm_attn"]), sh1x, sc1x)
    hcn = _modulate(_rmsnorm(hc, p["norm_attn"]), sh1c, sc1c)
    cq_x, ckv_x, kr_x, qb_x, kb_x, vb_x = _split_in(hx, p["w_in"])
    cq_c, ckv_c, kr_c, qb_c, kb_c, vb_c = _split_in(hcn, p["w_in"])

    ka_c, va_c = _mla_kv(ckv_c, kr_c, p, None)
    kb_c, vb_c = _gqa_kv(kb_c, vb_c, p, None)
    ka_x, va_x = _mla_kv(ckv_x, kr_x, p, pos)
    kb_x, vb_x = _gqa_kv(kb_x, vb_x, p, pos)

    oa = _block_attention(_mla_q(cq_x, p, pos), jnp.concatenate([ka_x, ka_c], axis=2),
                          jnp.concatenate([va_x, va_c], axis=2), MLA_SCALE)
    ob = _block_attention(_gqa_q(qb_x, p, pos), jnp.concatenate([kb_x, kb_c], axis=2),
                          jnp.concatenate([vb_x, vb_c], axis=2), GQA_SCALE)
    x = x + g1x * _merge(oa, ob, p["w_out"])
    x = x + g2x * _ffn(_modulate(_rmsnorm(x, p["norm_ffn"]), sh2x, sc2x), p, moe)

    if not last:
        oa_c = _block_attention(_mla_q(cq_c, p, None), ka_c, va_c, MLA_SCALE)
        ob_c = _block_attention(_gqa_q(qb_c, p, None), kb_c, vb_c, GQA_SCALE)
        hc = hc + g1c * _merge(oa_c, ob_c, p["w_out"])
        hc = hc + g2c * _ffn(_modulate(_rmsnorm(hc, p["norm_ffn"]), sh2c, sc2c), p, moe)
    return x, hc


def _layer_params(key, prefix, moe):
    ks = jax.random.split(key, 20)
    f32 = jnp.float32

    def nrm(k, shape, s):
        return jax.random.normal(k, shape, f32) * s

    def gain(k, n):
        return 1.0 + 0.05 * jax.random.normal(k, (n,), f32)

    p = {}
    p[prefix + "w_mod"] = nrm(ks[0], (D_MODEL, 6 * D_MODEL), 0.5 * D_MODEL ** -0.5)
    p[prefix + "b_mod"] = nrm(ks[1], (6 * D_MODEL,), 0.02)
    p[prefix + "norm_attn"] = gain(ks[2], D_MODEL)
    p[prefix + "w_in"] = nrm(ks[3], (D_MODEL, IN_WIDTH), D_MODEL ** -0.5)
    p[prefix + "q_lat_norm"] = gain(ks[4], MLA_Q_RANK)
    p[prefix + "w_uq"] = nrm(ks[5], (MLA_Q_RANK, MLA_HEADS * (MLA_NOPE + MLA_ROPE)), MLA_Q_RANK ** -0.5)
    p[prefix + "kv_lat_norm"] = gain(ks[6], MLA_KV_RANK)
    p[prefix + "w_ukv"] = nrm(ks[7], (MLA_KV_RANK, MLA_HEADS * (MLA_NOPE + MLA_V)), MLA_KV_RANK ** -0.5)
    p[prefix + "mla_q_gain"] = gain(ks[8], MLA_NOPE + MLA_ROPE)
    p[prefix + "mla_k_gain"] = gain(ks[9], MLA_NOPE + MLA_ROPE)
    p[prefix + "gqa_q_gain"] = gain(ks[10], GQA_HEAD_DIM)
    p[prefix + "gqa_k_gain"] = gain(ks[11], GQA_HEAD_DIM)
    p[prefix + "w_out"] = nrm(ks[12], (MIX_WIDTH, D_MODEL), MIX_WIDTH ** -0.5)
    p[prefix + "norm_ffn"] = gain(ks[13], D_MODEL)
    if moe:
        p[prefix + "router"] = nrm(ks[14], (D_MODEL, N_EXPERTS), D_MODEL ** -0.5)
        p[prefix + "exp_w_gate"] = nrm(ks[15], (N_EXPERTS, D_MODEL, EXPERT_FF), D_MODEL ** -0.5)
        p[prefix + "exp_w_up"] = nrm(ks[16], (N_EXPERTS, D_MODEL, EXPERT_FF), D_MODEL ** -0.5)
        p[prefix + "exp_w_down"] = nrm(ks[17], (N_EXPERTS, EXPERT_FF, D_MODEL), EXPERT_FF ** -0.5)
    else:
        p[prefix + "ffn_w_gate"] = nrm(ks[14], (D_MODEL, DENSE_FF), D_MODEL ** -0.5)
        p[prefix + "ffn_w_up"] = nrm(ks[15], (D_MODEL, DENSE_FF), D_MODEL ** -0.5)
        p[prefix + "ffn_w_down"] = nrm(ks[16], (DENSE_FF, D_MODEL), DENSE_FF ** -0.5)
    return p


def setup_inputs(seed: int = 0) -> dict:
    key = jax.random.key(seed)
    kx, kc, kctx, kcc, k0, k1 = jax.random.split(key, 6)
    inputs = {
        "x": jax.random.normal(kx, (BATCH, SEQ, D_MODEL), jnp.float32),
        "c": jax.random.normal(kc, (BATCH, D_MODEL), jnp.float32),
        "ctx": jax.random.normal(kctx, (BATCH, CTX_LEN, D_MODEL), jnp.float32),
        "c_ctx": jax.random.normal(kcc, (D_MODEL,), jnp.float32),
    }
    inputs.update(_layer_params(k0, "l0_", moe=False))
    inputs.update(_layer_params(k1, "l1_", moe=True))
    return inputs


def reference(x, c, ctx, c_ctx,
              l0_w_mod, l0_b_mod, l0_norm_attn, l0_w_in, l0_q_lat_norm, l0_w_uq, l0_kv_lat_norm,
              l0_w_ukv, l0_mla_q_gain, l0_mla_k_gain, l0_gqa_q_gain, l0_gqa_k_gain, l0_w_out,
              l0_norm_ffn, l0_ffn_w_gate, l0_ffn_w_up, l0_ffn_w_down,
              l1_w_mod, l1_b_mod, l1_norm_attn, l1_w_in, l1_q_lat_norm, l1_w_uq, l1_kv_lat_norm,
              l1_w_ukv, l1_mla_q_gain, l1_mla_k_gain, l1_gqa_q_gain, l1_gqa_k_gain, l1_w_out,
              l1_norm_ffn, l1_router, l1_exp_w_gate, l1_exp_w_up, l1_exp_w_down):
    p0 = dict(w_mod=l0_w_mod, b_mod=l0_b_mod, norm_attn=l0_norm_attn, w_in=l0_w_in,
              q_lat_norm=l0_q_lat_norm, w_uq=l0_w_uq, kv_lat_norm=l0_kv_lat_norm, w_ukv=l0_w_ukv,
              mla_q_gain=l0_mla_q_gain, mla_k_gain=l0_mla_k_gain, gqa_q_gain=l0_gqa_q_gain,
              gqa_k_gain=l0_gqa_k_gain, w_out=l0_w_out, norm_ffn=l0_norm_ffn,
              ffn=(l0_ffn_w_gate, l0_ffn_w_up, l0_ffn_w_down))
    p1 = dict(w_mod=l1_w_mod, b_mod=l1_b_mod, norm_attn=l1_norm_attn, w_in=l1_w_in,
              q_lat_norm=l1_q_lat_norm, w_uq=l1_w_uq, kv_lat_norm=l1_kv_lat_norm, w_ukv=l1_w_ukv,
              mla_q_gain=l1_mla_q_gain, mla_k_gain=l1_mla_k_gain, gqa_q_gain=l1_gqa_q_gain,
              gqa_k_gain=l1_gqa_k_gain, w_out=l1_w_out, norm_ffn=l1_norm_ffn,
              ffn=(l1_router, l1_exp_w_gate, l1_exp_w_up, l1_exp_w_down))
    layers = [p0, p1]

    n_tok = x.shape[1]
    rows = n_tok // GRID_W
    row = jnp.repeat(jnp.arange(rows, dtype=jnp.float32), GRID_W)
    col = jnp.tile(jnp.arange(GRID_W, dtype=jnp.float32), rows)
    pos = (row, col)

    hc = ctx
    for i in range(DEPTH):
        x, hc = _layer(x, hc, c, c_ctx, layers[i], pos, moe=(i % 2 == 1), last=(i == DEPTH - 1))
    return x
```

```python
import numpy as np
from contextlib import ExitStack
import concourse.bass as bass
import concourse.mybir as mybir
from concourse.bass_utils import run_bass_kernel_spmd

F32 = mybir.dt.float32
BF16 = mybir.dt.bfloat16
I32 = mybir.dt.int32
AF = mybir.ActivationFunctionType
ALU = mybir.AluOpType
AX = mybir.AxisListType

ENGS = ("pe", "act", "dve", "pool", "sp")
ENGOBJ = {"pe": "tensor", "act": "scalar", "dve": "vector", "pool": "gpsimd", "sp": "sync"}

T_LAT, T_CTX, T_ALL = 2048, 256, 2304
EPS = 1e-6
THETA = 10000.0
FF = 2816
NFC = FF // 128


class _Op:
    __slots__ = ("eng", "fn", "deps", "is_dma", "dkey", "sig", "idx", "dcount", "ndma", "cond")

    def __init__(self, eng, fn, is_dma=False, dkey=None, ndma=1, cond=None):
        self.cond = cond
        self.eng = eng
        self.fn = fn
        self.deps = []
        self.is_dma = is_dma
        self.dkey = dkey
        self.sig = False
        self.idx = None
        self.dcount = None
        self.ndma = ndma


class Prog:
    NDMA = 14

    def __init__(self, nc):
        self.nc = nc
        self.sets = []
        for s in range(2):
            d = {e: nc.alloc_semaphore(name=f"s{s}_{e}") for e in ENGS}
            d["dma"] = [nc.alloc_semaphore(name=f"s{s}_d{i}") for i in range(self.NDMA)]
            self.sets.append(d)
        self.phase_no = 0
        self.count = 0
        self.dtot = {}
        self.limit = None
        self.ops = None
        self.dirty = [False, False]
        self.flags_ap = None
        self.nlvl = 6
        self.regs = {}
        self.loaded = {}
        with nc.Block() as block:
            for e in ENGS:
                def make(e):
                    def body(eng):
                        for st in self.sets:
                            eng.sem_clear(st[e])
                            if e == "pool":
                                for sm in st["dma"]:
                                    eng.sem_clear(sm)
                        if e == "pool":
                            r = eng.alloc_register("idma_bound")
                            eng.reg_mov(r, 8 * T_LAT - 1)
                            self.bc = eng.snap(r)
                    return body
                getattr(block, ENGOBJ[e])(make(e))

    def begin(self):
        self.ops = []
        self.lastw = {}
        self.readers = {}
        self.dkeys = {}

    def _add(self, op, reads, writes):
        deps = []
        for r in reads:
            w = self.lastw.get(r)
            if w is not None:
                deps.append(w)
        for wk in writes:
            w = self.lastw.get(wk)
            if w is not None:
                deps.append(w)
            deps.extend(self.readers.get(wk, ()))
        seen = set()
        for d in deps:
            if d is op or id(d) in seen:
                continue
            seen.add(id(d))
            if d.eng == "pe" and op.eng == "pe" and not d.is_dma and not op.is_dma:
                continue
            op.deps.append(d)
        for r in reads:
            self.readers.setdefault(r, []).append(op)
        for wk in writes:
            self.lastw[wk] = op
            self.readers[wk] = []
        self.ops.append(op)
        return op

    def op(self, eng, fn, reads=(), writes=(), cond=None):
        return self._add(_Op(eng, fn, cond=cond), list(reads), list(writes))

    def dma(self, queue, fn, reads=(), writes=(), key=None, n=1):
        if key not in self.dkeys:
            self.dkeys[key] = len(self.dkeys)
            assert len(self.dkeys) <= self.NDMA, "too many dma keys in phase"
        return self._add(_Op(queue, fn, True, key, n), list(reads), list(writes))

    def end(self):
        nc = self.nc
        ops = self.ops
        self.count += 1
        if self.limit is not None and self.count > self.limit:
            self.ops = None
            return
        if self.limit is not None and self.count == self.limit and getattr(self, "oplimit", None) is not None:
            ops = ops[:self.oplimit]
            print("phase ops total", len(self.ops), "emitting", len(ops))
        cur = self.phase_no % 2
        sems = self.sets[cur]
        other = self.sets[1 - cur]
        for o in ops:
            for d in o.deps:
                d.sig = True
        cnt = {e: 0 for e in ENGS}
        dcnt = {k: self.dtot.get(i, 0) for k, i in self.dkeys.items()}
        for o in ops:
            if o.is_dma:
                dcnt[o.dkey] = dcnt.get(o.dkey, 0) + 16 * o.ndma
                o.dcount = dcnt[o.dkey]
            elif o.sig:
                cnt[o.eng] += 1
                o.idx = cnt[o.eng]
        per = {e: [] for e in ENGS}
        for o in ops:
            per[o.eng].append(o)
        clear_other = self.dirty[1 - cur]
        dkeys = self.dkeys

        def dep_kv(d):
            if d.is_dma:
                return ("dma", dkeys[d.dkey]), d.dcount
            return d.eng, d.idx

        def do_waits(eng, need, waited):
            for k, v in need.items():
                s = self.sets[0]["dma"][k[1]] if isinstance(k, tuple) else sems[k]
                eng.wait_ge(s, v)
                waited[k] = v

        def emit_op(e, eng, o, waited):
            need = {}
            for d in o.deps:
                k, v = dep_kv(d)
                if waited.get(k, 0) >= v:
                    continue
                if need.get(k, 0) < v:
                    need[k] = v
            do_waits(eng, need, waited)
            ins = o.fn(eng)
            if o.is_dma:
                lst = ins if isinstance(ins, (list, tuple)) else [ins]
                assert len(lst) == o.ndma, (len(lst), o.ndma)
                for i_ in lst:
                    i_.then_inc(self.sets[0]["dma"][dkeys[o.dkey]], 16)
            elif o.sig:
                ins.then_inc(sems[e], 1)

        NLVL = self.nlvl

        def skip_regions(e, eng, regions, conds, snap):
            emitted = False
            for region in regions:
                need = {}
                nsig = 0
                for r_ in region:
                    if r_.sig:
                        nsig += 1
                    for d in r_.deps:
                        if d.cond in conds:
                            continue
                        k, v = dep_kv(d)
                        if snap.get(k, 0) >= v:
                            continue
                        if need.get(k, 0) < v:
                            need[k] = v
                do_waits(eng, need, snap)
                if nsig:
                    eng.sem_inc(sems[e], nsig)
                    emitted = True
            if not emitted:
                eng.nop()

        def emit_chain(e, eng, regions, waited):
            region = regions[0]
            lvl = region[0].cond[2]
            reg = self.regs[(e, lvl)]
            snap = dict(waited)
            conds = set(r[0].cond for r in regions)
            with eng.If_ne(reg, 0):
                w2 = dict(snap)
                for r_ in region:
                    emit_op(e, eng, r_, w2)
                if len(regions) > 1:
                    emit_chain(e, eng, regions[1:], w2)
            with eng.Else():
                skip_regions(e, eng, regions, conds, snap)
            return snap

        def emit(e, eng):
            waited = {}
            if clear_other:
                eng.sem_clear(other[e])
            lst = per[e]
            i = 0
            while i < len(lst):
                o = lst[i]
                if o.cond is None:
                    emit_op(e, eng, o, waited)
                    i += 1
                    continue
                eg = o.cond[:2]
                j = i
                while j < len(lst) and lst[j].cond is not None and lst[j].cond[:2] == eg:
                    j += 1
                chain = lst[i:j]
                i = j
                assert all(not r_.is_dma for r_ in chain)
                regions = []
                for r_ in chain:
                    if regions and regions[-1][0].cond == r_.cond:
                        regions[-1].append(r_)
                    else:
                        regions.append([r_])
                lv = [r[0].cond[2] for r in regions]
                assert lv == sorted(set(lv)), lv
                eidx = eg[0]
                if self.loaded.get(e) != eidx:
                    for lvl in range(1, NLVL + 1):
                        if (e, lvl) not in self.regs:
                            self.regs[(e, lvl)] = eng.alloc_register(f"fl_{e}_{lvl}")
                        c_ = eidx * NLVL + lvl - 1
                        eng.reg_load(self.regs[(e, lvl)], self.flags_ap[0:1, c_:c_ + 1])
                    self.loaded[e] = eidx
                waited = emit_chain(e, eng, regions, waited)
            last = {}
            for o in per[e]:
                if o.is_dma:
                    last[o.dkey] = o.dcount
            for k, v in last.items():
                if waited.get(("dma", dkeys[k]), 0) < v:
                    eng.wait_ge(self.sets[0]["dma"][dkeys[k]], v)

        with nc.Block() as block:
            for e in ENGS:
                def make(e):
                    def body(eng):
                        emit(e, eng)
                    return body
                getattr(block, ENGOBJ[e])(make(e))
        for k, i in self.dkeys.items():
            self.dtot[i] = dcnt[k]
        self.dirty[cur] = True
        self.phase_no += 1
        self.ops = None


def MM(out, lhsT, rhs, start=True, stop=True):
    return lambda e: e.matmul(out, lhsT=lhsT, rhs=rhs, start=start, stop=stop)


def TR(out, in_, ident):
    return lambda e: e.transpose(out, in_, ident)


def ACT(out, in_, func, bias=None, scale=None):
    kw = {}
    if bias is not None:
        kw["bias"] = bias
    if scale is not None:
        kw["scale"] = scale
    return lambda e: e.activation(out=out, in_=in_, func=func, **kw)


def TT(out, in0, in1, op):
    return lambda e: e.tensor_tensor(out=out, in0=in0, in1=in1, op=op)


def STT(out, in0, scalar, in1, op0, op1):
    return lambda e: e.scalar_tensor_tensor(out=out, in0=in0, scalar=scalar, in1=in1, op0=op0, op1=op1)


def TS(out, in0, s1, s2, op0, op1=None):
    if op1 is None:
        return lambda e: e.tensor_scalar(out=out, in0=in0, scalar1=s1, scalar2=None, op0=op0)
    return lambda e: e.tensor_scalar(out=out, in0=in0, scalar1=s1, scalar2=s2, op0=op0, op1=op1)


def CP(out, in_):
    return lambda e: e.tensor_copy(out=out, in_=in_)


def RECIP(out, in_):
    return lambda e: e.reciprocal(out=out, in_=in_)


def MEMSET(ap, v):
    return lambda e: e.memset(ap, v)


def RMAX(out, in_):
    return lambda e: e.reduce_max(out=out, in_=in_, axis=AX.X)


def DMA(out, in_):
    return lambda e: e.dma_start(out=out, in_=in_)


def RSUM(out, in_):
    return lambda e: e.reduce_sum(out=out, in_=in_, axis=AX.X)


def ISCAT(dram, idx_ap, src, bound):
    return lambda e: e.indirect_dma_start(out=dram, out_offset=bass.IndirectOffsetOnAxis(ap=idx_ap, axis=0), in_=src,
                                          in_offset=None, bounds_check=bound, oob_is_err=False)


def IGATH(dst, dram, idx_ap, bound):
    return lambda e: e.indirect_dma_start(out=dst, out_offset=None, in_=dram,
                                          in_offset=bass.IndirectOffsetOnAxis(ap=idx_ap, axis=0),
                                          bounds_check=bound, oob_is_err=False)


def DMAS(pairs):
    def f(e):
        return [e.dma_start(out=o, in_=i) for (o, i) in pairs]
    return f


def make_consts():
    c = {}
    c["ident_f"] = np.eye(128, dtype=np.float32)
    cb = np.zeros((128, 6, 128), np.float32)
    cb[:, 0, :] = np.eye(128)
    cb[:, 1, :] = 1.0
    cb[0:64, 2, 0:64] = 1.0
    cb[64:128, 2, 64:128] = 1.0
    for base in (0, 16):
        for i in range(8):
            a = 64 + base + i
            b = a + 8
            cb[b, 3, a] = -1.0
            cb[a, 3, b] = 1.0
    for hb in (0, 64):
        for base in (0, 32):
            for i in range(16):
                a = hb + base + i
                b = a + 16
                cb[b, 4, a] = -1.0
                cb[a, 4, b] = 1.0
    cb[:, 5, :] = np.triu(np.ones((128, 128), np.float32), 1)
    c["cb"] = cb
    c["ebase"] = np.ascontiguousarray(np.broadcast_to(np.tile(np.arange(8, dtype=np.float32) * 2048.0, 16)[None, :], (128, 128)))
    sel96 = np.zeros((32, 96), np.float32)
    for i in range(32):
        sel96[i, 64 + i] = 1.0
    c["sel96"] = sel96
    t = np.arange(T_LAT)
    row = (t // 64).astype(np.float64)
    col = (t % 64).astype(np.float64)
    tabs = np.zeros((128, 4, T_LAT), np.float32)
    tabs[:, 0, :] = 1.0
    tabs[:, 2, :] = 1.0
    fa = THETA ** (-np.arange(8, dtype=np.float64) / 8.0)
    fb = THETA ** (-np.arange(16, dtype=np.float64) / 16.0)
    fa32 = fa.astype(np.float32).astype(np.float64)
    fb32 = fb.astype(np.float32).astype(np.float64)
    for r in range(32):
        pos = row if r < 16 else col
        ang = (pos * fa32[r % 8]).astype(np.float32).astype(np.float64)
        tabs[64 + r, 0, :] = np.cos(ang)
        tabs[64 + r, 1, :] = np.sin(ang)
    for hb in (0, 64):
        for d in range(64):
            pos = row if d < 32 else col
            ang = (pos * fb32[d % 16]).astype(np.float32).astype(np.float64)
            tabs[hb + d, 2, :] = np.cos(ang)
            tabs[hb + d, 3, :] = np.sin(ang)
    c["tabs"] = tabs
    return c


LAYER_W = ["w_mod", "vecs", "w_in", "w_uq", "w_ukv", "mla_q_gain", "mla_k_gain", "gqa_q_gain",
           "gqa_k_gain", "w_out"]


def build_program(dbg=None, limit=None, dbg_fn=None, oplimit=None):
    nc = bass.Bass("TRN2", target_bir_lowering=False)

    def din(name, shape):
        return nc.dram_tensor(name, list(shape), F32, kind="ExternalInput").ap()

    x_d = din("x", [T_LAT, 1024])
    ctx_d = din("ctx", [T_CTX, 1024])
    cvec_d = din("cvec", [16, 128])
    identf_d = din("ident_f", [128, 128])
    cb_d = din("cb", [128, 6, 128])
    ebase_d = din("ebase", [128, 128])
    sel96_d = din("sel96", [32, 96])
    tabs_d = din("tabs", [128, 4, T_LAT])
    W = []
    for L in range(2):
        d = {}
        d["w_mod"] = din(f"l{L}_w_mod", [1024, 6144])
        d["vecs"] = din(f"l{L}_vecs", [67, 128])
        d["w_in"] = din(f"l{L}_w_in", [1024, 1184])
        d["w_uq"] = din(f"l{L}_w_uq", [256, 768])
        d["w_ukv"] = din(f"l{L}_w_ukv", [128, 1024])
        d["mla_q_gain"] = din(f"l{L}_mla_q_gain", [96, 1])
        d["mla_k_gain"] = din(f"l{L}_mla_k_gain", [96, 1])
        d["gqa_q_gain"] = din(f"l{L}_gqa_q_gain", [64, 1])
        d["gqa_k_gain"] = din(f"l{L}_gqa_k_gain", [64, 1])
        d["w_out"] = din(f"l{L}_w_out", [1024, 1024])
        if L == 0:
            d["wg"] = [din("l0_ffn_w_gate", [1024, FF])]
            d["wu"] = [din("l0_ffn_w_up", [1024, FF])]
            d["wd"] = [din("l0_ffn_w_down", [FF, 1024])]
        else:
            d["router"] = din("l1_router", [1024, 8])
            ne_ = 8 if (limit is None or limit > 12) else 1
            wg = din("l1_exp_w_gate", [ne_, 1024, FF])
            wu = din("l1_exp_w_up", [ne_, 1024, FF])
            wd = din("l1_exp_w_down", [ne_, FF, 1024])
            wg = [wg[min(e, ne_ - 1)] for e in range(8)]
            wu = [wu[min(e, ne_ - 1)] for e in range(8)]
            wd = [wd[min(e, ne_ - 1)] for e in range(8)]
            d["wg"] = wg
            d["wu"] = wu
            d["wd"] = wd
        W.append(d)
    out_d = nc.dram_tensor("out", [T_LAT, 1024], F32, kind="ExternalOutput").ap()
    xs = nc.dram_tensor("xs", [8, 128, T_ALL], F32, kind="Internal").ap()
    xs_v = xs.rearrange("k p t -> p k t")
    NSLOT = 8 * T_LAT
    Gd = nc.dram_tensor("Gd", [NSLOT, 1024], BF16, kind="Internal").ap()
    Yd = nc.dram_tensor("Yd", [NSLOT, 1024], F32, kind="Internal").ap()
    dbg_d = None
    if dbg is not None:
        dbg_d = nc.dram_tensor("dbg", [128, dbg], F32, kind="ExternalOutput").ap()

    P = Prog(nc)
    P.limit = limit
    P.oplimit = oplimit
    BLOCKS = [(0, 512), (512, 512), (1024, 512), (1536, 512), (2048, 256)]

    with ExitStack() as top:
        uid = [0]

        def sb(st, name, shape, dt):
            uid[0] += 1
            return st.enter_context(nc.sbuf_tensor(f"sb{uid[0]}_{name}", list(shape), dt))

        PS2 = [top.enter_context(nc.psum_tensor(f"ps{i}", [128, 1024], F32)) for i in range(4)]

        def bank(i):
            return PS2[i // 2][:, (i % 2) * 512:(i % 2) * 512 + 512]

        rr = [0]

        def nb():
            i = rr[0] % 8
            rr[0] += 1
            return i

        ident_f = sb(top, "ident_f", [128, 128], F32)
        cbt = sb(top, "cbt", [128, 6, 128], BF16)
        sel96 = sb(top, "sel96", [128, 96], BF16)
        epsc = sb(top, "epsc", [128, 1], F32)
        cols = sb(top, "cols", [128, 83], F32)
        modc = sb(top, "modc", [128, 48, 2], F32)
        A1 = sb(top, "A1", [128, 8, 2], F32)
        A2 = sb(top, "A2", [128, 8, 2], F32)
        gq = sb(top, "gq", [128, 4], F32)
        ident_b = cbt[:, 0, :]
        ones_b = cbt[:, 1, :]
        bonesB = cbt[:, 2, :]
        PA = cbt[:, 3, :]
        PB = cbt[:, 4, :]
        triU = cbt[:, 5, :]

        P.begin()
        P.dma("sp", DMA(ident_f[:], identf_d[:, :]), writes=["ident_f"], key="c0")
        P.dma("pool", DMA(cbt[:], cb_d[:, :, :]), writes=["cbt"], key="c1")
        P.dma("pool", DMA(sel96[0:32, :], sel96_d[:, :]), writes=["sel96"], key="c2")
        P.op("dve", MEMSET(epsc[:], EPS), writes=["epsc"])
        P.end()

        with ExitStack() as st:
            xin = [sb(st, f"xin{i}", [128, 1024], F32) for i in range(2)]
            xblk = sb(st, "xblk", [128, 8, 512], F32)
            P.begin()
            ti = 0
            for (c0, n) in BLOCKS:
                nt = n // 128
                for tt in range(nt):
                    tok0 = c0 + tt * 128
                    src = x_d[tok0:tok0 + 128, :] if tok0 < T_LAT else ctx_d[tok0 - T_LAT:tok0 - T_LAT + 128, :]
                    b_ = ti % 2
                    P.dma("sp", DMA(xin[b_][:], src), writes=[f"xin{b_}"], key=f"xin{b_}")
                    for k in range(8):
                        P.op("pe", TR(bank(k)[:, tt * 128:(tt + 1) * 128], xin[b_][:, k * 128:(k + 1) * 128], ident_f[:]),
                             reads=[f"xin{b_}", "ident_f"], writes=[f"B{k}"])
                    ti += 1
                for k in range(8):
                    eng = "act" if k % 2 == 0 else "dve"
                    fn = ACT(xblk[:, k, 0:n], bank(k)[:, 0:n], AF.Copy) if eng == "act" else CP(xblk[:, k, 0:n], bank(k)[:, 0:n])
                    P.op(eng, fn, reads=[f"B{k}"], writes=[f"xblk{k}"])
                P.dma("sp", DMA(xs_v[:, :, c0:c0 + n], xblk[:, :, 0:n]), reads=[f"xblk{k}" for k in range(8)], key="xo")
            P.end()

        for L in range(2):
            WL = W[L]
            last = (L == 1)
            with ExitStack() as st:
                stage = sb(st, "stage", [128, 128], F32)
                scb = sb(st, "scb", [128, 8, 2], BF16)
                wm = [sb(st, f"wm{i}", [128, 8, 1024], BF16) for i in range(2)]
                wmod_v = WL["w_mod"].rearrange("(k p) n -> p k n", p=128)
                P.begin()
                P.op("dve", MEMSET(stage[:], 0.0), writes=["stage"])
                P.dma("sp", DMA(stage[0:67, :], WL["vecs"][:, :]), reads=["stage"], writes=["stage"], key="st")
                P.dma("sp", DMA(stage[67:83, :], cvec_d[:, :]), writes=["stage"], key="st")
                P.dma("sp", DMAS([(gq[0:96, 0:1], WL["mla_q_gain"][:, :]), (gq[0:96, 1:2], WL["mla_k_gain"][:, :]),
                                  (gq[0:64, 2:3], WL["gqa_q_gain"][:, :]), (gq[64:128, 2:3], WL["gqa_q_gain"][:, :]),
                                  (gq[0:64, 3:4], WL["gqa_k_gain"][:, :]), (gq[64:128, 3:4], WL["gqa_k_gain"][:, :])]),
                      writes=["gq"], key="gq", n=6)
                b0 = nb()
                P.op("pe", TR(bank(b0)[:, 0:128], stage[:, :], ident_f[:, :]), reads=["stage", "ident_f"], writes=[f"B{b0}"])
                P.op("dve", CP(cols[:, :], bank(b0)[:, 0:83]), reads=[f"B{b0}"], writes=["cols"])
                P.op("act", ACT(scb[:, :, 0], cols[:, 67:75], AF.Silu), reads=["cols"], writes=["scb"])
                P.op("act", ACT(scb[:, :, 1], cols[:, 75:83], AF.Silu), reads=["scb", "cols"], writes=["scb"])
                bm = nb()
                psm = bank(bm)[:, 0:96].rearrange("p (m i) -> p m i", i=2)
                for sec in range(6):
                    b_ = sec % 2
                    P.dma("pool", DMAS([(wm[b_][:, k, :], wmod_v[:, k, sec * 1024:(sec + 1) * 1024]) for k in range(8)]),
                          writes=[f"wm{b_}"], key=f"wm{b_}", n=8)
                    for m in range(8):
                        for k in range(8):
                            P.op("pe", MM(psm[:, sec * 8 + m, :], wm[b_][:, k, m * 128:(m + 1) * 128], scb[:, k, :],
                                          start=(k == 0), stop=(k == 7)),
                                 reads=[f"wm{b_}", "scb"], writes=[f"B{bm}"])
                for i in range(2):
                    P.op("dve", TT(modc[:, :, i], psm[:, :, i], cols[:, 0:48], ALU.add), reads=[f"B{bm}", "cols"], writes=["modc"])
                for i in range(2):
                    P.op("dve", STT(A1[:, :, i], modc[:, 8:16, i], 1.0, cols[:, 48:56], ALU.add, ALU.mult),
                         reads=["modc", "cols"], writes=["A1"])
                    P.op("dve", STT(A2[:, :, i], modc[:, 32:40, i], 1.0, cols[:, 56:64], ALU.add, ALU.mult),
                         reads=["modc", "cols"], writes=["A2"])
                P.end()

            def Bcol(sec, k, i):
                return modc[:, sec * 8 + k, i:i + 1]

            def mk_sets(st, ns):
                sets = []
                for i_ in range(ns):
                    sets.append(dict(i=i_, sq=sb(st, f"sq{i_}", [128, 512], BF16), ms=sb(st, f"ms{i_}", [128, 512], F32),
                                     kn=sb(st, f"kn{i_}", [128, 512], BF16), t1=sb(st, f"t1{i_}", [128, 512], BF16),
                                     t2=sb(st, f"t2{i_}", [128, 512], BF16)))
                return sets

            job = [0]

            def next_set(SR):
                S = SR[job[0] % len(SR)]
                job[0] += 1
                return S

            def rstd_from(srcs, rows, n, inv_sqrt_d, ones_ap, sqs, ms):
                bi = nb()
                mst, msk = ms
                for i, (ap, rk) in enumerate(srcs):
                    s_, sk = sqs[i % len(sqs)]
                    P.op("act", ACT(s_[0:rows, 0:n], ap, AF.Square, scale=inv_sqrt_d), reads=rk, writes=[sk])
                    P.op("pe", MM(bank(bi)[0:rows, 0:n], ones_ap, s_[0:rows, 0:n], start=(i == 0), stop=(i == len(srcs) - 1)),
                         reads=[sk, "cbt"], writes=[f"B{bi}"])
                P.op("act", ACT(mst[0:rows, 0:n], bank(bi)[0:rows, 0:n], AF.Ln, bias=epsc[0:rows, 0:1], scale=1.0),
                     reads=[f"B{bi}", "epsc"], writes=[msk])
                P.op("act", ACT(mst[0:rows, 0:n], mst[0:rows, 0:n], AF.Exp, scale=-0.5), reads=[msk], writes=[msk])

            def norm_mod(xj, n, Acol, sec, i, hj, SR, xk="xj", hk="hj"):
                rstd_from([(xj[:, k, 0:n], [xk]) for k in range(8)], 128, n, 1.0 / 32.0, ones_b,
                          [(SR[0]["sq"], "sq0"), (SR[1]["sq"], "sq1")], (SR[2]["ms"], "ms2"))
                for k in range(8):
                    t_, tk = SR[k % 2]["ms"], f"ms{k % 2}"
                    P.op("dve", STT(t_[:, 0:n], xj[:, k, 0:n], Acol[:, k, i:i + 1], SR[2]["ms"][:, 0:n], ALU.mult, ALU.mult),
                         reads=[xk, "ms2", "A1", "A2"], writes=[tk])
                    if k % 2 == 0:
                        P.op("dve", TS(hj[:, k, 0:n], t_[:, 0:n], Bcol(sec, k, i), None, ALU.add),
                             reads=[tk, "modc"], writes=[f"{hk}{k}"])
                    else:
                        P.op("act", ACT(hj[:, k, 0:n], t_[:, 0:n], AF.Identity, bias=Bcol(sec, k, i), scale=1.0),
                             reads=[tk, "modc"], writes=[f"{hk}{k}"])

            with ExitStack() as att:
                tabs = sb(att, "tabs", [128, 4, T_LAT], BF16)
                KaT = sb(att, "KaT", [128, 8, T_ALL], BF16)
                KbT = sb(att, "KbT", [128, 2, T_ALL], BF16)
                Vst = sb(att, "Vst", [128, 18, 1152], BF16)
                cosA, sinA, cosB, sinB = tabs[:, 0, :], tabs[:, 1, :], tabs[:, 2, :], tabs[:, 3, :]

                def head_norm_rope(pre_bank, rows, n, inv_sqrt_d, ones_ap, gcol, perm, cos_t, sin_t, c0, dst, rope,
                                   S, dstkey):
                    si = S["i"]
                    rstdt, knt, t1t, t2t = S["ms"], S["kn"], S["t1"], S["t2"]
                    pre = bank(pre_bank)[0:rows, 0:n]
                    rstd_from([(pre, [f"B{pre_bank}"])], rows, n, inv_sqrt_d, ones_ap, [(S["sq"], f"sq{si}")], (rstdt, f"ms{si}"))
                    if not rope:
                        P.op("dve", STT(dst, pre, gcol, rstdt[0:rows, 0:n], ALU.mult, ALU.mult),
                             reads=[f"B{pre_bank}", f"ms{si}", "gq"], writes=[dstkey])
                        return
                    P.op("dve", STT(knt[0:rows, 0:n], pre, gcol, rstdt[0:rows, 0:n], ALU.mult, ALU.mult),
                         reads=[f"B{pre_bank}", f"ms{si}", "gq"], writes=[f"kn{si}"])
                    br = pre_bank
                    P.op("pe", MM(bank(br)[0:rows, 0:n], perm[0:rows, 0:rows], knt[0:rows, 0:n]),
                         reads=[f"kn{si}", "cbt"], writes=[f"B{br}"])
                    P.op("pool", TT(t1t[0:rows, 0:n], knt[0:rows, 0:n], cos_t[0:rows, c0:c0 + n], ALU.mult),
                         reads=[f"kn{si}", "tabs"], writes=[f"t1{si}"])
                    P.op("dve", TT(t2t[0:rows, 0:n], bank(br)[0:rows, 0:n], sin_t[0:rows, c0:c0 + n], ALU.mult),
                         reads=[f"B{br}", "tabs"], writes=[f"t2{si}"])
                    P.op("dve", TT(dst, t1t[0:rows, 0:n], t2t[0:rows, 0:n], ALU.add),
                         reads=[f"t1{si}", f"t2{si}"], writes=[dstkey])

                def head_job(SRl, pre_mm, pre_reads, rows, n, inv_sqrt_d, ones_ap, gcol, perm, cos_t, sin_t, c0, dst, rope, dstkey):
                    stt = {}

                    def A():
                        S = next_set(SRl)
                        bp = nb()
                        stt["S"], stt["bp"] = S, bp
                        pre_mm(bp)
                        P.op("act", ACT(S["sq"][0:rows, 0:n], bank(bp)[0:rows, 0:n], AF.Square, scale=inv_sqrt_d),
                             reads=[f"B{bp}"], writes=[f"sq{S['i']}"])

                    def B():
                        S, bp = stt["S"], stt["bp"]
                        si = S["i"]
                        pre = bank(bp)[0:rows, 0:n]
                        ms = S["ms"][0:rows, 0:n]
                        bi = nb()
                        P.op("pe", MM(bank(bi)[0:rows, 0:n], ones_ap, S["sq"][0:rows, 0:n]), reads=[f"sq{si}", "cbt"], writes=[f"B{bi}"])
                        P.op("act", ACT(ms, bank(bi)[0:rows, 0:n], AF.Ln, bias=epsc[0:rows, 0:1], scale=1.0),
                             reads=[f"B{bi}", "epsc"], writes=[f"ms{si}"])
                        P.op("act", ACT(ms, ms, AF.Exp, scale=-0.5), reads=[f"ms{si}"], writes=[f"ms{si}"])
                        if not rope:
                            P.op("dve", STT(dst, pre, gcol, ms, ALU.mult, ALU.mult), reads=[f"B{bp}", f"ms{si}", "gq"], writes=[dstkey])
                        else:
                            P.op("dve", STT(S["kn"][0:rows, 0:n], pre, gcol, ms, ALU.mult, ALU.mult),
                                 reads=[f"B{bp}", f"ms{si}", "gq"], writes=[f"kn{si}"])

                    def C():
                        if not rope:
                            return
                        S, bp = stt["S"], stt["bp"]
                        si = S["i"]
                        knt, t1t, t2t = S["kn"], S["t1"], S["t2"]
                        P.op("pe", MM(bank(bp)[0:rows, 0:n], perm[0:rows, 0:rows], knt[0:rows, 0:n]),
                             reads=[f"kn{si}", "cbt"], writes=[f"B{bp}"])
                        P.op("pool", TT(t1t[0:rows, 0:n], knt[0:rows, 0:n], cos_t[0:rows, c0:c0 + n], ALU.mult),
                             reads=[f"kn{si}", "tabs"], writes=[f"t1{si}"])
                        P.op("dve", TT(t2t[0:rows, 0:n], bank(bp)[0:rows, 0:n], sin_t[0:rows, c0:c0 + n], ALU.mult),
                             reads=[f"B{bp}", "tabs"], writes=[f"t2{si}"])
                        P.op("dve", TT(dst, t1t[0:rows, 0:n], t2t[0:rows, 0:n], ALU.add),
                             reads=[f"t1{si}", f"t2{si}"], writes=[dstkey])
                    return [A, B, C]

                def run_pipeline(jobs):
                    ns = 3
                    for t in range(len(jobs) + ns - 1):
                        for s in range(ns):
                            j = t - s
                            if 0 <= j < len(jobs):
                                jobs[j][s]()

                with ExitStack() as st:
                    w_in = sb(st, "w_in", [128, 8, 160], BF16)
                    WKB = sb(st, "WKB", [128, 8, 2, 128], BF16)
                    WVB = sb(st, "WVB", [128, 8, 128], BF16)
                    WN = sb(st, "WN", [128, 8, 96], BF16)
                    WV = sb(st, "WV", [128, 8, 64], BF16)
                    xjs = [sb(st, f"xj{i}", [128, 8, 512], F32) for i in range(2)]
                    hjs = [sb(st, f"hj{i}", [128, 8, 512], BF16) for i in range(2)]
                    ckvn2 = [sb(st, f"ckvn{i}", [128, 512], BF16) for i in range(2)]
                    krope2 = [sb(st, f"krope{i}", [128, 512], BF16) for i in range(2)]
                    SR = mk_sets(st, 4)
                    win_v = WL["w_in"].rearrange("(k p) n -> p k n", p=128)
                    wukv_v = WL["w_ukv"].rearrange("p (h c) -> p h c", c=128)
                    P.begin()
                    P.dma("pool", DMA(tabs[:], tabs_d[:, :, :]), writes=["tabs"], key="tabs")
                    P.dma("pool", DMAS([(w_in[:, k, 0:160], win_v[:, k, 256:416]) for k in range(8)]), writes=["w_in"], key="w_in", n=8)
                    P.dma("pool", DMAS([(WKB[:, k, g, h_ * 64:(h_ + 1) * 64], win_v[:, k, 928 + g * 64:928 + (g + 1) * 64])
                                        for k in range(8) for g in range(2) for h_ in range(2)]), writes=["WKB"], key="WKB", n=32)
                    P.dma("pool", DMAS([(WVB[:, k, :], win_v[:, k, 1056:1184]) for k in range(8)]), writes=["WVB"], key="WVB", n=8)
                    P.op("dve", MEMSET(WN[:], 0.0), writes=["WN"])
                    P.dma("pool", DMA(WN[:, :, 0:64], wukv_v[:, :, 0:64]), reads=["WN"], writes=["WN"], key="WN")
                    P.dma("pool", DMA(WV[:, :, :], wukv_v[:, :, 64:128]), writes=["WV"], key="WV")
                    P.op("pool", MEMSET(Vst[:], 1.0), writes=["Vst"])
                    if L == 0:
                        zt = sb(st, "zt", [128, 4, 1024], BF16)
                        P.op("pool", MEMSET(zt[:], 0.0), writes=["zt"])
                    def kv_block(bj_, c0, n):
                        i_mod = 0 if c0 < T_LAT else 1
                        rope = c0 < T_LAT
                        p2 = bj_ % 2
                        xj = xjs[p2]
                        hj = hjs[p2]
                        ckvn = ckvn2[p2]
                        krope = krope2[p2]
                        xk_ = f"xj{p2}"
                        hk_ = f"hj{p2}_"
                        ck_ = f"ckvn{p2}"
                        kr_ = f"krope{p2}"

                        def preA():
                            P.dma("sp", DMA(xj[:, :, 0:n], xs_v[:, :, c0:c0 + n]), writes=[xk_], key=xk_)
                            if L == 0 and bj_ >= 1:
                                for zi in range((bj_ - 1) * 8, bj_ * 8):
                                    P.dma("sp", DMA(Gd[zi * 512:(zi + 1) * 512, :].rearrange("(s p) d -> p s d", p=128), zt[:, :, :]),
                                          reads=["zt"], key="zf")
                            norm_mod(xj, n, A1, 0, i_mod, hj, SR, xk=xk_, hk=hk_)

                        def preB():
                            bc = nb()
                            for k in range(8):
                                P.op("pe", MM(bank(bc)[:, 0:n], w_in[:, k, 0:128], hj[:, k, 0:n], start=(k == 0), stop=(k == 7)),
                                     reads=["w_in", f"{hk_}{k}"], writes=[f"B{bc}"])
                            S_ = next_set(SR)
                            rstd_from([(bank(bc)[:, 0:n], [f"B{bc}"])], 128, n, 128 ** -0.5, ones_b, [(S_["sq"], f"sq{S_['i']}")], (S_["ms"], f"ms{S_['i']}"))
                            P.op("dve", STT(ckvn[:, 0:n], bank(bc)[:, 0:n], cols[:, 66:67], S_["ms"][:, 0:n], ALU.mult, ALU.mult),
                                 reads=[f"B{bc}", f"ms{S_['i']}", "cols"], writes=[ck_])
                            bk = nb()
                            for k in range(8):
                                P.op("pe", MM(bank(bk)[0:32, 0:n], w_in[:, k, 128:160], hj[:, k, 0:n], start=(k == 0), stop=(k == 7)),
                                     reads=["w_in", f"{hk_}{k}"], writes=[f"B{bk}"])
                            P.op("act", ACT(krope[0:32, 0:n], bank(bk)[0:32, 0:n], AF.Copy), reads=[f"B{bk}"], writes=[kr_])

                        def main():
                            jobs = []
                            for h in range(8):
                                def pre_mla(bp, h=h):
                                    P.op("pe", MM(bank(bp)[0:96, 0:n], WN[:, h, :], ckvn[:, 0:n], start=True, stop=False),
                                         reads=["WN", ck_], writes=[f"B{bp}"])
                                    P.op("pe", MM(bank(bp)[0:96, 0:n], sel96[0:32, :], krope[0:32, 0:n], start=False, stop=True),
                                         reads=["sel96", kr_], writes=[f"B{bp}"])
                                jobs.append(head_job(SR, pre_mla, None, 96, n, 96 ** -0.5, ones_b[0:96, 0:96], gq[0:96, 1:2], PA, cosA, sinA, c0,
                                                     KaT[0:96, h, c0:c0 + n], rope, "KaT"))
                            for g in range(2):
                                def pre_gqa(bp, g=g):
                                    for k in range(8):
                                        P.op("pe", MM(bank(bp)[:, 0:n], WKB[:, k, g, :], hj[:, k, 0:n], start=(k == 0), stop=(k == 7)),
                                             reads=["WKB", f"{hk_}{k}"], writes=[f"B{bp}"])
                                jobs.append(head_job(SR, pre_gqa, None, 128, n, 0.125, bonesB, gq[:, 3:4], PB, cosB, sinB, c0,
                                                     KbT[:, g, c0:c0 + n], rope, "KbT"))
                            run_pipeline(jobs)

                        def vals():
                            for tt in range(n // 128):
                                kc = (c0 + tt * 128) // 128
                                bv = nb()
                                P.op("pe", MM(bank(bv)[:, 0:512], ckvn[:, tt * 128:(tt + 1) * 128], WV[:, :, :].rearrange("p h c -> p (h c)"),
                                              start=True, stop=True), reads=[ck_, "WV"], writes=[f"B{bv}"])
                                src = bank(bv)[:, 0:512].rearrange("p (a s c) -> p a s c", s=2, c=64)
                                dst = Vst[:, kc, 0:768].rearrange("p (a s c) -> p a s c", s=3, c=64)[:, :, 0:3:2, :]
                                P.op("act", ACT(dst, src, AF.Copy), reads=[f"B{bv}"], writes=["Vst"])
                                bw = nb()
                                for k in range(8):
                                    P.op("pe", MM(bank(bw)[:, 0:128], hj[:, k, tt * 128:(tt + 1) * 128], WVB[:, k, :], start=(k == 0), stop=(k == 7)),
                                         reads=["WVB", f"{hk_}{k}"], writes=[f"B{bw}"])
                                srcb = bank(bw)[:, 0:128].rearrange("p (g c) -> p g c", c=64)
                                for s_ in (0, 2):
                                    dstb = Vst[:, kc, 768:1152].rearrange("p (g s c) -> p g s c", s=3, c=64)[:, :, s_, :]
                                    P.op("dve", CP(dstb, srcb), reads=[f"B{bw}"], writes=["Vst"])
                        return preA, preB, main, vals

                    kvb = [kv_block(bj_, c0, n) for bj_, (c0, n) in enumerate(BLOCKS)]
                    kvb[0][0]()
                    kvb[0][1]()
                    for bj_ in range(len(BLOCKS)):
                        if bj_ + 1 < len(BLOCKS):
                            kvb[bj_ + 1][0]()
                        kvb[bj_][2]()
                        if bj_ + 1 < len(BLOCKS):
                            kvb[bj_ + 1][1]()
                        kvb[bj_][3]()
                    P.end()

                with ExitStack() as st:
                    wq = sb(st, "wq", [128, 8, 768], BF16)
                    wuq = sb(st, "wuq", [128, 2, 768], BF16)
                    wout = sb(st, "wout", [128, 8, 1024], BF16)
                    xj = sb(st, "xj", [128, 8, 512], F32)
                    hj = sb(st, "hj", [128, 8, 512], BF16)
                    QaT = sb(st, "QaT", [128, 8, 512], BF16)
                    QbT = sb(st, "QbT", [128, 4, 512], BF16)
                    cqn = sb(st, "cqn", [128, 2, 512], BF16)
                    PT = [sb(st, f"PT{i}", [128, 2, 512], BF16) for i in range(3)]
                    SR = mk_sets(st, 4)
                    rden = [SR[0]["ms"], SR[1]["ms"]]
                    win_v = WL["w_in"].rearrange("(k p) n -> p k n", p=128)
                    wuq_v = WL["w_uq"].rearrange("(k p) n -> p k n", p=128)
                    wout_v = WL["w_out"].rearrange("(k p) n -> p k n", p=128)
                    P.begin()
                    P.dma("pool", DMAS([(wq[:, k, 0:256], win_v[:, k, 0:256]) for k in range(8)]), writes=["wq"], key="wq", n=8)
                    P.dma("pool", DMAS([(wq[:, k, 256:768], win_v[:, k, 416:928]) for k in range(8)]), reads=["wq"], writes=["wq"], key="wq", n=8)
                    P.dma("pool", DMAS([(wuq[:, k, :], wuq_v[:, k, :]) for k in range(2)]), writes=["wuq"], key="wuq", n=2)
                    P.dma("pool", DMAS([(wout[:, k, :], wout_v[:, k, :]) for k in range(8)]), writes=["wout"], key="wout", n=8)
                    qblocks = BLOCKS[:4] if last else BLOCKS
                    for (c0, n) in qblocks:
                        lat = c0 < T_LAT
                        i_mod = 0 if lat else 1
                        P.dma("sp", DMA(xj[:, :, 0:n], xs_v[:, :, c0:c0 + n]), writes=["xj"], key="xj")
                        norm_mod(xj, n, A1, 0, i_mod, hj, SR)
                        bq = [nb(), nb()]
                        for c_ in range(2):
                            for k in range(8):
                                P.op("pe", MM(bank(bq[c_])[:, 0:n], wq[:, k, c_ * 128:(c_ + 1) * 128], hj[:, k, 0:n], start=(k == 0), stop=(k == 7)),
                                     reads=["wq", f"hj{k}"], writes=[f"B{bq[c_]}"])
                        S_ = next_set(SR)
                        S2_ = next_set(SR)
                        rstd_from([(bank(bq[c_])[:, 0:n], [f"B{bq[c_]}"]) for c_ in range(2)], 128, n, 1.0 / 16.0, ones_b,
                                  [(S_["sq"], f"sq{S_['i']}"), (S2_["sq"], f"sq{S2_['i']}")], (S_["ms"], f"ms{S_['i']}"))
                        for c_ in range(2):
                            P.op("dve", STT(cqn[:, c_, 0:n], bank(bq[c_])[:, 0:n], cols[:, 64 + c_:65 + c_], S_["ms"][:, 0:n], ALU.mult, ALU.mult),
                                 reads=[f"B{bq[c_]}", f"ms{S_['i']}", "cols"], writes=["cqn"])
                        jobs = []
                        for h in range(8):
                            def pre_q(bp, h=h, n=n):
                                for c_ in range(2):
                                    P.op("pe", MM(bank(bp)[0:96, 0:n], wuq[:, c_, h * 96:(h + 1) * 96], cqn[:, c_, 0:n], start=(c_ == 0), stop=(c_ == 1)),
                                         reads=["wuq", "cqn"], writes=[f"B{bp}"])
                            jobs.append(head_job(SR, pre_q, None, 96, n, 96 ** -0.5, ones_b[0:96, 0:96], gq[0:96, 0:1], PA, cosA, sinA, c0,
                                                 QaT[0:96, h, 0:n], lat, f"QaT{h}"))
                        for m in range(4):
                            def pre_qb(bp, m=m, n=n):
                                for k in range(8):
                                    P.op("pe", MM(bank(bp)[:, 0:n], wq[:, k, 256 + m * 128:256 + (m + 1) * 128], hj[:, k, 0:n], start=(k == 0), stop=(k == 7)),
                                         reads=["wq", f"hj{k}"], writes=[f"B{bp}"])
                            jobs.append(head_job(SR, pre_qb, None, 128, n, 0.125, bonesB, gq[:, 2:3], PB, cosB, sinB, c0,
                                                 QbT[:, m, 0:n], lat, f"QbT{m}"))
                        run_pipeline(jobs)
                        kchunks = list(range(18)) if lat else [16, 17]
                        items = []
                        for hh in range(16):
                            for pi in range(0, len(kchunks), 2):
                                items.append((hh, kchunks[pi:pi + 2], pi == 0, pi + 2 >= len(kchunks)))

                        def head_ops(hh):
                            if hh < 8:
                                return (lambda kc: KaT[0:96, hh, kc * 128:(kc + 1) * 128], QaT[0:96, hh, 0:n], f"QaT{hh}", "KaT",
                                        96 ** -0.5, (hh // 2) * 192 + (hh % 2) * 64, hh // 2, hh % 2)
                            q = hh - 8
                            kv = q // 4
                            r0 = (q % 2) * 64
                            return (lambda kc: KbT[r0:r0 + 64, kv, kc * 128:(kc + 1) * 128], QbT[r0:r0 + 64, q // 2, 0:n], f"QbT{q // 2}", "KbT",
                                    0.125, 768 + kv * 192 + (q % 2) * 64, 4 + q // 2, q % 2)

                        SB = [(0, 1), (2, 3), (4, 5)]
                        OB = [6, 7]
                        pend = []
                        for it, (hh, kcs, first, lastp) in enumerate(items):
                            kfn, qap, qkey, kkey, scl, voff, ochunk, par = head_ops(hh)
                            sp_ = it % 3
                            ps2 = PS2[sp_]
                            for i_, kc in enumerate(kcs):
                                P.op("pe", MM(ps2[:, i_ * 512:i_ * 512 + n], kfn(kc), qap, start=True, stop=True),
                                     reads=[kkey, qkey], writes=[f"B{SB[sp_][i_]}"])
                            if len(pend) >= 2:
                                pend.pop(0)()
                            ptv = PT[sp_]
                            nk = len(kcs)
                            P.op("act", ACT(ptv[:, 0:nk, 0:n], ps2[:, 0:nk * 512].rearrange("p (a c) -> p a c", c=512)[:, :, 0:n], AF.Exp, scale=scl),
                                 reads=[f"B{SB[sp_][i_]}" for i_ in range(nk)], writes=[f"PT{sp_}"])

                            def mk(hh=hh, kcs=kcs, first=first, lastp=lastp, sp_=sp_, voff=voff, ochunk=ochunk, par=par):
                                def run():
                                    ob = OB[hh % 2]
                                    for i_, kc in enumerate(kcs):
                                        P.op("pe", MM(bank(ob)[:, 0:n], Vst[:, kc, voff:voff + 128], PT[sp_][:, i_, 0:n],
                                                      start=(first and i_ == 0), stop=(lastp and i_ == len(kcs) - 1)),
                                             reads=["Vst", f"PT{sp_}"], writes=[f"B{ob}"])
                                    if lastp:
                                        o0 = par * 64
                                        d0 = 64 - o0
                                        rd = rden[hh % 2]
                                        P.op("dve", RECIP(rd[o0:o0 + 64, 0:n], bank(ob)[d0:d0 + 64, 0:n]), reads=[f"B{ob}"], writes=[f"ms{hh % 2}"])
                                        P.op("dve", TT(hj[o0:o0 + 64, ochunk, 0:n], bank(ob)[o0:o0 + 64, 0:n], rd[o0:o0 + 64, 0:n], ALU.mult),
                                             reads=[f"B{ob}", f"ms{hh % 2}"], writes=[f"hj{ochunk}"])
                                return run
                            pend.append(mk())
                        while pend:
                            pend.pop(0)()
                        rr[0] = 0
                        for m in range(8):
                            bo = m % 6
                            for c_ in range(8):
                                P.op("pe", MM(bank(bo)[:, 0:n], wout[:, c_, m * 128:(m + 1) * 128], hj[:, c_, 0:n], start=(c_ == 0), stop=(c_ == 7)),
                                     reads=["wout", f"hj{c_}"], writes=[f"B{bo}"])
                            P.op("dve", STT(xj[:, m, 0:n], bank(bo)[:, 0:n], Bcol(2, m, i_mod), xj[:, m, 0:n], ALU.mult, ALU.add),
                                 reads=[f"B{bo}", "xj", "modc"], writes=["xj"])
                        P.dma("sp", DMA(xs_v[:, :, c0:c0 + n], xj[:, :, 0:n]), reads=["xj"], key="xo")
                    P.end()

            if last:
                with ExitStack() as ffn:
                    ridx = sb(ffn, "ridx", [128, 2, 16], I32)
                    wts = sb(ffn, "wts", [128, 2, 16], F32)
                    flags = sb(ffn, "flags", [128, 48], I32)
                    P.flags_ap = flags
                    with ExitStack() as st:
                        SR = mk_sets(st, 4)
                        xT = sb(st, "xT", [128, 8, T_LAT], F32)
                        h2T = sb(st, "h2T", [128, 8, T_LAT], BF16)
                        hf = sb(st, "hf", [128, 8, 512], F32)
                        rt = sb(st, "rt", [128, 8, 8], F32)
                        Lg = sb(st, "Lg", [128, 16, 8], F32)
                        L2 = sb(st, "L2", [128, 16, 8], F32)
                        mk1 = sb(st, "mk1", [128, 16, 8], F32)
                        mk2 = sb(st, "mk2", [128, 16, 8], F32)
                        sm = sb(st, "sm", [128, 4, 16], F32)
                        maskb = sb(st, "maskb", [128, 16, 8], BF16)
                        tot_s = sb(st, "tot_s", [128, 16, 8], F32)
                        cum = sb(st, "cum", [128, 17, 8], F32)
                        val = sb(st, "val", [128, 16, 8], F32)
                        tmp = sb(st, "tmp", [128, 16, 8], F32)
                        rf = sb(st, "rf", [128, 2, 16], F32)
                        flagsf = sb(st, "flagsf", [128, 8, 6], F32)
                        ebase = sb(st, "ebase", [128, 16, 8], F32)
                        htok = [sb(st, f"htok{i}", [128, 1024], BF16) for i in range(2)]
                        fblocks = BLOCKS[:4]
                        P.begin()
                        for bi_, (c0, n) in enumerate(fblocks):
                            P.dma("sp", DMA(xT[:, :, c0:c0 + n], xs_v[:, :, c0:c0 + n]), writes=[f"xT{bi_}"], key=f"xT{bi_}")
                        P.dma("sp", DMA(rt[:], WL["router"].rearrange("(k p) e -> p k e", p=128)), writes=["rt"], key="rt")
                        P.dma("sp", DMA(ebase[:].rearrange("p a b -> p (a b)"), ebase_d[:, :]), writes=["ebase"], key="eb")
                        for bi_, (c0, n) in enumerate(fblocks):
                            r3 = bi_ % 2
                            rstd_from([(xT[:, k, c0:c0 + n], [f"xT{bi_}"]) for k in range(8)], 128, n, 1.0 / 32.0, ones_b,
                                      [(SR[0]["sq"], "sq0"), (SR[1]["sq"], "sq1")], (SR[2 + r3]["ms"], f"ms{2 + r3}"))
                            rst_ = SR[2 + r3]["ms"]
                            for k in range(8):
                                t_, tk = SR[k % 2]["ms"], f"ms{k % 2}"
                                P.op("dve", STT(t_[:, 0:n], xT[:, k, c0:c0 + n], A2[:, k, 0:1], rst_[:, 0:n], ALU.mult, ALU.mult),
                                     reads=[f"xT{bi_}", f"ms{2 + r3}", "A2"], writes=[tk])
                                P.op("act", ACT(hf[:, k, 0:n], t_[:, 0:n], AF.Identity, bias=Bcol(3, k, 0), scale=1.0),
                                     reads=[tk, "modc"], writes=[f"hf{k}"])
                                P.op("pool", CP(h2T[:, k, c0:c0 + n], hf[:, k, 0:n]), reads=[f"hf{k}"], writes=[f"h2T{k}"])
                            for tt in range(n // 128):
                                tg = (c0 // 128) + tt
                                bl = nb()
                                for k in range(8):
                                    P.op("pe", MM(bank(bl)[:, 0:8], hf[:, k, tt * 128:(tt + 1) * 128], rt[:, k, :], start=(k == 0), stop=(k == 7)),
                                         reads=[f"hf{k}", "rt"], writes=[f"B{bl}"])
                                P.op("dve", CP(Lg[:, tg, :], bank(bl)[:, 0:8]), reads=[f"B{bl}"], writes=["Lg"])
                        m1, m2, df, e2 = (sm[:, i, :] for i in range(4))
                        g1, g2 = wts[:, 0, :], wts[:, 1, :]
                        P.op("dve", RMAX(m1, Lg[:, :, :]), reads=["Lg"], writes=["m1"])
                        for e_ in range(8):
                            P.op("dve", TT(mk1[:, :, e_], Lg[:, :, e_], m1, ALU.is_equal), reads=["Lg", "m1"], writes=["mk1"])
                        P.op("dve", STT(L2[:, :, :], mk1[:, :, :], -1e30, Lg[:, :, :], ALU.mult, ALU.add), reads=["mk1", "Lg"], writes=["L2"])
                        P.op("dve", RMAX(m2, L2[:, :, :]), reads=["L2"], writes=["m2"])
                        for e_ in range(8):
                            P.op("dve", TT(mk2[:, :, e_], L2[:, :, e_], m2, ALU.is_equal), reads=["L2", "m2"], writes=["mk2"])
                        P.op("dve", TT(df, m2, m1, ALU.subtract), reads=["m1", "m2"], writes=["df"])
                        P.op("act", ACT(e2, df, AF.Exp), reads=["df"], writes=["e2"])
                        P.op("dve", TS(g1, e2, 1.0, None, ALU.add), reads=["e2"], writes=["wts"])
                        P.op("dve", RECIP(g1, g1), reads=["wts"], writes=["wts"])
                        P.op("dve", TT(g2, e2, g1, ALU.mult), reads=["e2", "wts"], writes=["wts"])
                        P.op("dve", TT(maskb[:, :, :], mk1[:, :, :], mk2[:, :, :], ALU.add), reads=["mk1", "mk2"], writes=["maskb"])
                        mflat = maskb[:, :, :].rearrange("p a b -> p (a b)")
                        bw_, bt_ = nb(), nb()
                        P.op("pe", MM(bank(bw_)[:, 0:128], triU, mflat), reads=["cbt", "maskb"], writes=[f"B{bw_}"])
                        P.op("pe", MM(bank(bt_)[:, 0:128], ones_b, mflat), reads=["cbt", "maskb"], writes=[f"B{bt_}"])
                        P.op("dve", CP(tot_s[:, :, :].rearrange("p a b -> p (a b)"), bank(bt_)[:, 0:128]), reads=[f"B{bt_}"], writes=["tot_s"])
                        P.op("dve", MEMSET(cum[:, 0, :], 0.0), writes=["cum"])
                        for tg in range(16):
                            P.op("dve", TT(cum[:, tg + 1, :], cum[:, tg, :], tot_s[:, tg, :], ALU.add), reads=["cum", "tot_s"], writes=["cum"])
                        P.op("dve", TT(val[:, :, :].rearrange("p a b -> p (a b)"), bank(bw_)[:, 0:128],
                                       cum[:, 0:16, :].rearrange("p a b -> p (a b)"), ALU.add), reads=[f"B{bw_}", "cum"], writes=["val"])
                        P.op("dve", TT(val[:, :, :], val[:, :, :], ebase[:, :, :], ALU.add), reads=["val", "ebase"], writes=["val"])
                        for j, mk_ in enumerate((mk1, mk2)):
                            P.op("dve", TT(tmp[:, :, :], mk_[:, :, :], val[:, :, :], ALU.mult), reads=["val", f"mk{j + 1}"], writes=["tmp"])
                            P.op("dve", RSUM(rf[:, j, :], tmp[:, :, :]), reads=["tmp"], writes=["rf"])
                        P.op("dve", CP(ridx[:, :, :], rf[:, :, :]), reads=["rf"], writes=["ridx"])
                        for b_ in range(6):
                            P.op("dve", TS(flagsf[:, :, b_], cum[:, 16, :], 512.0 + 256.0 * b_, None, ALU.is_gt), reads=["cum"], writes=["flagsf"])
                        P.op("dve", CP(flags[:, :], flagsf[:, :, :].rearrange("p a b -> p (a b)")), reads=["flagsf"], writes=["flags"])
                        for tg in range(16):
                            pz = PS2[tg % 2]
                            bk0 = 2 * (tg % 2)
                            for k in range(8):
                                P.op("pe", MM(pz[:, k * 128:(k + 1) * 128], h2T[:, k, tg * 128:(tg + 1) * 128], ident_b),
                                     reads=[f"h2T{k}", "cbt"], writes=[f"B{bk0 + k // 4}"])
                            ht = htok[tg % 2]
                            P.op("act", ACT(ht[:, 0:512], pz[:, 0:512], AF.Copy), reads=[f"B{bk0}"], writes=[f"htok{tg % 2}a"])
                            P.op("dve", CP(ht[:, 512:1024], pz[:, 512:1024]), reads=[f"B{bk0 + 1}"], writes=[f"htok{tg % 2}b"])
                            for j in range(2):
                                P.dma("pool", ISCAT(Gd[:, :], ridx[:, j, tg:tg + 1], ht[:, :], P.bc),
                                      reads=[f"htok{tg % 2}a", f"htok{tg % 2}b", "ridx"], key=f"sc{tg % 2}")
                        P.end()

                    GS = 4
                    groups = []
                    f0 = 0
                    while f0 < NFC:
                        gs = min(GS, NFC - f0)
                        groups.append((f0, gs))
                        f0 += gs
                    NRING = 4
                    with ExitStack() as st:
                        wgt = [sb(st, f"wgt{i}", [128, 8, GS * 128], BF16) for i in range(2)]
                        wut = [sb(st, f"wut{i}", [128, 8, GS * 128], BF16) for i in range(2)]
                        wdt = [sb(st, f"wdt{i}", [128, GS, 1024], BF16) for i in range(2)]
                        sgt = [sb(st, f"sgt{i}", [128, 512], F32) for i in range(2)]
                        actt = [sb(st, f"actt{i}", [128, GS, 512], BF16) for i in range(2)]
                        hT0 = [sb(st, f"hT0_{i}", [128, 8, 512], BF16) for i in range(2)]
                        hTx = [sb(st, f"hTx{i}", [128, 8, 256], BF16) for i in range(6)]
                        ya0 = sb(st, "ya0", [128, 4, 1024], F32)
                        yax = sb(st, "yax", [128, 12, 1024], F32)

                        def geom(blk):
                            return (0, 512) if blk == 0 else (512 + 256 * (blk - 1), 256)

                        def yslice(blk, s4, half):
                            if blk == 0:
                                return ya0[:, s4, half * 512:(half + 1) * 512]
                            return yax[:, (blk - 1) * 2 + s4, half * 512:(half + 1) * 512]
                        gtok = [sb(st, f"gtok{i}", [128, 1024], BF16) for i in range(NRING)]
                        P.begin()
                        ring = [0]
                        evq = [0]

                        def hbuf(e_, blk):
                            if blk == 0:
                                return hT0[e_ % 2], f"hT0_{e_ % 2}"
                            return hTx[blk - 1], f"hTx{blk - 1}"

                        def prep(e_, blk, cond):
                            hT, hkey = hbuf(e_, blk)
                            s0, W = geom(blk)
                            nst = W // 128
                            bufs = []
                            for s4 in range(nst):
                                r = ring[0] % NRING
                                ring[0] += 1
                                row0 = e_ * T_LAT + s0 + s4 * 128
                                P.dma("sp", DMA(gtok[r][:, :], Gd[row0:row0 + 128, :]), writes=[f"gt{r}"], key=f"gl{r}")
                                bufs.append(r)
                            for k in range(8):
                                bi = k % 4
                                for s4 in range(nst):
                                    P.op("pe", MM(bank(bi)[:, s4 * 128:(s4 + 1) * 128], gtok[bufs[s4]][:, k * 128:(k + 1) * 128], ident_b),
                                         reads=[f"gt{bufs[s4]}", "cbt"], writes=[f"B{bi}"], cond=cond)
                                evq[0] += 1
                                if evq[0] % 2 == 0:
                                    P.op("act", ACT(hT[:, k, 0:W], bank(bi)[:, 0:W], AF.Copy), reads=[f"B{bi}"], writes=[f"{hkey}_{k}"], cond=cond)
                                else:
                                    P.op("dve", CP(hT[:, k, 0:W], bank(bi)[:, 0:W]), reads=[f"B{bi}"], writes=[f"{hkey}_{k}"], cond=cond)

                        gi = 0
                        ai = 0
                        cq = [0]

                        def block_compute(e_, gidx, gs, b_, blk, cond):
                            nonlocal ai
                            hT, hkey = hbuf(e_, blk)
                            s0, W = geom(blk)
                            nst = W // 128
                            a_ = ai % 2
                            ai += 1
                            at = actt[a_]
                            for f in range(gs):
                                pg = (f % 2) * 2
                                pu = pg + 1
                                for k in range(8):
                                    P.op("pe", MM(bank(pg)[:, 0:W], wgt[b_][:, k, f * 128:(f + 1) * 128], hT[:, k, 0:W], start=(k == 0), stop=(k == 7)),
                                         reads=[f"wgt{b_}", f"{hkey}_{k}"], writes=[f"B{pg}"], cond=cond)
                                for k in range(8):
                                    P.op("pe", MM(bank(pu)[:, 0:W], wut[b_][:, k, f * 128:(f + 1) * 128], hT[:, k, 0:W], start=(k == 0), stop=(k == 7)),
                                         reads=[f"wut{b_}", f"{hkey}_{k}"], writes=[f"B{pu}"], cond=cond)
                                sg = sgt[f % 2]
                                P.op("act", ACT(sg[:, 0:W], bank(pg)[:, 0:W], AF.Silu), reads=[f"B{pg}"], writes=[f"sg{f % 2}"], cond=cond)
                                P.op("dve", TT(at[:, f, 0:W], sg[:, 0:W], bank(pu)[:, 0:W], ALU.mult),
                                     reads=[f"sg{f % 2}", f"B{pu}"], writes=[f"act{a_}_{f}"], cond=cond)
                            for half in range(2):
                                for s4 in range(nst):
                                    bd = 4 + (half * nst + s4) % 4
                                    for f in range(gs):
                                        P.op("pe", MM(bank(bd)[:, 0:512], at[:, f, s4 * 128:(s4 + 1) * 128], wdt[b_][:, f, half * 512:(half + 1) * 512],
                                                      start=(f == 0), stop=(f == gs - 1)),
                                             reads=[f"wdt{b_}", f"act{a_}_{f}"], writes=[f"B{bd}"], cond=cond)
                                    ya = yslice(blk, s4, half)
                                    yk = f"yacc{blk}_{s4}_{half}"
                                    if gidx == 0:
                                        cq[0] += 1
                                        if cq[0] % 2 == 0:
                                            P.op("act", ACT(ya, bank(bd)[:, 0:512], AF.Copy), reads=[f"B{bd}"], writes=[yk], cond=cond)
                                        else:
                                            P.op("dve", CP(ya, bank(bd)[:, 0:512]), reads=[f"B{bd}"], writes=[yk], cond=cond)
                                    else:
                                        P.op("dve", TT(ya, bank(bd)[:, 0:512], ya, ALU.add), reads=[f"B{bd}", yk], writes=[yk], cond=cond)

                        prep(0, 0, None)
                        for e_ in range(8):
                            wg_v = WL["wg"][e_].rearrange("(k p) f -> p k f", p=128)
                            wu_v = WL["wu"][e_].rearrange("(k p) f -> p k f", p=128)
                            wd_v = WL["wd"][e_].rearrange("(f p) d -> p f d", p=128)
                            for gidx, (f0, gs) in enumerate(groups):
                                b_ = gi % 2
                                gi += 1
                                P.dma("pool", DMAS([(wgt[b_][:, k, 0:gs * 128], wg_v[:, k, f0 * 128:(f0 + gs) * 128]) for k in range(8)]),
                                      writes=[f"wgt{b_}"], key=f"wg{b_}", n=8)
                                P.dma("pool", DMAS([(wut[b_][:, k, 0:gs * 128], wu_v[:, k, f0 * 128:(f0 + gs) * 128]) for k in range(8)]),
                                      writes=[f"wut{b_}"], key=f"wu{b_}", n=8)
                                P.dma("pool", DMAS([(wdt[b_][:, f, :], wd_v[:, f0 + f, :]) for f in range(gs)]),
                                      writes=[f"wdt{b_}"], key=f"wd{b_}", n=gs)
                                for blk in range(7):
                                    cond = None if blk == 0 else (e_, gidx, blk)
                                    if gidx == 0 and blk > 0:
                                        prep(e_, blk, cond)
                                    block_compute(e_, gidx, gs, b_, blk, cond)
                                if gidx == 3 and e_ < 7:
                                    prep(e_ + 1, 0, None)
                            row0 = e_ * T_LAT
                            P.dma("sp", DMA(Yd[row0:row0 + 512, :].rearrange("(s p) d -> p s d", p=128), ya0[:, :, :]),
                                  reads=[f"yacc0_{s4}_{half}" for s4 in range(4) for half in range(2)], key="yw0")
                            P.dma("sp", DMA(Yd[row0 + 512:row0 + T_LAT, :].rearrange("(s p) d -> p s d", p=128), yax[:, :, :]),
                                  reads=[f"yacc{blk}_{s4}_{half}" for blk in range(1, 7) for s4 in range(2) for half in range(2)], key="ywx")
                        P.end()

                    with ExitStack() as st:
                        xo = [sb(st, f"xo{i}", [128, 8, 512], F32) for i in range(2)]
                        yg = [[sb(st, f"yg{i}_{j}", [128, 1024], F32) for j in range(2)] for i in range(3)]
                        ttl = [sb(st, f"ttl{i}", [128, 1024], F32) for i in range(2)]
                        ot = [sb(st, f"ot{i}", [128, 1024], F32) for i in range(2)]
                        g2row = sb(st, "g2row", [128, 1024], F32)
                        dg = [sb(st, f"dg{i}", [128, 128], F32) for i in range(2)]
                        onesf = sb(st, "onesf", [128, 128], F32)
                        P.begin()
                        P.op("dve", MEMSET(onesf[:], 1.0), writes=["onesf"])
                        for m in range(8):
                            d_ = dg[m % 2]
                            P.op("dve", TS(d_[:, :], ident_f[:, :], Bcol(5, m, 0), None, ALU.mult), reads=["ident_f", "modc"], writes=[f"dg{m % 2}"])
                            bq_ = 6 + (m % 2)
                            P.op("pe", MM(bank(bq_)[:, 0:128], onesf[:, :], d_[:, :]), reads=["onesf", f"dg{m % 2}"], writes=[f"B{bq_}"])
                            P.op("act", ACT(g2row[:, m * 128:(m + 1) * 128], bank(bq_)[:, 0:128], AF.Copy), reads=[f"B{bq_}"], writes=["g2row"])
                        for tg in range(16):
                            b4 = tg // 4
                            xb = xo[b4 % 2]
                            if tg % 4 == 0:
                                P.dma("sp", DMA(xb[:, :, :], xs_v[:, :, b4 * 512:(b4 + 1) * 512]), writes=[f"xo{b4 % 2}"], key=f"xo{b4 % 2}")
                            pz = PS2[tg % 2]
                            bk0 = 2 * (tg % 2)
                            for k in range(8):
                                P.op("pe", TR(pz[:, k * 128:(k + 1) * 128], xb[:, k, (tg % 4) * 128:(tg % 4 + 1) * 128], ident_f[:, :]),
                                     reads=[f"xo{b4 % 2}", "ident_f"], writes=[f"B{bk0 + k // 4}"])
                            y0, y1 = yg[tg % 3]
                            for j in range(2):
                                P.dma("pool", IGATH(yg[tg % 3][j][:, :], Yd[:, :], ridx[:, j, tg:tg + 1], P.bc),
                                      reads=["ridx"], writes=[f"yg{tg % 3}_{j}"], key=f"yg{tg % 3}_{j}")
                            t_ = ttl[tg % 2]
                            tk = f"ttl{tg % 2}"
                            P.op("act", ACT(t_[:, :], y1[:, :], AF.Identity, scale=wts[:, 1, tg:tg + 1]), reads=[f"yg{tg % 3}_1", "wts"], writes=[tk])
                            P.op("dve", STT(t_[:, :], y0[:, :], wts[:, 0, tg:tg + 1], t_[:, :], ALU.mult, ALU.add),
                                 reads=[f"yg{tg % 3}_0", "wts", tk], writes=[tk])
                            P.op("dve", TT(t_[:, :], t_[:, :], g2row[:, :], ALU.mult), reads=[tk, "g2row"], writes=[tk])
                            o_ = ot[tg % 2]
                            P.op("dve", TT(o_[:, 0:512], t_[:, 0:512], pz[:, 0:512], ALU.add), reads=[tk, f"B{bk0}"], writes=[f"ot{tg % 2}a"])
                            P.op("dve", TT(o_[:, 512:1024], t_[:, 512:1024], pz[:, 512:1024], ALU.add), reads=[tk, f"B{bk0 + 1}"], writes=[f"ot{tg % 2}b"])
                            P.dma("sp", DMA(out_d[tg * 128:(tg + 1) * 128, :], o_[:, :]), reads=[f"ot{tg % 2}a", f"ot{tg % 2}b"], key=f"out{tg % 2}")
                        P.end()
                continue

            with ExitStack() as ffn:
                ntok = T_LAT if last else T_ALL
                fblocks = BLOCKS[:4] if last else BLOCKS
                xT = sb(ffn, "xT", [128, 8, ntok], F32)
                h2T = sb(ffn, "h2T", [128, 8, ntok], BF16)
                if last:
                    gT = sb(ffn, "gT", [128, T_LAT], F32)
                with ExitStack() as st:
                    SR = mk_sets(st, 4)
                    if last:
                        hf = sb(st, "hf", [128, 8, 512], F32)
                        rt = sb(st, "rt", [128, 8, 8], F32)
                        Lg = sb(st, "Lg", [128, 16, 8], F32)
                        L2 = sb(st, "L2", [128, 16, 8], F32)
                        mk1 = sb(st, "mk1", [128, 16, 8], F32)
                        mk2 = sb(st, "mk2", [128, 16, 8], F32)
                        gts = sb(st, "gts", [128, 16, 8], F32)
                        sm = sb(st, "sm", [128, 6, 16], F32)
                    P.begin()
                    for bi_, (c0, n) in enumerate(fblocks):
                        P.dma("sp", DMA(xT[:, :, c0:c0 + n], xs_v[:, :, c0:c0 + n]), writes=[f"xT{bi_}"], key=f"xT{bi_}")
                    if last:
                        P.dma("sp", DMA(rt[:], WL["router"].rearrange("(k p) e -> p k e", p=128)), writes=["rt"], key="rt")

                    for bi_, (c0, n) in enumerate(fblocks):
                        i_mod = 0 if c0 < T_LAT else 1

                        r3 = bi_ % 2
                        rstd_from([(xT[:, k, c0:c0 + n], [f"xT{bi_}"]) for k in range(8)], 128, n, 1.0 / 32.0, ones_b,
                                  [(SR[0]["sq"], "sq0"), (SR[1]["sq"], "sq1")], (SR[2 + r3]["ms"], f"ms{2 + r3}"))
                        rst_ = SR[2 + r3]["ms"]
                        for k in range(8):
                            t_, tk = SR[k % 2]["ms"], f"ms{k % 2}"
                            P.op("dve", STT(t_[:, 0:n], xT[:, k, c0:c0 + n], A2[:, k, i_mod:i_mod + 1], rst_[:, 0:n], ALU.mult, ALU.mult),
                                 reads=[f"xT{bi_}", f"ms{2 + r3}", "A2"], writes=[tk])
                            if not last:
                                if k % 2 == 0:
                                    P.op("dve", TS(h2T[:, k, c0:c0 + n], t_[:, 0:n], Bcol(3, k, i_mod), None, ALU.add),
                                         reads=[tk, "modc"], writes=[f"h2T{k}"])
                                else:
                                    P.op("act", ACT(h2T[:, k, c0:c0 + n], t_[:, 0:n], AF.Identity, bias=Bcol(3, k, i_mod), scale=1.0),
                                         reads=[tk, "modc"], writes=[f"h2T{k}"])
                            else:
                                P.op("act", ACT(hf[:, k, 0:n], t_[:, 0:n], AF.Identity, bias=Bcol(3, k, i_mod), scale=1.0),
                                     reads=[tk, "modc"], writes=[f"hf{k}"])
                                P.op("pool", CP(h2T[:, k, c0:c0 + n], hf[:, k, 0:n]), reads=[f"hf{k}"], writes=[f"h2T{k}"])
                        if last:
                            for tt in range(n // 128):
                                tg = (c0 // 128) + tt
                                bl = nb()
                                for k in range(8):
                                    P.op("pe", MM(bank(bl)[:, 0:8], hf[:, k, tt * 128:(tt + 1) * 128], rt[:, k, :], start=(k == 0), stop=(k == 7)),
                                         reads=[f"hf{k}", "rt"], writes=[f"B{bl}"])
                                P.op("dve", CP(Lg[:, tg, :], bank(bl)[:, 0:8]), reads=[f"B{bl}"], writes=["Lg"])
                    if last:
                        m1, m2, df, e2, g1, g2 = (sm[:, i, :] for i in range(6))
                        P.op("dve", RMAX(m1, Lg[:, :, :]), reads=["Lg"], writes=["m1"])
                        for e_ in range(8):
                            P.op("dve", TT(mk1[:, :, e_], Lg[:, :, e_], m1, ALU.is_equal), reads=["Lg", "m1"], writes=["mk1"])
                        P.op("dve", STT(L2[:, :, :], mk1[:, :, :], -1e30, Lg[:, :, :], ALU.mult, ALU.add), reads=["mk1", "Lg"], writes=["L2"])
                        P.op("dve", RMAX(m2, L2[:, :, :]), reads=["L2"], writes=["m2"])
                        for e_ in range(8):
                            P.op("dve", TT(mk2[:, :, e_], L2[:, :, e_], m2, ALU.is_equal), reads=["L2", "m2"], writes=["mk2"])
                        P.op("dve", TT(df, m2, m1, ALU.subtract), reads=["m1", "m2"], writes=["df"])
                        P.op("act", ACT(e2, df, AF.Exp), reads=["df"], writes=["e2"])
                        P.op("dve", TS(g1, e2, 1.0, None, ALU.add), reads=["e2"], writes=["g1"])
                        P.op("dve", RECIP(g1, g1), reads=["g1"], writes=["g1"])
                        P.op("dve", TT(g2, e2, g1, ALU.mult), reads=["e2", "g1"], writes=["g2"])
                        for e_ in range(8):
                            P.op("dve", TT(gts[:, :, e_], mk1[:, :, e_], g1, ALU.mult), reads=["mk1", "g1"], writes=["gts"])
                            P.op("dve", TT(mk2[:, :, e_], mk2[:, :, e_], g2, ALU.mult), reads=["mk2", "g2"], writes=["mk2"])
                        P.op("dve", TT(gts[:, :, :], gts[:, :, :], mk2[:, :, :], ALU.add), reads=["gts", "mk2"], writes=["gts"])
                        for q4 in range(4):
                            bg = nb()
                            for tt in range(4):
                                tg = q4 * 4 + tt
                                P.op("pe", TR(bank(bg)[0:8, tt * 128:(tt + 1) * 128], gts[:, tg, :], ident_f[:, :]),
                                     reads=["gts", "ident_f"], writes=[f"B{bg}"])
                            P.op("dve", CP(gT[0:8, q4 * 512:(q4 + 1) * 512], bank(bg)[0:8, 0:512]), reads=[f"B{bg}"], writes=["gT"])
                    P.end()

                GS = 4
                groups = []
                f0 = 0
                while f0 < NFC:
                    gs = min(GS, NFC - f0)
                    groups.append((f0, gs))
                    f0 += gs
                with ExitStack() as st:
                    wgt = [sb(st, f"wgt{i}", [128, 8, GS * 128], BF16) for i in range(2)]
                    wut = [sb(st, f"wut{i}", [128, 8, GS * 128], BF16) for i in range(2)]
                    wdt = [sb(st, f"wdt{i}", [128, GS, 1024], BF16) for i in range(2)]
                    sgt = [sb(st, f"sgt{i}", [128, 512], F32) for i in range(2)]
                    actt = [sb(st, f"actt{i}", [128, GS, 512], BF16) for i in range(2)]
                    if last:
                        Ge = [sb(st, f"Ge{i}", [128, T_LAT], BF16) for i in range(2)]
                        selE = sb(st, "selE", [128, 8, 128], F32)
                    nexp = 8 if last else 1
                    P.begin()
                    if last:
                        P.dma("sp", DMA(selE[0:8, :, :], selE_d[:, :, :]), writes=["selE"], key="selE")
                    gi = 0
                    ai = 0
                    rr[0] = 0
                    for e_ in range(nexp):
                        wg_v = WL["wg"][e_].rearrange("(k p) f -> p k f", p=128)
                        wu_v = WL["wu"][e_].rearrange("(k p) f -> p k f", p=128)
                        wd_v = WL["wd"][e_].rearrange("(f p) d -> p f d", p=128)
                        if last:
                            ge = Ge[e_ % 2]
                            for q4 in range(4):
                                bg = 6 + (q4 % 2)
                                P.op("pe", MM(bank(bg)[:, 0:512], selE[0:8, e_, :], gT[0:8, q4 * 512:(q4 + 1) * 512]),
                                     reads=["selE", "gT"], writes=[f"B{bg}"])
                                P.op("act", ACT(ge[:, q4 * 512:(q4 + 1) * 512], bank(bg)[:, 0:512], AF.Copy), reads=[f"B{bg}"], writes=[f"Ge{e_ % 2}"])
                        for (f0, gs) in groups:
                            b_ = gi % 2
                            gi += 1
                            P.dma("pool", DMAS([(wgt[b_][:, k, 0:gs * 128], wg_v[:, k, f0 * 128:(f0 + gs) * 128]) for k in range(8)]),
                                  writes=[f"wgt{b_}"], key=f"wg{b_}", n=8)
                            P.dma("pool", DMAS([(wut[b_][:, k, 0:gs * 128], wu_v[:, k, f0 * 128:(f0 + gs) * 128]) for k in range(8)]),
                                  writes=[f"wut{b_}"], key=f"wu{b_}", n=8)
                            P.dma("pool", DMAS([(wdt[b_][:, f, :], wd_v[:, f0 + f, :]) for f in range(gs)]),
                                  writes=[f"wdt{b_}"], key=f"wd{b_}", n=gs)
                            for (c0, n) in fblocks:
                                i_mod = 0 if c0 < T_LAT else 1
                                a_ = ai % 2
                                ai += 1
                                at = actt[a_]
                                for f in range(gs):
                                    pg = (f % 2) * 2
                                    pu = pg + 1
                                    for k in range(8):
                                        P.op("pe", MM(bank(pg)[:, 0:n], wgt[b_][:, k, f * 128:(f + 1) * 128], h2T[:, k, c0:c0 + n], start=(k == 0), stop=(k == 7)),
                                             reads=[f"wgt{b_}", "h2T"], writes=[f"B{pg}"])
                                    for k in range(8):
                                        P.op("pe", MM(bank(pu)[:, 0:n], wut[b_][:, k, f * 128:(f + 1) * 128], h2T[:, k, c0:c0 + n], start=(k == 0), stop=(k == 7)),
                                             reads=[f"wut{b_}", "h2T"], writes=[f"B{pu}"])
                                    sg = sgt[f % 2]
                                    P.op("act", ACT(sg[:, 0:n], bank(pg)[:, 0:n], AF.Silu), reads=[f"B{pg}"], writes=[f"sg{f % 2}"])
                                    if last:
                                        P.op("dve", TT(sg[:, 0:n], sg[:, 0:n], Ge[e_ % 2][:, c0:c0 + n], ALU.mult),
                                             reads=[f"sg{f % 2}", f"Ge{e_ % 2}"], writes=[f"sg{f % 2}"])
                                    P.op("dve", TT(at[:, f, 0:n], sg[:, 0:n], bank(pu)[:, 0:n], ALU.mult),
                                         reads=[f"sg{f % 2}", f"B{pu}"], writes=[f"act{a_}_{f}"])
                                for m in range(8):
                                    bd = 4 + (m % 4)
                                    for f in range(gs):
                                        P.op("pe", MM(bank(bd)[:, 0:n], wdt[b_][:, f, m * 128:(m + 1) * 128], at[:, f, 0:n], start=(f == 0), stop=(f == gs - 1)),
                                             reads=[f"wdt{b_}", f"act{a_}_{f}"], writes=[f"B{bd}"])
                                    P.op("dve", STT(xT[:, m, c0:c0 + n], bank(bd)[:, 0:n], Bcol(5, m, i_mod), xT[:, m, c0:c0 + n], ALU.mult, ALU.add),
                                         reads=[f"B{bd}", "modc", f"xT{m}_{c0}"], writes=[f"xT{m}_{c0}"])
                                    if (not last) and e_ == nexp - 1 and f0 + gs == NFC:
                                        P.dma("sp", DMA(xs_v[:, m, c0:c0 + n], xT[:, m, c0:c0 + n]), reads=[f"xT{m}_{c0}"], key="xo")
                    P.end()

                if not last:
                    pass
                else:
                    with ExitStack() as st:
                        ot = [sb(st, f"ot{i}", [128, 1024], F32) for i in range(2)]
                        P.begin()
                        for tt in range(16):
                            o_ = ot[tt % 2]
                            pz = PS2[tt % 2]
                            for k in range(8):
                                P.op("pe", TR(pz[:, k * 128:(k + 1) * 128], xT[:, k, tt * 128:(tt + 1) * 128], ident_f[:, :]),
                                     reads=["ident_f"], writes=[f"PZ{tt % 2}"])
                            P.op("act", ACT(o_[:, 0:512], pz[:, 0:512], AF.Copy), reads=[f"PZ{tt % 2}"], writes=[f"ot{tt % 2}a"])
                            P.op("dve", CP(o_[:, 512:1024], pz[:, 512:1024]), reads=[f"PZ{tt % 2}"], writes=[f"ot{tt % 2}b"])
                            P.dma("sp", DMA(out_d[tt * 128:(tt + 1) * 128, :], o_[:, :]), reads=[f"ot{tt % 2}a", f"ot{tt % 2}b"], key=f"out{tt % 2}")
                        P.end()
    return nc


_CACHE = {}


def _prep_inputs(inputs):
    f = lambda a: np.ascontiguousarray(np.asarray(a, dtype=np.float32))
    consts = make_consts()
    shared = dict(consts)
    for L in range(2):
        p = f"l{L}_"
        shared[p + "w_mod"] = f(inputs[p + "w_mod"])
        vecs = np.concatenate([f(inputs[p + "b_mod"]).reshape(48, 128), f(inputs[p + "norm_attn"]).reshape(8, 128),
                               f(inputs[p + "norm_ffn"]).reshape(8, 128), f(inputs[p + "q_lat_norm"]).reshape(2, 128),
                               f(inputs[p + "kv_lat_norm"]).reshape(1, 128)], axis=0)
        shared[p + "vecs"] = np.ascontiguousarray(vecs)
        for nm in ("w_in", "w_uq", "w_ukv", "w_out"):
            shared[p + nm] = f(inputs[p + nm])
        for nm in ("mla_q_gain", "mla_k_gain", "gqa_q_gain", "gqa_k_gain"):
            shared[p + nm] = f(inputs[p + nm]).reshape(-1, 1)
    for nm in ("l0_ffn_w_gate", "l0_ffn_w_up", "l0_ffn_w_down", "l1_router", "l1_exp_w_gate", "l1_exp_w_up", "l1_exp_w_down"):
        shared[nm] = f(inputs[nm])
    x = f(inputs["x"])
    ctx = f(inputs["ctx"])
    c = f(inputs["c"])
    cc = f(inputs["c_ctx"]).reshape(8, 128)
    in_maps = []
    for b in range(8):
        m = dict(shared)
        m["x"] = x[b]
        m["ctx"] = ctx[b]
        m["cvec"] = np.ascontiguousarray(np.concatenate([c[b].reshape(8, 128), cc], axis=0))
        in_maps.append(m)
    return in_maps


def kernel(**inputs):
    if "nc" not in _CACHE:
        _CACHE["nc"] = build_program()
    nc = _CACHE["nc"]
    in_maps = _prep_inputs(inputs)
    res = run_bass_kernel_spmd(nc, in_maps, core_ids=list(range(8)))
    out = np.stack([np.asarray(res.results[b]["out"], dtype=np.float32) for b in range(8)], axis=0)
    return out
```

```python
import numpy as np
from contextlib import ExitStack
import concourse.bass as bass
import concourse.mybir as mybir
from concourse.bass_utils import run_bass_kernel_spmd

F32 = mybir.dt.float32
BF16 = mybir.dt.bfloat16
I32 = mybir.dt.int32
AF = mybir.ActivationFunctionType
ALU = mybir.AluOpType
AX = mybir.AxisListType

ENGS = ("pe", "act", "dve", "pool", "sp")
ENGOBJ = {"pe": "tensor", "act": "scalar", "dve": "vector", "pool": "gpsimd", "sp": "sync"}

T_LAT, T_CTX, T_ALL = 2048, 256, 2304
EPS = 1e-6
THETA = 10000.0
FF = 2816
NFC = FF // 128


class _Op:
    __slots__ = ("eng", "fn", "deps", "is_dma", "dkey", "sig", "idx", "dcount", "ndma", "cond")

    def __init__(self, eng, fn, is_dma=False, dkey=None, ndma=1, cond=None):
        self.cond = cond
        self.eng = eng
        self.fn = fn
        self.deps = []
        self.is_dma = is_dma
        self.dkey = dkey
        self.sig = False
        self.idx = None
        self.dcount = None
        self.ndma = ndma


class Prog:
    NDMA = 14

    def __init__(self, nc):
        self.nc = nc
        self.sets = []
        for s in range(2):
            d = {e: nc.alloc_semaphore(name=f"s{s}_{e}") for e in ENGS}
            d["dma"] = [nc.alloc_semaphore(name=f"s{s}_d{i}") for i in range(self.NDMA)]
            self.sets.append(d)
        self.phase_no = 0
        self.count = 0
        self.dtot = {}
        self.limit = None
        self.ops = None
        self.dirty = [False, False]
        self.flags_ap = None
        self.nlvl = 6
        self.regs = {}
        self.loaded = {}
        with nc.Block() as block:
            for e in ENGS:
                def make(e):
                    def body(eng):
                        for st in self.sets:
                            eng.sem_clear(st[e])
                            if e == "pool":
                                for sm in st["dma"]:
                                    eng.sem_clear(sm)
                        if e == "pool":
                            r = eng.alloc_register("idma_bound")
                            eng.reg_mov(r, 8 * T_LAT - 1)
                            self.bc = eng.snap(r)
                    return body
                getattr(block, ENGOBJ[e])(make(e))

    def begin(self):
        self.ops = []
        self.lastw = {}
        self.readers = {}
        self.dkeys = {}

    def _add(self, op, reads, writes):
        deps = []
        for r in reads:
            w = self.lastw.get(r)
            if w is not None:
                deps.append(w)
        for wk in writes:
            w = self.lastw.get(wk)
            if w is not None:
                deps.append(w)
            deps.extend(self.readers.get(wk, ()))
        seen = set()
        for d in deps:
            if d is op or id(d) in seen:
                continue
            seen.add(id(d))
            if d.eng == "pe" and op.eng == "pe" and not d.is_dma and not op.is_dma:
                continue
            op.deps.append(d)
        for r in reads:
            self.readers.setdefault(r, []).append(op)
        for wk in writes:
            self.lastw[wk] = op
            self.readers[wk] = []
        self.ops.append(op)
        return op

    def op(self, eng, fn, reads=(), writes=(), cond=None):
        return self._add(_Op(eng, fn, cond=cond), list(reads), list(writes))

    def dma(self, queue, fn, reads=(), writes=(), key=None, n=1):
        if key not in self.dkeys:
            self.dkeys[key] = len(self.dkeys)
            assert len(self.dkeys) <= self.NDMA, "too many dma keys in phase"
        return self._add(_Op(queue, fn, True, key, n), list(reads), list(writes))

    def end(self):
        nc = self.nc
        ops = self.ops
        self.count += 1
        if self.limit is not None and self.count > self.limit:
            self.ops = None
            return
        if self.limit is not None and self.count == self.limit and getattr(self, "oplimit", None) is not None:
            ops = ops[:self.oplimit]
            print("phase ops total", len(self.ops), "emitting", len(ops))
        cur = self.phase_no % 2
        sems = self.sets[cur]
        other = self.sets[1 - cur]
        for o in ops:
            for d in o.deps:
                d.sig = True
        cnt = {e: 0 for e in ENGS}
        dcnt = {k: self.dtot.get(i, 0) for k, i in self.dkeys.items()}
        for o in ops:
            if o.is_dma:
                dcnt[o.dkey] = dcnt.get(o.dkey, 0) + 16 * o.ndma
                o.dcount = dcnt[o.dkey]
            elif o.sig:
                cnt[o.eng] += 1
                o.idx = cnt[o.eng]
        per = {e: [] for e in ENGS}
        for o in ops:
            per[o.eng].append(o)
        clear_other = self.dirty[1 - cur]
        dkeys = self.dkeys

        def dep_kv(d):
            if d.is_dma:
                return ("dma", dkeys[d.dkey]), d.dcount
            return d.eng, d.idx

        def do_waits(eng, need, waited):
            for k, v in need.items():
                s = self.sets[0]["dma"][k[1]] if isinstance(k, tuple) else sems[k]
                eng.wait_ge(s, v)
                waited[k] = v

        def emit_op(e, eng, o, waited):
            need = {}
            for d in o.deps:
                k, v = dep_kv(d)
                if waited.get(k, 0) >= v:
                    continue
                if need.get(k, 0) < v:
                    need[k] = v
            do_waits(eng, need, waited)
            ins = o.fn(eng)
            if o.is_dma:
                lst = ins if isinstance(ins, (list, tuple)) else [ins]
                assert len(lst) == o.ndma, (len(lst), o.ndma)
                for i_ in lst:
                    i_.then_inc(self.sets[0]["dma"][dkeys[o.dkey]], 16)
            elif o.sig:
                ins.then_inc(sems[e], 1)

        NLVL = self.nlvl

        def skip_regions(e, eng, regions, conds, snap):
            emitted = False
            for region in regions:
                need = {}
                nsig = 0
                for r_ in region:
                    if r_.sig:
                        nsig += 1
                    for d in r_.deps:
                        if d.cond in conds:
                            continue
                        k, v = dep_kv(d)
                        if snap.get(k, 0) >= v:
                            continue
                        if need.get(k, 0) < v:
                            need[k] = v
                do_waits(eng, need, snap)
                if nsig:
                    eng.sem_inc(sems[e], nsig)
                    emitted = True
            if not emitted:
                eng.nop()

        def emit_chain(e, eng, regions, waited):
            region = regions[0]
            lvl = region[0].cond[2]
            reg = self.regs[(e, lvl)]
            snap = dict(waited)
            conds = set(r[0].cond for r in regions)
            with eng.If_ne(reg, 0):
                w2 = dict(snap)
                for r_ in region:
                    emit_op(e, eng, r_, w2)
                if len(regions) > 1:
                    emit_chain(e, eng, regions[1:], w2)
            with eng.Else():
                skip_regions(e, eng, regions, conds, snap)
            return snap

        def emit(e, eng):
            waited = {}
            if clear_other:
                eng.sem_clear(other[e])
            lst = per[e]
            i = 0
            while i < len(lst):
                o = lst[i]
                if o.cond is None:
                    emit_op(e, eng, o, waited)
                    i += 1
                    continue
                eg = o.cond[:2]
                j = i
                while j < len(lst) and lst[j].cond is not None and lst[j].cond[:2] == eg:
                    j += 1
                chain = lst[i:j]
                i = j
                assert all(not r_.is_dma for r_ in chain)
                regions = []
                for r_ in chain:
                    if regions and regions[-1][0].cond == r_.cond:
                        regions[-1].append(r_)
                    else:
                        regions.append([r_])
                lv = [r[0].cond[2] for r in regions]
                assert lv == sorted(set(lv)), lv
                eidx = eg[0]
                if self.loaded.get(e) != eidx:
                    for lvl in range(1, NLVL + 1):
                        if (e, lvl) not in self.regs:
                            self.regs[(e, lvl)] = eng.alloc_register(f"fl_{e}_{lvl}")
                        c_ = eidx * NLVL + lvl - 1
                        eng.reg_load(self.regs[(e, lvl)], self.flags_ap[0:1, c_:c_ + 1])
                    self.loaded[e] = eidx
                waited = emit_chain(e, eng, regions, waited)
            last = {}
            for o in per[e]:
                if o.is_dma:
                    last[o.dkey] = o.dcount
            for k, v in last.items():
                if waited.get(("dma", dkeys[k]), 0) < v:
                    eng.wait_ge(self.sets[0]["dma"][dkeys[k]], v)

        with nc.Block() as block:
            for e in ENGS:
                def make(e):
                    def body(eng):
                        emit(e, eng)
                    return body
                getattr(block, ENGOBJ[e])(make(e))
        for k, i in self.dkeys.items():
            self.dtot[i] = dcnt[k]
        self.dirty[cur] = True
        self.phase_no += 1
        self.ops = None


def MM(out, lhsT, rhs, start=True, stop=True):
    return lambda e: e.matmul(out, lhsT=lhsT, rhs=rhs, start=start, stop=stop)


def TR(out, in_, ident):
    return lambda e: e.transpose(out, in_, ident)


def ACT(out, in_, func, bias=None, scale=None):
    kw = {}
    if bias is not None:
        kw["bias"] = bias
    if scale is not None:
        kw["scale"] = scale
    return lambda e: e.activation(out=out, in_=in_, func=func, **kw)


def TT(out, in0, in1, op):
    return lambda e: e.tensor_tensor(out=out, in0=in0, in1=in1, op=op)


def STT(out, in0, scalar, in1, op0, op1):
    return lambda e: e.scalar_tensor_tensor(out=out, in0=in0, scalar=scalar, in1=in1, op0=op0, op1=op1)


def TS(out, in0, s1, s2, op0, op1=None):
    if op1 is None:
        return lambda e: e.tensor_scalar(out=out, in0=in0, scalar1=s1, scalar2=None, op0=op0)
    return lambda e: e.tensor_scalar(out=out, in0=in0, scalar1=s1, scalar2=s2, op0=op0, op1=op1)


def CP(out, in_):
    return lambda e: e.tensor_copy(out=out, in_=in_)


def RECIP(out, in_):
    return lambda e: e.reciprocal(out=out, in_=in_)


def MEMSET(ap, v):
    return lambda e: e.memset(ap, v)


def RMAX(out, in_):
    return lambda e: e.reduce_max(out=out, in_=in_, axis=AX.X)


def DMA(out, in_):
    return lambda e: e.dma_start(out=out, in_=in_)


def RSUM(out, in_):
    return lambda e: e.reduce_sum(out=out, in_=in_, axis=AX.X)


def ISCAT(dram, idx_ap, src, bound):
    return lambda e: e.indirect_dma_start(out=dram, out_offset=bass.IndirectOffsetOnAxis(ap=idx_ap, axis=0), in_=src,
                                          in_offset=None, bounds_check=bound, oob_is_err=False)


def IGATH(dst, dram, idx_ap, bound):
    return lambda e: e.indirect_dma_start(out=dst, out_offset=None, in_=dram,
                                          in_offset=bass.IndirectOffsetOnAxis(ap=idx_ap, axis=0),
                                          bounds_check=bound, oob_is_err=False)


def DMAS(pairs):
    def f(e):
        return [e.dma_start(out=o, in_=i) for (o, i) in pairs]
    return f


def make_consts():
    c = {}
    c["ident_f"] = np.eye(128, dtype=np.float32)
    cb = np.zeros((128, 6, 128), np.float32)
    cb[:, 0, :] = np.eye(128)
    cb[:, 1, :] = 1.0
    cb[0:64, 2, 0:64] = 1.0
    cb[64:128, 2, 64:128] = 1.0
    for base in (0, 16):
        for i in range(8):
            a = 64 + base + i
            b = a + 8
            cb[b, 3, a] = -1.0
            cb[a, 3, b] = 1.0
    for hb in (0, 64):
        for base in (0, 32):
            for i in range(16):
                a = hb + base + i
                b = a + 16
                cb[b, 4, a] = -1.0
                cb[a, 4, b] = 1.0
    cb[:, 5, :] = np.triu(np.ones((128, 128), np.float32), 1)
    c["cb"] = cb
    c["ebase"] = np.ascontiguousarray(np.broadcast_to(np.tile(np.arange(8, dtype=np.float32) * 2048.0, 16)[None, :], (128, 128)))
    sel96 = np.zeros((32, 96), np.float32)
    for i in range(32):
        sel96[i, 64 + i] = 1.0
    c["sel96"] = sel96
    t = np.arange(T_LAT)
    row = (t // 64).astype(np.float64)
    col = (t % 64).astype(np.float64)
    tabs = np.zeros((128, 4, T_LAT), np.float32)
    tabs[:, 0, :] = 1.0
    tabs[:, 2, :] = 1.0
    fa = THETA ** (-np.arange(8, dtype=np.float64) / 8.0)
    fb = THETA ** (-np.arange(16, dtype=np.float64) / 16.0)
    fa32 = fa.astype(np.float32).astype(np.float64)
    fb32 = fb.astype(np.float32).astype(np.float64)
    for r in range(32):
        pos = row if r < 16 else col
        ang = (pos * fa32[r % 8]).astype(np.float32).astype(np.float64)
        tabs[64 + r, 0, :] = np.cos(ang)
        tabs[64 + r, 1, :] = np.sin(ang)
    for hb in (0, 64):
        for d in range(64):
            pos = row if d < 32 else col
            ang = (pos * fb32[d % 16]).astype(np.float32).astype(np.float64)
            tabs[hb + d, 2, :] = np.cos(ang)
            tabs[hb + d, 3, :] = np.sin(ang)
    c["tabs"] = tabs
    return c


LAYER_W = ["w_mod", "vecs", "w_in", "w_uq", "w_ukv", "mla_q_gain", "mla_k_gain", "gqa_q_gain",
           "gqa_k_gain", "w_out"]


def build_program(dbg=None, limit=None, dbg_fn=None, oplimit=None):
    nc = bass.Bass("TRN2", target_bir_lowering=False)

    def din(name, shape):
        return nc.dram_tensor(name, list(shape), F32, kind="ExternalInput").ap()

    x_d = din("x", [T_LAT, 1024])
    ctx_d = din("ctx", [T_CTX, 1024])
    cvec_d = din("cvec", [16, 128])
    identf_d = din("ident_f", [128, 128])
    cb_d = din("cb", [128, 6, 128])
    ebase_d = din("ebase", [128, 128])
    sel96_d = din("sel96", [32, 96])
    tabs_d = din("tabs", [128, 4, T_LAT])
    W = []
    for L in range(2):
        d = {}
        d["w_mod"] = din(f"l{L}_w_mod", [1024, 6144])
        d["vecs"] = din(f"l{L}_vecs", [67, 128])
        d["w_in"] = din(f"l{L}_w_in", [1024, 1184])
        d["w_uq"] = din(f"l{L}_w_uq", [256, 768])
        d["w_ukv"] = din(f"l{L}_w_ukv", [128, 1024])
        d["mla_q_gain"] = din(f"l{L}_mla_q_gain", [96, 1])
        d["mla_k_gain"] = din(f"l{L}_mla_k_gain", [96, 1])
        d["gqa_q_gain"] = din(f"l{L}_gqa_q_gain", [64, 1])
        d["gqa_k_gain"] = din(f"l{L}_gqa_k_gain", [64, 1])
        d["w_out"] = din(f"l{L}_w_out", [1024, 1024])
        if L == 0:
            d["wg"] = [din("l0_ffn_w_gate", [1024, FF])]
            d["wu"] = [din("l0_ffn_w_up", [1024, FF])]
            d["wd"] = [din("l0_ffn_w_down", [FF, 1024])]
        else:
            d["router"] = din("l1_router", [1024, 8])
            ne_ = 8 if (limit is None or limit > 12) else 1
            wg = din("l1_exp_w_gate", [ne_, 1024, FF])
            wu = din("l1_exp_w_up", [ne_, 1024, FF])
            wd = din("l1_exp_w_down", [ne_, FF, 1024])
            wg = [wg[min(e, ne_ - 1)] for e in range(8)]
            wu = [wu[min(e, ne_ - 1)] for e in range(8)]
            wd = [wd[min(e, ne_ - 1)] for e in range(8)]
            d["wg"] = wg
            d["wu"] = wu
            d["wd"] = wd
        W.append(d)
    out_d = nc.dram_tensor("out", [T_LAT, 1024], F32, kind="ExternalOutput").ap()
    xs = nc.dram_tensor("xs", [8, 128, T_ALL], F32, kind="Internal").ap()
    xs_v = xs.rearrange("k p t -> p k t")
    NSLOT = 8 * T_LAT
    Gd = nc.dram_tensor("Gd", [NSLOT, 1024], BF16, kind="Internal").ap()
    Yd = nc.dram_tensor("Yd", [NSLOT, 1024], F32, kind="Internal").ap()
    dbg_d = None
    if dbg is not None:
        dbg_d = nc.dram_tensor("dbg", [128, dbg], F32, kind="ExternalOutput").ap()

    P = Prog(nc)
    P.limit = limit
    P.oplimit = oplimit
    BLOCKS = [(0, 512), (512, 512), (1024, 512), (1536, 512), (2048, 256)]

    with ExitStack() as top:
        uid = [0]

        def sb(st, name, shape, dt):
            uid[0] += 1
            return st.enter_context(nc.sbuf_tensor(f"sb{uid[0]}_{name}", list(shape), dt))

        PS2 = [top.enter_context(nc.psum_tensor(f"ps{i}", [128, 1024], F32)) for i in range(4)]

        def bank(i):
            return PS2[i // 2][:, (i % 2) * 512:(i % 2) * 512 + 512]

        rr = [0]

        def nb():
            i = rr[0] % 8
            rr[0] += 1
            return i

        ident_f = sb(top, "ident_f", [128, 128], F32)
        cbt = sb(top, "cbt", [128, 6, 128], BF16)
        sel96 = sb(top, "sel96", [128, 96], BF16)
        epsc = sb(top, "epsc", [128, 1], F32)
        cols = sb(top, "cols", [128, 83], F32)
        modc = sb(top, "modc", [128, 48, 2], F32)
        A1 = sb(top, "A1", [128, 8, 2], F32)
        A2 = sb(top, "A2", [128, 8, 2], F32)
        gq = sb(top, "gq", [128, 4], F32)
        ident_b = cbt[:, 0, :]
        ones_b = cbt[:, 1, :]
        bonesB = cbt[:, 2, :]
        PA = cbt[:, 3, :]
        PB = cbt[:, 4, :]
        triU = cbt[:, 5, :]

        P.begin()
        P.dma("sp", DMA(ident_f[:], identf_d[:, :]), writes=["ident_f"], key="c0")
        P.dma("pool", DMA(cbt[:], cb_d[:, :, :]), writes=["cbt"], key="c1")
        P.dma("pool", DMA(sel96[0:32, :], sel96_d[:, :]), writes=["sel96"], key="c2")
        P.op("dve", MEMSET(epsc[:], EPS), writes=["epsc"])
        P.end()

        with ExitStack() as st:
            xin = [sb(st, f"xin{i}", [128, 1024], F32) for i in range(2)]
            xblk = sb(st, "xblk", [128, 8, 512], F32)
            P.begin()
            ti = 0
            for (c0, n) in BLOCKS:
                nt = n // 128
                for tt in range(nt):
                    tok0 = c0 + tt * 128
                    src = x_d[tok0:tok0 + 128, :] if tok0 < T_LAT else ctx_d[tok0 - T_LAT:tok0 - T_LAT + 128, :]
                    b_ = ti % 2
                    P.dma("sp", DMA(xin[b_][:], src), writes=[f"xin{b_}"], key=f"xin{b_}")
                    for k in range(8):
                        P.op("pe", TR(bank(k)[:, tt * 128:(tt + 1) * 128], xin[b_][:, k * 128:(k + 1) * 128], ident_f[:]),
                             reads=[f"xin{b_}", "ident_f"], writes=[f"B{k}"])
                    ti += 1
                for k in range(8):
                    eng = "act" if k % 2 == 0 else "dve"
                    fn = ACT(xblk[:, k, 0:n], bank(k)[:, 0:n], AF.Copy) if eng == "act" else CP(xblk[:, k, 0:n], bank(k)[:, 0:n])
                    P.op(eng, fn, reads=[f"B{k}"], writes=[f"xblk{k}"])
                P.dma("sp", DMA(xs_v[:, :, c0:c0 + n], xblk[:, :, 0:n]), reads=[f"xblk{k}" for k in range(8)], key="xo")
            P.end()

        for L in range(2):
            WL = W[L]
            last = (L == 1)
            with ExitStack() as st:
                stage = sb(st, "stage", [128, 128], F32)
                scb = sb(st, "scb", [128, 8, 2], BF16)
                wm = [sb(st, f"wm{i}", [128, 8, 1024], BF16) for i in range(2)]
                wmod_v = WL["w_mod"].rearrange("(k p) n -> p k n", p=128)
                P.begin()
                P.op("dve", MEMSET(stage[:], 0.0), writes=["stage"])
                P.dma("sp", DMA(stage[0:67, :], WL["vecs"][:, :]), reads=["stage"], writes=["stage"], key="st")
                P.dma("sp", DMA(stage[67:83, :], cvec_d[:, :]), writes=["stage"], key="st")
                P.dma("sp", DMAS([(gq[0:96, 0:1], WL["mla_q_gain"][:, :]), (gq[0:96, 1:2], WL["mla_k_gain"][:, :]),
                                  (gq[0:64, 2:3], WL["gqa_q_gain"][:, :]), (gq[64:128, 2:3], WL["gqa_q_gain"][:, :]),
                                  (gq[0:64, 3:4], WL["gqa_k_gain"][:, :]), (gq[64:128, 3:4], WL["gqa_k_gain"][:, :])]),
                      writes=["gq"], key="gq", n=6)
                b0 = nb()
                P.op("pe", TR(bank(b0)[:, 0:128], stage[:, :], ident_f[:, :]), reads=["stage", "ident_f"], writes=[f"B{b0}"])
                P.op("dve", CP(cols[:, :], bank(b0)[:, 0:83]), reads=[f"B{b0}"], writes=["cols"])
                P.op("act", ACT(scb[:, :, 0], cols[:, 67:75], AF.Silu), reads=["cols"], writes=["scb"])
                P.op("act", ACT(scb[:, :, 1], cols[:, 75:83], AF.Silu), reads=["scb", "cols"], writes=["scb"])
                bm = nb()
                psm = bank(bm)[:, 0:96].rearrange("p (m i) -> p m i", i=2)
                for sec in range(6):
                    b_ = sec % 2
                    P.dma("pool", DMAS([(wm[b_][:, k, :], wmod_v[:, k, sec * 1024:(sec + 1) * 1024]) for k in range(8)]),
                          writes=[f"wm{b_}"], key=f"wm{b_}", n=8)
                    for m in range(8):
                        for k in range(8):
                            P.op("pe", MM(psm[:, sec * 8 + m, :], wm[b_][:, k, m * 128:(m + 1) * 128], scb[:, k, :],
                                          start=(k == 0), stop=(k == 7)),
                                 reads=[f"wm{b_}", "scb"], writes=[f"B{bm}"])
                for i in range(2):
                    P.op("dve", TT(modc[:, :, i], psm[:, :, i], cols[:, 0:48], ALU.add), reads=[f"B{bm}", "cols"], writes=["modc"])
                for i in range(2):
                    P.op("dve", STT(A1[:, :, i], modc[:, 8:16, i], 1.0, cols[:, 48:56], ALU.add, ALU.mult),
                         reads=["modc", "cols"], writes=["A1"])
                    P.op("dve", STT(A2[:, :, i], modc[:, 32:40, i], 1.0, cols[:, 56:64], ALU.add, ALU.mult),
                         reads=["modc", "cols"], writes=["A2"])
                P.end()

            def Bcol(sec, k, i):
                return modc[:, sec * 8 + k, i:i + 1]

            def mk_sets(st, ns):
                sets = []
                for i_ in range(ns):
                    sets.append(dict(i=i_, sq=sb(st, f"sq{i_}", [128, 512], BF16), ms=sb(st, f"ms{i_}", [128, 512], F32),
                                     kn=sb(st, f"kn{i_}", [128, 512], BF16), t1=sb(st, f"t1{i_}", [128, 512], BF16),
                                     t2=sb(st, f"t2{i_}", [128, 512], BF16)))
                return sets

            job = [0]

            def next_set(SR):
                S = SR[job[0] % len(SR)]
                job[0] += 1
                return S

            def rstd_from(srcs, rows, n, inv_sqrt_d, ones_ap, sqs, ms):
                bi = nb()
                mst, msk = ms
                for i, (ap, rk) in enumerate(srcs):
                    s_, sk = sqs[i % len(sqs)]
                    P.op("act", ACT(s_[0:rows, 0:n], ap, AF.Square, scale=inv_sqrt_d), reads=rk, writes=[sk])
                    P.op("pe", MM(bank(bi)[0:rows, 0:n], ones_ap, s_[0:rows, 0:n], start=(i == 0), stop=(i == len(srcs) - 1)),
                         reads=[sk, "cbt"], writes=[f"B{bi}"])
                P.op("act", ACT(mst[0:rows, 0:n], bank(bi)[0:rows, 0:n], AF.Ln, bias=epsc[0:rows, 0:1], scale=1.0),
                     reads=[f"B{bi}", "epsc"], writes=[msk])
                P.op("act", ACT(mst[0:rows, 0:n], mst[0:rows, 0:n], AF.Exp, scale=-0.5), reads=[msk], writes=[msk])

            def norm_mod(xj, n, Acol, sec, i, hj, SR, xk="xj", hk="hj"):
                rstd_from([(xj[:, k, 0:n], [xk]) for k in range(8)], 128, n, 1.0 / 32.0, ones_b,
                          [(SR[0]["sq"], "sq0"), (SR[1]["sq"], "sq1")], (SR[2]["ms"], "ms2"))
                for k in range(8):
                    t_, tk = SR[k % 2]["ms"], f"ms{k % 2}"
                    P.op("dve", STT(t_[:, 0:n], xj[:, k, 0:n], Acol[:, k, i:i + 1], SR[2]["ms"][:, 0:n], ALU.mult, ALU.mult),
                         reads=[xk, "ms2", "A1", "A2"], writes=[tk])
                    if k % 2 == 0:
                        P.op("dve", TS(hj[:, k, 0:n], t_[:, 0:n], Bcol(sec, k, i), None, ALU.add),
                             reads=[tk, "modc"], writes=[f"{hk}{k}"])
                    else:
                        P.op("act", ACT(hj[:, k, 0:n], t_[:, 0:n], AF.Identity, bias=Bcol(sec, k, i), scale=1.0),
                             reads=[tk, "modc"], writes=[f"{hk}{k}"])

            with ExitStack() as att:
                tabs = sb(att, "tabs", [128, 4, T_LAT], BF16)
                KaT = sb(att, "KaT", [128, 8, T_ALL], BF16)
                KbT = sb(att, "KbT", [128, 2, T_ALL], BF16)
                Vst = sb(att, "Vst", [128, 18, 1152], BF16)
                cosA, sinA, cosB, sinB = tabs[:, 0, :], tabs[:, 1, :], tabs[:, 2, :], tabs[:, 3, :]

                def head_norm_rope(pre_bank, rows, n, inv_sqrt_d, ones_ap, gcol, perm, cos_t, sin_t, c0, dst, rope,
                                   S, dstkey):
                    si = S["i"]
                    rstdt, knt, t1t, t2t = S["ms"], S["kn"], S["t1"], S["t2"]
                    pre = bank(pre_bank)[0:rows, 0:n]
                    rstd_from([(pre, [f"B{pre_bank}"])], rows, n, inv_sqrt_d, ones_ap, [(S["sq"], f"sq{si}")], (rstdt, f"ms{si}"))
                    if not rope:
                        P.op("dve", STT(dst, pre, gcol, rstdt[0:rows, 0:n], ALU.mult, ALU.mult),
                             reads=[f"B{pre_bank}", f"ms{si}", "gq"], writes=[dstkey])
                        return
                    P.op("dve", STT(knt[0:rows, 0:n], pre, gcol, rstdt[0:rows, 0:n], ALU.mult, ALU.mult),
                         reads=[f"B{pre_bank}", f"ms{si}", "gq"], writes=[f"kn{si}"])
                    br = pre_bank
                    P.op("pe", MM(bank(br)[0:rows, 0:n], perm[0:rows, 0:rows], knt[0:rows, 0:n]),
                         reads=[f"kn{si}", "cbt"], writes=[f"B{br}"])
                    P.op("pool", TT(t1t[0:rows, 0:n], knt[0:rows, 0:n], cos_t[0:rows, c0:c0 + n], ALU.mult),
                         reads=[f"kn{si}", "tabs"], writes=[f"t1{si}"])
                    P.op("dve", TT(t2t[0:rows, 0:n], bank(br)[0:rows, 0:n], sin_t[0:rows, c0:c0 + n], ALU.mult),
                         reads=[f"B{br}", "tabs"], writes=[f"t2{si}"])
                    P.op("dve", TT(dst, t1t[0:rows, 0:n], t2t[0:rows, 0:n], ALU.add),
                         reads=[f"t1{si}", f"t2{si}"], writes=[dstkey])

                def head_job(SRl, pre_mm, pre_reads, rows, n, inv_sqrt_d, ones_ap, gcol, perm, cos_t, sin_t, c0, dst, rope, dstkey):
                    stt = {}

                    def A():
                        S = next_set(SRl)
                        bp = nb()
                        stt["S"], stt["bp"] = S, bp
                        pre_mm(bp)
                        P.op("act", ACT(S["sq"][0:rows, 0:n], bank(bp)[0:rows, 0:n], AF.Square, scale=inv_sqrt_d),
                             reads=[f"B{bp}"], writes=[f"sq{S['i']}"])

                    def B():
                        S, bp = stt["S"], stt["bp"]
                        si = S["i"]
                        pre = bank(bp)[0:rows, 0:n]
                        ms = S["ms"][0:rows, 0:n]
                        bi = nb()
                        P.op("pe", MM(bank(bi)[0:rows, 0:n], ones_ap, S["sq"][0:rows, 0:n]), reads=[f"sq{si}", "cbt"], writes=[f"B{bi}"])
                        P.op("act", ACT(ms, bank(bi)[0:rows, 0:n], AF.Ln, bias=epsc[0:rows, 0:1], scale=1.0),
                             reads=[f"B{bi}", "epsc"], writes=[f"ms{si}"])
                        P.op("act", ACT(ms, ms, AF.Exp, scale=-0.5), reads=[f"ms{si}"], writes=[f"ms{si}"])
                        if not rope:
                            P.op("dve", STT(dst, pre, gcol, ms, ALU.mult, ALU.mult), reads=[f"B{bp}", f"ms{si}", "gq"], writes=[dstkey])
                        else:
                            P.op("dve", STT(S["kn"][0:rows, 0:n], pre, gcol, ms, ALU.mult, ALU.mult),
                                 reads=[f"B{bp}", f"ms{si}", "gq"], writes=[f"kn{si}"])

                    def C():
                        if not rope:
                            return
                        S, bp = stt["S"], stt["bp"]
                        si = S["i"]
                        knt, t1t, t2t = S["kn"], S["t1"], S["t2"]
                        P.op("pe", MM(bank(bp)[0:rows, 0:n], perm[0:rows, 0:rows], knt[0:rows, 0:n]),
                             reads=[f"kn{si}", "cbt"], writes=[f"B{bp}"])
                        P.op("pool", TT(t1t[0:rows, 0:n], knt[0:rows, 0:n], cos_t[0:rows, c0:c0 + n], ALU.mult),
                             reads=[f"kn{si}", "tabs"], writes=[f"t1{si}"])
                        P.op("dve", TT(t2t[0:rows, 0:n], bank(bp)[0:rows, 0:n], sin_t[0:rows, c0:c0 + n], ALU.mult),
                             reads=[f"B{bp}", "tabs"], writes=[f"t2{si}"])
                        P.op("dve", TT(dst, t1t[0:rows, 0:n], t2t[0:rows, 0:n], ALU.add),
                             reads=[f"t1{si}", f"t2{si}"], writes=[dstkey])
                    return [A, B, C]

                def run_pipeline(jobs):
                    ns = 3
                    for t in range(len(jobs) + ns - 1):
                        for s in range(ns):
                            j = t - s
                            if 0 <= j < len(jobs):
                                jobs[j][s]()

                with ExitStack() as st:
                    w_in = sb(st, "w_in", [128, 8, 160], BF16)
                    WKB = sb(st, "WKB", [128, 8, 2, 128], BF16)
                    WVB = sb(st, "WVB", [128, 8, 128], BF16)
                    WN = sb(st, "WN", [128, 8, 96], BF16)
                    WV = sb(st, "WV", [128, 8, 64], BF16)
                    xjs = [sb(st, f"xj{i}", [128, 8, 512], F32) for i in range(2)]
                    hjs = [sb(st, f"hj{i}", [128, 8, 512], BF16) for i in range(2)]
                    ckvn2 = [sb(st, f"ckvn{i}", [128, 512], BF16) for i in range(2)]
                    krope2 = [sb(st, f"krope{i}", [128, 512], BF16) for i in range(2)]
                    SR = mk_sets(st, 4)
                    win_v = WL["w_in"].rearrange("(k p) n -> p k n", p=128)
                    wukv_v = WL["w_ukv"].rearrange("p (h c) -> p h c", c=128)
                    P.begin()
                    P.dma("pool", DMAS([(w_in[:, k, 0:160], win_v[:, k, 256:416]) for k in range(8)]), writes=["w_in"], key="w_in", n=8)
                    P.op("dve", MEMSET(WN[:], 0.0), writes=["WN"])
                    P.dma("pool", DMA(WN[:, :, 0:64], wukv_v[:, :, 0:64]), reads=["WN"], writes=["WN"], key="WN")
                    P.dma("pool", DMA(WV[:, :, :], wukv_v[:, :, 64:128]), writes=["WV"], key="WV")
                    P.dma("pool", DMA(tabs[:], tabs_d[:, :, :]), writes=["tabs"], key="tabs")
                    P.dma("pool", DMAS([(WKB[:, k, g, h_ * 64:(h_ + 1) * 64], win_v[:, k, 928 + g * 64:928 + (g + 1) * 64])
                                        for k in range(8) for g in range(2) for h_ in range(2)]), writes=["WKB"], key="WKB", n=32)
                    P.dma("pool", DMAS([(WVB[:, k, :], win_v[:, k, 1056:1184]) for k in range(8)]), writes=["WVB"], key="WVB", n=8)
                    P.op("dve", MEMSET(Vst[:, :, :].rearrange("p k (a s c) -> p k a s c", s=3, c=64)[:, :, :, 1, :], 1.0), writes=["Vst"])
                    if L == 0:
                        zt = sb(st, "zt", [128, 4, 1024], BF16)
                        P.op("pool", MEMSET(zt[:], 0.0), writes=["zt"])
                    def kv_block(bj_, c0, n):
                        i_mod = 0 if c0 < T_LAT else 1
                        rope = c0 < T_LAT
                        p2 = bj_ % 2
                        xj = xjs[p2]
                        hj = hjs[p2]
                        ckvn = ckvn2[p2]
                        krope = krope2[p2]
                        xk_ = f"xj{p2}"
                        hk_ = f"hj{p2}_"
                        ck_ = f"ckvn{p2}"
                        kr_ = f"krope{p2}"

                        def preA():
                            P.dma("sp", DMA(xj[:, :, 0:n], xs_v[:, :, c0:c0 + n]), writes=[xk_], key=xk_)
                            if L == 0 and bj_ >= 1:
                                for zi in range((bj_ - 1) * 8, bj_ * 8):
                                    P.dma("sp", DMA(Gd[zi * 512:(zi + 1) * 512, :].rearrange("(s p) d -> p s d", p=128), zt[:, :, :]),
                                          reads=["zt"], key="zf")
                            norm_mod(xj, n, A1, 0, i_mod, hj, SR, xk=xk_, hk=hk_)

                        def preB():
                            bc = nb()
                            for k in range(8):
                                P.op("pe", MM(bank(bc)[:, 0:n], w_in[:, k, 0:128], hj[:, k, 0:n], start=(k == 0), stop=(k == 7)),
                                     reads=["w_in", f"{hk_}{k}"], writes=[f"B{bc}"])
                            S_ = next_set(SR)
                            rstd_from([(bank(bc)[:, 0:n], [f"B{bc}"])], 128, n, 128 ** -0.5, ones_b, [(S_["sq"], f"sq{S_['i']}")], (S_["ms"], f"ms{S_['i']}"))
                            P.op("dve", STT(ckvn[:, 0:n], bank(bc)[:, 0:n], cols[:, 66:67], S_["ms"][:, 0:n], ALU.mult, ALU.mult),
                                 reads=[f"B{bc}", f"ms{S_['i']}", "cols"], writes=[ck_])
                            bk = nb()
                            for k in range(8):
                                P.op("pe", MM(bank(bk)[0:32, 0:n], w_in[:, k, 128:160], hj[:, k, 0:n], start=(k == 0), stop=(k == 7)),
                                     reads=["w_in", f"{hk_}{k}"], writes=[f"B{bk}"])
                            P.op("act", ACT(krope[0:32, 0:n], bank(bk)[0:32, 0:n], AF.Copy), reads=[f"B{bk}"], writes=[kr_])

                        def main():
                            jobs = []
                            for h in range(8):
                                def pre_mla(bp, h=h):
                                    P.op("pe", MM(bank(bp)[0:96, 0:n], WN[:, h, :], ckvn[:, 0:n], start=True, stop=False),
                                         reads=["WN", ck_], writes=[f"B{bp}"])
                                    P.op("pe", MM(bank(bp)[0:96, 0:n], sel96[0:32, :], krope[0:32, 0:n], start=False, stop=True),
                                         reads=["sel96", kr_], writes=[f"B{bp}"])
                                jobs.append(head_job(SR, pre_mla, None, 96, n, 96 ** -0.5, ones_b[0:96, 0:96], gq[0:96, 1:2], PA, cosA, sinA, c0,
                                                     KaT[0:96, h, c0:c0 + n], rope, "KaT"))
                            for g in range(2):
                                def pre_gqa(bp, g=g):
                                    for k in range(8):
                                        P.op("pe", MM(bank(bp)[:, 0:n], WKB[:, k, g, :], hj[:, k, 0:n], start=(k == 0), stop=(k == 7)),
                                             reads=["WKB", f"{hk_}{k}"], writes=[f"B{bp}"])
                                jobs.append(head_job(SR, pre_gqa, None, 128, n, 0.125, bonesB, gq[:, 3:4], PB, cosB, sinB, c0,
                                                     KbT[:, g, c0:c0 + n], rope, "KbT"))
                            run_pipeline(jobs)

                        def vals():
                            for tt in range(n // 128):
                                kc = (c0 + tt * 128) // 128
                                bv = nb()
                                P.op("pe", MM(bank(bv)[:, 0:512], ckvn[:, tt * 128:(tt + 1) * 128], WV[:, :, :].rearrange("p h c -> p (h c)"),
                                              start=True, stop=True), reads=[ck_, "WV"], writes=[f"B{bv}"])
                                src = bank(bv)[:, 0:512].rearrange("p (a s c) -> p a s c", s=2, c=64)
                                dst = Vst[:, kc, 0:768].rearrange("p (a s c) -> p a s c", s=3, c=64)[:, :, 0:3:2, :]
                                P.op("act", ACT(dst, src, AF.Copy), reads=[f"B{bv}"], writes=["Vst"])
                                bw = nb()
                                for k in range(8):
                                    P.op("pe", MM(bank(bw)[:, 0:128], hj[:, k, tt * 128:(tt + 1) * 128], WVB[:, k, :], start=(k == 0), stop=(k == 7)),
                                         reads=["WVB", f"{hk_}{k}"], writes=[f"B{bw}"])
                                srcb = bank(bw)[:, 0:128].rearrange("p (g c) -> p g c", c=64)
                                for s_ in (0, 2):
                                    dstb = Vst[:, kc, 768:1152].rearrange("p (g s c) -> p g s c", s=3, c=64)[:, :, s_, :]
                                    P.op("dve", CP(dstb, srcb), reads=[f"B{bw}"], writes=["Vst"])
                        return preA, preB, main, vals

                    kvb = [kv_block(bj_, c0, n) for bj_, (c0, n) in enumerate(BLOCKS)]
                    kvb[0][0]()
                    kvb[0][1]()
                    for bj_ in range(len(BLOCKS)):
                        if bj_ + 1 < len(BLOCKS):
                            kvb[bj_ + 1][0]()
                        kvb[bj_][2]()
                        if bj_ + 1 < len(BLOCKS):
                            kvb[bj_ + 1][1]()
                        kvb[bj_][3]()
                    P.end()

                with ExitStack() as st:
                    wq = sb(st, "wq", [128, 8, 768], BF16)
                    wuq = sb(st, "wuq", [128, 2, 768], BF16)
                    wout = sb(st, "wout", [128, 8, 1024], BF16)
                    xj = sb(st, "xj", [128, 8, 512], F32)
                    hj = sb(st, "hj", [128, 8, 512], BF16)
                    QaT = sb(st, "QaT", [128, 8, 512], BF16)
                    QbT = sb(st, "QbT", [128, 4, 512], BF16)
                    cqn = sb(st, "cqn", [128, 2, 512], BF16)
                    PT = [sb(st, f"PT{i}", [128, 2, 512], BF16) for i in range(3)]
                    SR = mk_sets(st, 4)
                    rden = [SR[0]["ms"], SR[1]["ms"]]
                    win_v = WL["w_in"].rearrange("(k p) n -> p k n", p=128)
                    wuq_v = WL["w_uq"].rearrange("(k p) n -> p k n", p=128)
                    wout_v = WL["w_out"].rearrange("(k p) n -> p k n", p=128)
                    P.begin()
                    P.dma("pool", DMAS([(wq[:, k, 0:256], win_v[:, k, 0:256]) for k in range(8)]), writes=["wq"], key="wq", n=8)
                    P.dma("pool", DMAS([(wq[:, k, 256:768], win_v[:, k, 416:928]) for k in range(8)]), reads=["wq"], writes=["wq"], key="wq", n=8)
                    P.dma("pool", DMAS([(wuq[:, k, :], wuq_v[:, k, :]) for k in range(2)]), writes=["wuq"], key="wuq", n=2)
                    P.dma("pool", DMAS([(wout[:, k, :], wout_v[:, k, :]) for k in range(8)]), writes=["wout"], key="wout", n=8)
                    qblocks = BLOCKS[:4] if last else BLOCKS
                    for (c0, n) in qblocks:
                        lat = c0 < T_LAT
                        i_mod = 0 if lat else 1
                        P.dma("sp", DMA(xj[:, :, 0:n], xs_v[:, :, c0:c0 + n]), writes=["xj"], key="xj")
                        norm_mod(xj, n, A1, 0, i_mod, hj, SR)
                        bq = [nb(), nb()]
                        for c_ in range(2):
                            for k in range(8):
                                P.op("pe", MM(bank(bq[c_])[:, 0:n], wq[:, k, c_ * 128:(c_ + 1) * 128], hj[:, k, 0:n], start=(k == 0), stop=(k == 7)),
                                     reads=["wq", f"hj{k}"], writes=[f"B{bq[c_]}"])
                        S_ = next_set(SR)
                        S2_ = next_set(SR)
                        rstd_from([(bank(bq[c_])[:, 0:n], [f"B{bq[c_]}"]) for c_ in range(2)], 128, n, 1.0 / 16.0, ones_b,
                                  [(S_["sq"], f"sq{S_['i']}"), (S2_["sq"], f"sq{S2_['i']}")], (S_["ms"], f"ms{S_['i']}"))
                        for c_ in range(2):
                            P.op("dve", STT(cqn[:, c_, 0:n], bank(bq[c_])[:, 0:n], cols[:, 64 + c_:65 + c_], S_["ms"][:, 0:n], ALU.mult, ALU.mult),
                                 reads=[f"B{bq[c_]}", f"ms{S_['i']}", "cols"], writes=["cqn"])
                        jobs = []
                        for h in range(8):
                            def pre_q(bp, h=h, n=n):
                                for c_ in range(2):
                                    P.op("pe", MM(bank(bp)[0:96, 0:n], wuq[:, c_, h * 96:(h + 1) * 96], cqn[:, c_, 0:n], start=(c_ == 0), stop=(c_ == 1)),
                                         reads=["wuq", "cqn"], writes=[f"B{bp}"])
                            jobs.append(head_job(SR, pre_q, None, 96, n, 96 ** -0.5, ones_b[0:96, 0:96], gq[0:96, 0:1], PA, cosA, sinA, c0,
                                                 QaT[0:96, h, 0:n], lat, f"QaT{h}"))
                        for m in range(4):
                            def pre_qb(bp, m=m, n=n):
                                for k in range(8):
                                    P.op("pe", MM(bank(bp)[:, 0:n], wq[:, k, 256 + m * 128:256 + (m + 1) * 128], hj[:, k, 0:n], start=(k == 0), stop=(k == 7)),
                                         reads=["wq", f"hj{k}"], writes=[f"B{bp}"])
                            jobs.append(head_job(SR, pre_qb, None, 128, n, 0.125, bonesB, gq[:, 2:3], PB, cosB, sinB, c0,
                                                 QbT[:, m, 0:n], lat, f"QbT{m}"))
                        run_pipeline(jobs)
                        kchunks = list(range(18)) if lat else [16, 17]
                        items = []
                        for hh in range(16):
                            for pi in range(0, len(kchunks), 2):
                                items.append((hh, kchunks[pi:pi + 2], pi == 0, pi + 2 >= len(kchunks)))

                        def head_ops(hh):
                            if hh < 8:
                                return (lambda kc: KaT[0:96, hh, kc * 128:(kc + 1) * 128], QaT[0:96, hh, 0:n], f"QaT{hh}", "KaT",
                                        96 ** -0.5, (hh // 2) * 192 + (hh % 2) * 64, hh // 2, hh % 2)
                            q = hh - 8
                            kv = q // 4
                            r0 = (q % 2) * 64
                            return (lambda kc: KbT[r0:r0 + 64, kv, kc * 128:(kc + 1) * 128], QbT[r0:r0 + 64, q // 2, 0:n], f"QbT{q // 2}", "KbT",
                                    0.125, 768 + kv * 192 + (q % 2) * 64, 4 + q // 2, q % 2)

                        SB = [(0, 1), (2, 3), (4, 5)]
                        OB = [6, 7]
                        pend = []
                        for it, (hh, kcs, first, lastp) in enumerate(items):
                            kfn, qap, qkey, kkey, scl, voff, ochunk, par = head_ops(hh)
                            sp_ = it % 3
                            ps2 = PS2[sp_]
                            for i_, kc in enumerate(kcs):
                                P.op("pe", MM(ps2[:, i_ * 512:i_ * 512 + n], kfn(kc), qap, start=True, stop=True),
                                     reads=[kkey, qkey], writes=[f"B{SB[sp_][i_]}"])
                            if len(pend) >= 2:
                                pend.pop(0)()
                            ptv = PT[sp_]
                            nk = len(kcs)
                            P.op("act", ACT(ptv[:, 0:nk, 0:n], ps2[:, 0:nk * 512].rearrange("p (a c) -> p a c", c=512)[:, :, 0:n], AF.Exp, scale=scl),
                                 reads=[f"B{SB[sp_][i_]}" for i_ in range(nk)], writes=[f"PT{sp_}"])

                            def mk(hh=hh, kcs=kcs, first=first, lastp=lastp, sp_=sp_, voff=voff, ochunk=ochunk, par=par):
                                def run():
                                    ob = OB[hh % 2]
                                    for i_, kc in enumerate(kcs):
                                        P.op("pe", MM(bank(ob)[:, 0:n], Vst[:, kc, voff:voff + 128], PT[sp_][:, i_, 0:n],
                                                      start=(first and i_ == 0), stop=(lastp and i_ == len(kcs) - 1)),
                                             reads=["Vst", f"PT{sp_}"], writes=[f"B{ob}"])
                                    if lastp:
                                        o0 = par * 64
                                        d0 = 64 - o0
                                        rd = rden[hh % 2]
                                        P.op("dve", RECIP(rd[o0:o0 + 64, 0:n], bank(ob)[d0:d0 + 64, 0:n]), reads=[f"B{ob}"], writes=[f"ms{hh % 2}"])
                                        P.op("dve", TT(hj[o0:o0 + 64, ochunk, 0:n], bank(ob)[o0:o0 + 64, 0:n], rd[o0:o0 + 64, 0:n], ALU.mult),
                                             reads=[f"B{ob}", f"ms{hh % 2}"], writes=[f"hj{ochunk}"])
                                return run
                            pend.append(mk())
                        while pend:
                            pend.pop(0)()
                        rr[0] = 0
                        for m in range(8):
                            bo = m % 6
                            for c_ in range(8):
                                P.op("pe", MM(bank(bo)[:, 0:n], wout[:, c_, m * 128:(m + 1) * 128], hj[:, c_, 0:n], start=(c_ == 0), stop=(c_ == 7)),
                                     reads=["wout", f"hj{c_}"], writes=[f"B{bo}"])
                            P.op("dve", STT(xj[:, m, 0:n], bank(bo)[:, 0:n], Bcol(2, m, i_mod), xj[:, m, 0:n], ALU.mult, ALU.add),
                                 reads=[f"B{bo}", "xj", "modc"], writes=["xj"])
                        P.dma("sp", DMA(xs_v[:, :, c0:c0 + n], xj[:, :, 0:n]), reads=["xj"], key="xo")
                    P.end()

            if last:
                with ExitStack() as ffn:
                    ridx = sb(ffn, "ridx", [128, 2, 16], I32)
                    wts = sb(ffn, "wts", [128, 2, 16], F32)
                    flags = sb(ffn, "flags", [128, 48], I32)
                    P.flags_ap = flags
                    with ExitStack() as st:
                        SR = mk_sets(st, 4)
                        xT = sb(st, "xT", [128, 8, T_LAT], F32)
                        h2T = sb(st, "h2T", [128, 8, T_LAT], BF16)
                        hf = sb(st, "hf", [128, 8, 512], F32)
                        rt = sb(st, "rt", [128, 8, 8], F32)
                        Lg = sb(st, "Lg", [128, 16, 8], F32)
                        L2 = sb(st, "L2", [128, 16, 8], F32)
                        mk1 = sb(st, "mk1", [128, 16, 8], F32)
                        mk2 = sb(st, "mk2", [128, 16, 8], F32)
                        sm = sb(st, "sm", [128, 4, 16], F32)
                        maskb = sb(st, "maskb", [128, 16, 8], BF16)
                        tot_s = sb(st, "tot_s", [128, 16, 8], F32)
                        cum = sb(st, "cum", [128, 17, 8], F32)
                        val = sb(st, "val", [128, 16, 8], F32)
                        tmp = sb(st, "tmp", [128, 16, 8], F32)
                        rf = sb(st, "rf", [128, 2, 16], F32)
                        flagsf = sb(st, "flagsf", [128, 8, 6], F32)
                        ebase = sb(st, "ebase", [128, 16, 8], F32)
                        htok = [sb(st, f"htok{i}", [128, 1024], BF16) for i in range(2)]
                        fblocks = BLOCKS[:4]
                        P.begin()
                        for bi_, (c0, n) in enumerate(fblocks):
                            P.dma("sp", DMA(xT[:, :, c0:c0 + n], xs_v[:, :, c0:c0 + n]), writes=[f"xT{bi_}"], key=f"xT{bi_}")
                        P.dma("sp", DMA(rt[:], WL["router"].rearrange("(k p) e -> p k e", p=128)), writes=["rt"], key="rt")
                        P.dma("sp", DMA(ebase[:].rearrange("p a b -> p (a b)"), ebase_d[:, :]), writes=["ebase"], key="eb")
                        for bi_, (c0, n) in enumerate(fblocks):
                            r3 = bi_ % 2
                            rstd_from([(xT[:, k, c0:c0 + n], [f"xT{bi_}"]) for k in range(8)], 128, n, 1.0 / 32.0, ones_b,
                                      [(SR[0]["sq"], "sq0"), (SR[1]["sq"], "sq1")], (SR[2 + r3]["ms"], f"ms{2 + r3}"))
                            rst_ = SR[2 + r3]["ms"]
                            for k in range(8):
                                t_, tk = SR[k % 2]["ms"], f"ms{k % 2}"
                                P.op("dve", STT(t_[:, 0:n], xT[:, k, c0:c0 + n], A2[:, k, 0:1], rst_[:, 0:n], ALU.mult, ALU.mult),
                                     reads=[f"xT{bi_}", f"ms{2 + r3}", "A2"], writes=[tk])
                                P.op("act", ACT(hf[:, k, 0:n], t_[:, 0:n], AF.Identity, bias=Bcol(3, k, 0), scale=1.0),
                                     reads=[tk, "modc"], writes=[f"hf{k}"])
                                P.op("dve", CP(h2T[:, k, c0:c0 + n], hf[:, k, 0:n]), reads=[f"hf{k}"], writes=[f"h2T{k}"])
                            for tt in range(n // 128):
                                tg = (c0 // 128) + tt
                                bl = nb()
                                for k in range(8):
                                    P.op("pe", MM(bank(bl)[:, 0:8], hf[:, k, tt * 128:(tt + 1) * 128], rt[:, k, :], start=(k == 0), stop=(k == 7)),
                                         reads=[f"hf{k}", "rt"], writes=[f"B{bl}"])
                                P.op("dve", CP(Lg[:, tg, :], bank(bl)[:, 0:8]), reads=[f"B{bl}"], writes=["Lg"])
                        m1, m2, df, e2 = (sm[:, i, :] for i in range(4))
                        g1, g2 = wts[:, 0, :], wts[:, 1, :]
                        P.op("dve", RMAX(m1, Lg[:, :, :]), reads=["Lg"], writes=["m1"])
                        for e_ in range(8):
                            P.op("dve", TT(mk1[:, :, e_], Lg[:, :, e_], m1, ALU.is_equal), reads=["Lg", "m1"], writes=["mk1"])
                        P.op("dve", STT(L2[:, :, :], mk1[:, :, :], -1e30, Lg[:, :, :], ALU.mult, ALU.add), reads=["mk1", "Lg"], writes=["L2"])
                        P.op("dve", RMAX(m2, L2[:, :, :]), reads=["L2"], writes=["m2"])
                        for e_ in range(8):
                            P.op("dve", TT(mk2[:, :, e_], L2[:, :, e_], m2, ALU.is_equal), reads=["L2", "m2"], writes=["mk2"])
                        P.op("dve", TT(df, m2, m1, ALU.subtract), reads=["m1", "m2"], writes=["df"])
                        P.op("act", ACT(e2, df, AF.Exp), reads=["df"], writes=["e2"])
                        P.op("dve", TS(g1, e2, 1.0, None, ALU.add), reads=["e2"], writes=["wts"])
                        P.op("dve", RECIP(g1, g1), reads=["wts"], writes=["wts"])
                        P.op("dve", TT(g2, e2, g1, ALU.mult), reads=["e2", "wts"], writes=["wts"])
                        P.op("dve", TT(maskb[:, :, :], mk1[:, :, :], mk2[:, :, :], ALU.add), reads=["mk1", "mk2"], writes=["maskb"])
                        mflat = maskb[:, :, :].rearrange("p a b -> p (a b)")
                        bw_, bt_ = nb(), nb()
                        P.op("pe", MM(bank(bw_)[:, 0:128], triU, mflat), reads=["cbt", "maskb"], writes=[f"B{bw_}"])
                        P.op("pe", MM(bank(bt_)[:, 0:128], ones_b, mflat), reads=["cbt", "maskb"], writes=[f"B{bt_}"])
                        P.op("dve", CP(tot_s[:, :, :].rearrange("p a b -> p (a b)"), bank(bt_)[:, 0:128]), reads=[f"B{bt_}"], writes=["tot_s"])
                        P.op("dve", MEMSET(cum[:, 0, :], 0.0), writes=["cum"])
                        for tg in range(16):
                            P.op("dve", TT(cum[:, tg + 1, :], cum[:, tg, :], tot_s[:, tg, :], ALU.add), reads=["cum", "tot_s"], writes=["cum"])
                        P.op("dve", TT(val[:, :, :].rearrange("p a b -> p (a b)"), bank(bw_)[:, 0:128],
                                       cum[:, 0:16, :].rearrange("p a b -> p (a b)"), ALU.add), reads=[f"B{bw_}", "cum"], writes=["val"])
                        P.op("dve", TT(val[:, :, :], val[:, :, :], ebase[:, :, :], ALU.add), reads=["val", "ebase"], writes=["val"])
                        for j, mk_ in enumerate((mk1, mk2)):
                            P.op("dve", TT(tmp[:, :, :], mk_[:, :, :], val[:, :, :], ALU.mult), reads=["val", f"mk{j + 1}"], writes=["tmp"])
                            P.op("dve", RSUM(rf[:, j, :], tmp[:, :, :]), reads=["tmp"], writes=["rf"])
                        P.op("dve", CP(ridx[:, :, :], rf[:, :, :]), reads=["rf"], writes=["ridx"])
                        for b_ in range(6):
                            P.op("dve", TS(flagsf[:, :, b_], cum[:, 16, :], 512.0 + 256.0 * b_, None, ALU.is_gt), reads=["cum"], writes=["flagsf"])
                        P.op("dve", CP(flags[:, :], flagsf[:, :, :].rearrange("p a b -> p (a b)")), reads=["flagsf"], writes=["flags"])
                        for tg in range(16):
                            pz = PS2[tg % 2]
                            bk0 = 2 * (tg % 2)
                            for k in range(8):
                                P.op("pe", MM(pz[:, k * 128:(k + 1) * 128], h2T[:, k, tg * 128:(tg + 1) * 128], ident_b),
                                     reads=[f"h2T{k}", "cbt"], writes=[f"B{bk0 + k // 4}"])
                            ht = htok[tg % 2]
                            P.op("act", ACT(ht[:, 0:512], pz[:, 0:512], AF.Copy), reads=[f"B{bk0}"], writes=[f"htok{tg % 2}a"])
                            P.op("dve", CP(ht[:, 512:1024], pz[:, 512:1024]), reads=[f"B{bk0 + 1}"], writes=[f"htok{tg % 2}b"])
                            for j in range(2):
                                P.dma("pool", ISCAT(Gd[:, :], ridx[:, j, tg:tg + 1], ht[:, :], P.bc),
                                      reads=[f"htok{tg % 2}a", f"htok{tg % 2}b", "ridx"], key=f"sc{tg % 2}")
                        P.end()

                    GS = 4
                    groups = []
                    f0 = 0
                    while f0 < NFC:
                        gs = min(GS, NFC - f0)
                        groups.append((f0, gs))
                        f0 += gs
                    NRING = 4
                    with ExitStack() as st:
                        wgt = [sb(st, f"wgt{i}", [128, 8, GS * 128], BF16) for i in range(2)]
                        wut = [sb(st, f"wut{i}", [128, 8, GS * 128], BF16) for i in range(2)]
                        wdt = [sb(st, f"wdt{i}", [128, GS, 1024], BF16) for i in range(2)]
                        sgt = [sb(st, f"sgt{i}", [128, 512], F32) for i in range(2)]
                        actt = [sb(st, f"actt{i}", [128, GS, 512], BF16) for i in range(2)]
                        hT0 = [sb(st, f"hT0_{i}", [128, 8, 512], BF16) for i in range(2)]
                        hTx = [sb(st, f"hTx{i}", [128, 8, 256], BF16) for i in range(6)]
                        ya0 = sb(st, "ya0", [128, 4, 1024], F32)
                        yax = sb(st, "yax", [128, 12, 1024], F32)

                        def geom(blk):
                            return (0, 512) if blk == 0 else (512 + 256 * (blk - 1), 256)

                        def yslice(blk, s4, half):
                            if blk == 0:
                                return ya0[:, s4, half * 512:(half + 1) * 512]
                            return yax[:, (blk - 1) * 2 + s4, half * 512:(half + 1) * 512]
                        gtok = [sb(st, f"gtok{i}", [128, 1024], BF16) for i in range(NRING)]
                        P.begin()
                        ring = [0]
                        evq = [0]

                        def hbuf(e_, blk):
                            if blk == 0:
                                return hT0[e_ % 2], f"hT0_{e_ % 2}"
                            return hTx[blk - 1], f"hTx{blk - 1}"

                        def prep(e_, blk, cond):
                            hT, hkey = hbuf(e_, blk)
                            s0, W = geom(blk)
                            nst = W // 128
                            bufs = []
                            for s4 in range(nst):
                                r = ring[0] % NRING
                                ring[0] += 1
                                row0 = e_ * T_LAT + s0 + s4 * 128
                                P.dma("sp", DMA(gtok[r][:, :], Gd[row0:row0 + 128, :]), writes=[f"gt{r}"], key=f"gl{r}")
                                bufs.append(r)
                            for k in range(8):
                                bi = k % 4
                                for s4 in range(nst):
                                    P.op("pe", MM(bank(bi)[:, s4 * 128:(s4 + 1) * 128], gtok[bufs[s4]][:, k * 128:(k + 1) * 128], ident_b),
                                         reads=[f"gt{bufs[s4]}", "cbt"], writes=[f"B{bi}"], cond=cond)
                                evq[0] += 1
                                if evq[0] % 2 == 0:
                                    P.op("act", ACT(hT[:, k, 0:W], bank(bi)[:, 0:W], AF.Copy), reads=[f"B{bi}"], writes=[f"{hkey}_{k}"], cond=cond)
                                else:
                                    P.op("dve", CP(hT[:, k, 0:W], bank(bi)[:, 0:W]), reads=[f"B{bi}"], writes=[f"{hkey}_{k}"], cond=cond)

                        gi = 0
                        ai = 0
                        cq = [0]

                        def block_compute(e_, gidx, gs, b_, blk, cond):
                            nonlocal ai
                            hT, hkey = hbuf(e_, blk)
                            s0, W = geom(blk)
                            nst = W // 128
                            a_ = ai % 2
                            ai += 1
                            at = actt[a_]
                            for f in range(gs):
                                pg = (f % 2) * 2
                                pu = pg + 1
                                for k in range(8):
                                    P.op("pe", MM(bank(pg)[:, 0:W], wgt[b_][:, k, f * 128:(f + 1) * 128], hT[:, k, 0:W], start=(k == 0), stop=(k == 7)),
                                         reads=[f"wgt{b_}", f"{hkey}_{k}"], writes=[f"B{pg}"], cond=cond)
                                for k in range(8):
                                    P.op("pe", MM(bank(pu)[:, 0:W], wut[b_][:, k, f * 128:(f + 1) * 128], hT[:, k, 0:W], start=(k == 0), stop=(k == 7)),
                                         reads=[f"wut{b_}", f"{hkey}_{k}"], writes=[f"B{pu}"], cond=cond)
                                sg = sgt[f % 2]
                                P.op("act", ACT(sg[:, 0:W], bank(pg)[:, 0:W], AF.Silu), reads=[f"B{pg}"], writes=[f"sg{f % 2}"], cond=cond)
                                P.op("dve", TT(at[:, f, 0:W], sg[:, 0:W], bank(pu)[:, 0:W], ALU.mult),
                                     reads=[f"sg{f % 2}", f"B{pu}"], writes=[f"act{a_}_{f}"], cond=cond)
                            for half in range(2):
                                for s4 in range(nst):
                                    bd = 4 + (half * nst + s4) % 4
                                    for f in range(gs):
                                        P.op("pe", MM(bank(bd)[:, 0:512], at[:, f, s4 * 128:(s4 + 1) * 128], wdt[b_][:, f, half * 512:(half + 1) * 512],
                                                      start=(f == 0), stop=(f == gs - 1)),
                                             reads=[f"wdt{b_}", f"act{a_}_{f}"], writes=[f"B{bd}"], cond=cond)
                                    ya = yslice(blk, s4, half)
                                    yk = f"yacc{blk}_{s4}_{half}"
                                    if gidx == 0:
                                        cq[0] += 1
                                        if cq[0] % 2 == 0:
                                            P.op("act", ACT(ya, bank(bd)[:, 0:512], AF.Copy), reads=[f"B{bd}"], writes=[yk], cond=cond)
                                        else:
                                            P.op("dve", CP(ya, bank(bd)[:, 0:512]), reads=[f"B{bd}"], writes=[yk], cond=cond)
                                    else:
                                        P.op("dve", TT(ya, bank(bd)[:, 0:512], ya, ALU.add), reads=[f"B{bd}", yk], writes=[yk], cond=cond)

                        prep(0, 0, None)
                        for e_ in range(8):
                            wg_v = WL["wg"][e_].rearrange("(k p) f -> p k f", p=128)
                            wu_v = WL["wu"][e_].rearrange("(k p) f -> p k f", p=128)
                            wd_v = WL["wd"][e_].rearrange("(f p) d -> p f d", p=128)
                            for gidx, (f0, gs) in enumerate(groups):
                                b_ = gi % 2
                                gi += 1
                                P.dma("pool", DMAS([(wgt[b_][:, k, 0:gs * 128], wg_v[:, k, f0 * 128:(f0 + gs) * 128]) for k in range(8)]),
                                      writes=[f"wgt{b_}"], key=f"wg{b_}", n=8)
                                P.dma("pool", DMAS([(wut[b_][:, k, 0:gs * 128], wu_v[:, k, f0 * 128:(f0 + gs) * 128]) for k in range(8)]),
                                      writes=[f"wut{b_}"], key=f"wu{b_}", n=8)
                                P.dma("pool", DMAS([(wdt[b_][:, f, :], wd_v[:, f0 + f, :]) for f in range(gs)]),
                                      writes=[f"wdt{b_}"], key=f"wd{b_}", n=gs)
                                for blk in range(7):
                                    cond = None if blk == 0 else (e_, gidx, blk)
                                    if gidx == 0 and blk > 0:
                                        prep(e_, blk, cond)
                                    block_compute(e_, gidx, gs, b_, blk, cond)
                                if gidx == 3 and e_ < 7:
                                    prep(e_ + 1, 0, None)
                            row0 = e_ * T_LAT
                            P.dma("sp", DMA(Yd[row0:row0 + 512, :].rearrange("(s p) d -> p s d", p=128), ya0[:, :, :]),
                                  reads=[f"yacc0_{s4}_{half}" for s4 in range(4) for half in range(2)], key="yw0")
                            P.dma("sp", DMA(Yd[row0 + 512:row0 + T_LAT, :].rearrange("(s p) d -> p s d", p=128), yax[:, :, :]),
                                  reads=[f"yacc{blk}_{s4}_{half}" for blk in range(1, 7) for s4 in range(2) for half in range(2)], key="ywx")
                        P.end()

                    with ExitStack() as st:
                        xo = [sb(st, f"xo{i}", [128, 8, 512], F32) for i in range(2)]
                        yg = [[sb(st, f"yg{i}_{j}", [128, 1024], F32) for j in range(2)] for i in range(3)]
                        ttl = [sb(st, f"ttl{i}", [128, 1024], F32) for i in range(2)]
                        ot = [sb(st, f"ot{i}", [128, 1024], F32) for i in range(2)]
                        g2row = sb(st, "g2row", [128, 1024], F32)
                        dg = [sb(st, f"dg{i}", [128, 128], F32) for i in range(2)]
                        onesf = sb(st, "onesf", [128, 128], F32)
                        P.begin()
                        P.op("dve", MEMSET(onesf[:], 1.0), writes=["onesf"])
                        for m in range(8):
                            d_ = dg[m % 2]
                            P.op("dve", TS(d_[:, :], ident_f[:, :], Bcol(5, m, 0), None, ALU.mult), reads=["ident_f", "modc"], writes=[f"dg{m % 2}"])
                            bq_ = 6 + (m % 2)
                            P.op("pe", MM(bank(bq_)[:, 0:128], onesf[:, :], d_[:, :]), reads=["onesf", f"dg{m % 2}"], writes=[f"B{bq_}"])
                            P.op("act", ACT(g2row[:, m * 128:(m + 1) * 128], bank(bq_)[:, 0:128], AF.Copy), reads=[f"B{bq_}"], writes=["g2row"])
                        for tg in range(16):
                            b4 = tg // 4
                            xb = xo[b4 % 2]
                            if tg % 4 == 0:
                                P.dma("sp", DMA(xb[:, :, :], xs_v[:, :, b4 * 512:(b4 + 1) * 512]), writes=[f"xo{b4 % 2}"], key=f"xo{b4 % 2}")
                            pz = PS2[tg % 2]
                            bk0 = 2 * (tg % 2)
                            for k in range(8):
                                P.op("pe", TR(pz[:, k * 128:(k + 1) * 128], xb[:, k, (tg % 4) * 128:(tg % 4 + 1) * 128], ident_f[:, :]),
                                     reads=[f"xo{b4 % 2}", "ident_f"], writes=[f"B{bk0 + k // 4}"])
                            y0, y1 = yg[tg % 3]
                            for j in range(2):
                                P.dma("pool", IGATH(yg[tg % 3][j][:, :], Yd[:, :], ridx[:, j, tg:tg + 1], P.bc),
                                      reads=["ridx"], writes=[f"yg{tg % 3}_{j}"], key=f"yg{tg % 3}_{j}")
                            t_ = ttl[tg % 2]
                            tk = f"ttl{tg % 2}"
                            P.op("act", ACT(t_[:, :], y1[:, :], AF.Identity, scale=wts[:, 1, tg:tg + 1]), reads=[f"yg{tg % 3}_1", "wts"], writes=[tk])
                            P.op("dve", STT(t_[:, :], y0[:, :], wts[:, 0, tg:tg + 1], t_[:, :], ALU.mult, ALU.add),
                                 reads=[f"yg{tg % 3}_0", "wts", tk], writes=[tk])
                            P.op("dve", TT(t_[:, :], t_[:, :], g2row[:, :], ALU.mult), reads=[tk, "g2row"], writes=[tk])
                            o_ = ot[tg % 2]
                            P.op("dve", TT(o_[:, 0:512], t_[:, 0:512], pz[:, 0:512], ALU.add), reads=[tk, f"B{bk0}"], writes=[f"ot{tg % 2}a"])
                            P.op("dve", TT(o_[:, 512:1024], t_[:, 512:1024], pz[:, 512:1024], ALU.add), reads=[tk, f"B{bk0 + 1}"], writes=[f"ot{tg % 2}b"])
                            P.dma("sp", DMA(out_d[tg * 128:(tg + 1) * 128, :], o_[:, :]), reads=[f"ot{tg % 2}a", f"ot{tg % 2}b"], key=f"out{tg % 2}")
                        P.end()
                continue

            with ExitStack() as ffn:
                ntok = T_LAT if last else T_ALL
                fblocks = BLOCKS[:4] if last else BLOCKS
                xT = sb(ffn, "xT", [128, 8, ntok], F32)
                h2T = sb(ffn, "h2T", [128, 8, ntok], BF16)
                if last:
                    gT = sb(ffn, "gT", [128, T_LAT], F32)
                with ExitStack() as st:
                    SR = mk_sets(ffn, 4)
                    if last:
                        hf = sb(st, "hf", [128, 8, 512], F32)
                        rt = sb(st, "rt", [128, 8, 8], F32)
                        Lg = sb(st, "Lg", [128, 16, 8], F32)
                        L2 = sb(st, "L2", [128, 16, 8], F32)
                        mk1 = sb(st, "mk1", [128, 16, 8], F32)
                        mk2 = sb(st, "mk2", [128, 16, 8], F32)
                        gts = sb(st, "gts", [128, 16, 8], F32)
                        sm = sb(st, "sm", [128, 6, 16], F32)
                    P.begin()
                    for bi_, (c0, n) in enumerate(fblocks):
                        P.dma("sp", DMA(xT[:, :, c0:c0 + n], xs_v[:, :, c0:c0 + n]), writes=[f"xT{bi_}"], key=f"xT{bi_}")
                    if last:
                        P.dma("sp", DMA(rt[:], WL["router"].rearrange("(k p) e -> p k e", p=128)), writes=["rt"], key="rt")

                    norm_fns = []
                    for bi_, (c0, n) in enumerate(fblocks):
                        def norm_blk(bi_=bi_, c0=c0, n=n):
                            i_mod = 0 if c0 < T_LAT else 1

                            r3 = bi_ % 2
                            rstd_from([(xT[:, k, c0:c0 + n], [f"xT{bi_}"]) for k in range(8)], 128, n, 1.0 / 32.0, ones_b,
                                      [(SR[0]["sq"], "sq0"), (SR[1]["sq"], "sq1")], (SR[2 + r3]["ms"], f"ms{2 + r3}"))
                            rst_ = SR[2 + r3]["ms"]
                            for k in range(8):
                                t_, tk = SR[k % 2]["ms"], f"ms{k % 2}"
                                P.op("dve", STT(t_[:, 0:n], xT[:, k, c0:c0 + n], A2[:, k, i_mod:i_mod + 1], rst_[:, 0:n], ALU.mult, ALU.mult),
                                     reads=[f"xT{bi_}", f"ms{2 + r3}", "A2"], writes=[tk])
                                if not last:
                                    if k % 2 == 0:
                                        P.op("dve", TS(h2T[:, k, c0:c0 + n], t_[:, 0:n], Bcol(3, k, i_mod), None, ALU.add),
                                             reads=[tk, "modc"], writes=[f"h2T{k}_{bi_}"])
                                    else:
                                        P.op("act", ACT(h2T[:, k, c0:c0 + n], t_[:, 0:n], AF.Identity, bias=Bcol(3, k, i_mod), scale=1.0),
                                             reads=[tk, "modc"], writes=[f"h2T{k}_{bi_}"])
                                else:
                                    P.op("act", ACT(hf[:, k, 0:n], t_[:, 0:n], AF.Identity, bias=Bcol(3, k, i_mod), scale=1.0),
                                         reads=[tk, "modc"], writes=[f"hf{k}"])
                                    P.op("pool", CP(h2T[:, k, c0:c0 + n], hf[:, k, 0:n]), reads=[f"hf{k}"], writes=[f"h2T{k}"])
                            if last:
                                for tt in range(n // 128):
                                    tg = (c0 // 128) + tt
                                    bl = nb()
                                    for k in range(8):
                                        P.op("pe", MM(bank(bl)[:, 0:8], hf[:, k, tt * 128:(tt + 1) * 128], rt[:, k, :], start=(k == 0), stop=(k == 7)),
                                             reads=[f"hf{k}", "rt"], writes=[f"B{bl}"])
                                    P.op("dve", CP(Lg[:, tg, :], bank(bl)[:, 0:8]), reads=[f"B{bl}"], writes=["Lg"])
                        norm_fns.append(norm_blk)
                    norm_fns[0]()
                    if last:
                        m1, m2, df, e2, g1, g2 = (sm[:, i, :] for i in range(6))
                        P.op("dve", RMAX(m1, Lg[:, :, :]), reads=["Lg"], writes=["m1"])
                        for e_ in range(8):
                            P.op("dve", TT(mk1[:, :, e_], Lg[:, :, e_], m1, ALU.is_equal), reads=["Lg", "m1"], writes=["mk1"])
                        P.op("dve", STT(L2[:, :, :], mk1[:, :, :], -1e30, Lg[:, :, :], ALU.mult, ALU.add), reads=["mk1", "Lg"], writes=["L2"])
                        P.op("dve", RMAX(m2, L2[:, :, :]), reads=["L2"], writes=["m2"])
                        for e_ in range(8):
                            P.op("dve", TT(mk2[:, :, e_], L2[:, :, e_], m2, ALU.is_equal), reads=["L2", "m2"], writes=["mk2"])
                        P.op("dve", TT(df, m2, m1, ALU.subtract), reads=["m1", "m2"], writes=["df"])
                        P.op("act", ACT(e2, df, AF.Exp), reads=["df"], writes=["e2"])
                        P.op("dve", TS(g1, e2, 1.0, None, ALU.add), reads=["e2"], writes=["g1"])
                        P.op("dve", RECIP(g1, g1), reads=["g1"], writes=["g1"])
                        P.op("dve", TT(g2, e2, g1, ALU.mult), reads=["e2", "g1"], writes=["g2"])
                        for e_ in range(8):
                            P.op("dve", TT(gts[:, :, e_], mk1[:, :, e_], g1, ALU.mult), reads=["mk1", "g1"], writes=["gts"])
                            P.op("dve", TT(mk2[:, :, e_], mk2[:, :, e_], g2, ALU.mult), reads=["mk2", "g2"], writes=["mk2"])
                        P.op("dve", TT(gts[:, :, :], gts[:, :, :], mk2[:, :, :], ALU.add), reads=["gts", "mk2"], writes=["gts"])
                        for q4 in range(4):
                            bg = nb()
                            for tt in range(4):
                                tg = q4 * 4 + tt
                                P.op("pe", TR(bank(bg)[0:8, tt * 128:(tt + 1) * 128], gts[:, tg, :], ident_f[:, :]),
                                     reads=["gts", "ident_f"], writes=[f"B{bg}"])
                            P.op("dve", CP(gT[0:8, q4 * 512:(q4 + 1) * 512], bank(bg)[0:8, 0:512]), reads=[f"B{bg}"], writes=["gT"])

                GS = 4
                groups = []
                f0 = 0
                while f0 < NFC:
                    gs = min(GS, NFC - f0)
                    groups.append((f0, gs))
                    f0 += gs
                with ExitStack() as st:
                    wgt = [sb(st, f"wgt{i}", [128, 8, GS * 128], BF16) for i in range(2)]
                    wut = [sb(st, f"wut{i}", [128, 8, GS * 128], BF16) for i in range(2)]
                    wdt = [sb(st, f"wdt{i}", [128, GS, 1024], BF16) for i in range(2)]
                    sgt = [sb(st, f"sgt{i}", [128, 512], F32) for i in range(2)]
                    actt = [sb(st, f"actt{i}", [128, GS, 512], BF16) for i in range(2)]
                    if last:
                        Ge = [sb(st, f"Ge{i}", [128, T_LAT], BF16) for i in range(2)]
                        selE = sb(st, "selE", [128, 8, 128], F32)
                    nexp = 8 if last else 1
                    if last:
                        P.dma("sp", DMA(selE[0:8, :, :], selE_d[:, :, :]), writes=["selE"], key="selE")
                    gi = 0
                    ai = 0
                    rr[0] = 0
                    for e_ in range(nexp):
                        wg_v = WL["wg"][e_].rearrange("(k p) f -> p k f", p=128)
                        wu_v = WL["wu"][e_].rearrange("(k p) f -> p k f", p=128)
                        wd_v = WL["wd"][e_].rearrange("(f p) d -> p f d", p=128)
                        if last:
                            ge = Ge[e_ % 2]
                            for q4 in range(4):
                                bg = 6 + (q4 % 2)
                                P.op("pe", MM(bank(bg)[:, 0:512], selE[0:8, e_, :], gT[0:8, q4 * 512:(q4 + 1) * 512]),
                                     reads=["selE", "gT"], writes=[f"B{bg}"])
                                P.op("act", ACT(ge[:, q4 * 512:(q4 + 1) * 512], bank(bg)[:, 0:512], AF.Copy), reads=[f"B{bg}"], writes=[f"Ge{e_ % 2}"])
                        for (f0, gs) in groups:
                            b_ = gi % 2
                            gi += 1
                            P.dma("pool", DMAS([(wgt[b_][:, k, 0:gs * 128], wg_v[:, k, f0 * 128:(f0 + gs) * 128]) for k in range(8)]),
                                  writes=[f"wgt{b_}"], key=f"wg{b_}", n=8)
                            P.dma("pool", DMAS([(wut[b_][:, k, 0:gs * 128], wu_v[:, k, f0 * 128:(f0 + gs) * 128]) for k in range(8)]),
                                  writes=[f"wut{b_}"], key=f"wu{b_}", n=8)
                            P.dma("pool", DMAS([(wdt[b_][:, f, :], wd_v[:, f0 + f, :]) for f in range(gs)]),
                                  writes=[f"wdt{b_}"], key=f"wd{b_}", n=gs)
                            for bidx, (c0, n) in enumerate(fblocks):
                                if e_ == 0 and f0 == 0 and bidx + 1 < len(fblocks):
                                    norm_fns[bidx + 1]()
                                i_mod = 0 if c0 < T_LAT else 1
                                a_ = ai % 2
                                ai += 1
                                at = actt[a_]
                                for f in range(gs):
                                    pg = (f % 2) * 2
                                    pu = pg + 1
                                    for k in range(8):
                                        P.op("pe", MM(bank(pg)[:, 0:n], wgt[b_][:, k, f * 128:(f + 1) * 128], h2T[:, k, c0:c0 + n], start=(k == 0), stop=(k == 7)),
                                             reads=[f"wgt{b_}", f"h2T{k}_{bidx}"], writes=[f"B{pg}"])
                                    for k in range(8):
                                        P.op("pe", MM(bank(pu)[:, 0:n], wut[b_][:, k, f * 128:(f + 1) * 128], h2T[:, k, c0:c0 + n], start=(k == 0), stop=(k == 7)),
                                             reads=[f"wut{b_}", f"h2T{k}_{bidx}"], writes=[f"B{pu}"])
                                    sg = sgt[f % 2]
                                    P.op("act", ACT(sg[:, 0:n], bank(pg)[:, 0:n], AF.Silu), reads=[f"B{pg}"], writes=[f"sg{f % 2}"])
                                    if last:
                                        P.op("dve", TT(sg[:, 0:n], sg[:, 0:n], Ge[e_ % 2][:, c0:c0 + n], ALU.mult),
                                             reads=[f"sg{f % 2}", f"Ge{e_ % 2}"], writes=[f"sg{f % 2}"])
                                    P.op("dve", TT(at[:, f, 0:n], sg[:, 0:n], bank(pu)[:, 0:n], ALU.mult),
                                         reads=[f"sg{f % 2}", f"B{pu}"], writes=[f"act{a_}_{f}"])
                                for m in range(8):
                                    bd = 4 + (m % 4)
                                    for f in range(gs):
                                        P.op("pe", MM(bank(bd)[:, 0:n], wdt[b_][:, f, m * 128:(m + 1) * 128], at[:, f, 0:n], start=(f == 0), stop=(f == gs - 1)),
                                             reads=[f"wdt{b_}", f"act{a_}_{f}"], writes=[f"B{bd}"])
                                    P.op("dve", STT(xT[:, m, c0:c0 + n], bank(bd)[:, 0:n], Bcol(5, m, i_mod), xT[:, m, c0:c0 + n], ALU.mult, ALU.add),
                                         reads=[f"B{bd}", "modc", f"xT{m}_{c0}"], writes=[f"xT{m}_{c0}"])
                                    if (not last) and e_ == nexp - 1 and f0 + gs == NFC:
                                        P.dma("sp", DMA(xs_v[:, m, c0:c0 + n], xT[:, m, c0:c0 + n]), reads=[f"xT{m}_{c0}"], key="xo")
                    P.end()

                if not last:
                    pass
                else:
                    with ExitStack() as st:
                        ot = [sb(st, f"ot{i}", [128, 1024], F32) for i in range(2)]
                        P.begin()
                        for tt in range(16):
                            o_ = ot[tt % 2]
                            pz = PS2[tt % 2]
                            for k in range(8):
                                P.op("pe", TR(pz[:, k * 128:(k + 1) * 128], xT[:, k, tt * 128:(tt + 1) * 128], ident_f[:, :]),
                                     reads=["ident_f"], writes=[f"PZ{tt % 2}"])
                            P.op("act", ACT(o_[:, 0:512], pz[:, 0:512], AF.Copy), reads=[f"PZ{tt % 2}"], writes=[f"ot{tt % 2}a"])
                            P.op("dve", CP(o_[:, 512:1024], pz[:, 512:1024]), reads=[f"PZ{tt % 2}"], writes=[f"ot{tt % 2}b"])
                            P.dma("sp", DMA(out_d[tt * 128:(tt + 1) * 128, :], o_[:, :]), reads=[f"ot{tt % 2}a", f"ot{tt % 2}b"], key=f"out{tt % 2}")
                        P.end()
    return nc


_CACHE = {}


def _prep_inputs(inputs):
    f = lambda a: np.ascontiguousarray(np.asarray(a, dtype=np.float32))
    consts = make_consts()
    shared = dict(consts)
    for L in range(2):
        p = f"l{L}_"
        shared[p + "w_mod"] = f(inputs[p + "w_mod"])
        vecs = np.concatenate([f(inputs[p + "b_mod"]).reshape(48, 128), f(inputs[p + "norm_attn"]).reshape(8, 128),
                               f(inputs[p + "norm_ffn"]).reshape(8, 128), f(inputs[p + "q_lat_norm"]).reshape(2, 128),
                               f(inputs[p + "kv_lat_norm"]).reshape(1, 128)], axis=0)
        shared[p + "vecs"] = np.ascontiguousarray(vecs)
        for nm in ("w_in", "w_uq", "w_ukv", "w_out"):
            shared[p + nm] = f(inputs[p + nm])
        for nm in ("mla_q_gain", "mla_k_gain", "gqa_q_gain", "gqa_k_gain"):
            shared[p + nm] = f(inputs[p + nm]).reshape(-1, 1)
    for nm in ("l0_ffn_w_gate", "l0_ffn_w_up", "l0_ffn_w_down", "l1_router", "l1_exp_w_gate", "l1_exp_w_up", "l1_exp_w_down"):
        shared[nm] = f(inputs[nm])
    x = f(inputs["x"])
    ctx = f(inputs["ctx"])
    c = f(inputs["c"])
    cc = f(inputs["c_ctx"]).reshape(8, 128)
    in_maps = []
    for b in range(8):
        m = dict(shared)
        m["x"] = x[b]
        m["ctx"] = ctx[b]
        m["cvec"] = np.ascontiguousarray(np.concatenate([c[b].reshape(8, 128), cc], axis=0))
        in_maps.append(m)
    return in_maps


def kernel(**inputs):
    if "nc" not in _CACHE:
        _CACHE["nc"] = build_program()
    nc = _CACHE["nc"]
    in_maps = _prep_inputs(inputs)
    res = run_bass_kernel_spmd(nc, in_maps, core_ids=list(range(8)))
    out = np.stack([np.asarray(res.results[b]["out"], dtype=np.float32) for b in range(8)], axis=0)
    return out
```

```python
import numpy as np
from contextlib import ExitStack
import concourse.bass as bass
import concourse.mybir as mybir
from concourse.bass_utils import run_bass_kernel_spmd

F32 = mybir.dt.float32
BF16 = mybir.dt.bfloat16
I32 = mybir.dt.int32
AF = mybir.ActivationFunctionType
ALU = mybir.AluOpType
AX = mybir.AxisListType

ENGS = ("pe", "act", "dve", "pool", "sp")
ENGOBJ = {"pe": "tensor", "act": "scalar", "dve": "vector", "pool": "gpsimd", "sp": "sync"}

T_LAT, T_CTX, T_ALL = 2048, 256, 2304
EPS = 1e-6
THETA = 10000.0
FF = 2816
NFC = FF // 128


class _Op:
    __slots__ = ("eng", "fn", "deps", "is_dma", "dkey", "sig", "idx", "dcount", "ndma", "cond")

    def __init__(self, eng, fn, is_dma=False, dkey=None, ndma=1, cond=None):
        self.cond = cond
        self.eng = eng
        self.fn = fn
        self.deps = []
        self.is_dma = is_dma
        self.dkey = dkey
        self.sig = False
        self.idx = None
        self.dcount = None
        self.ndma = ndma


class Prog:
    NDMA = 14

    def __init__(self, nc):
        self.nc = nc
        self.sets = []
        for s in range(2):
            d = {e: nc.alloc_semaphore(name=f"s{s}_{e}") for e in ENGS}
            d["dma"] = [nc.alloc_semaphore(name=f"s{s}_d{i}") for i in range(self.NDMA)]
            self.sets.append(d)
        self.phase_no = 0
        self.count = 0
        self.dtot = {}
        self.limit = None
        self.ops = None
        self.dirty = [False, False]
        self.flags_ap = None
        self.nlvl = 6
        self.regs = {}
        self.loaded = {}
        with nc.Block() as block:
            for e in ENGS:
                def make(e):
                    def body(eng):
                        for st in self.sets:
                            eng.sem_clear(st[e])
                            if e == "pool":
                                for sm in st["dma"]:
                                    eng.sem_clear(sm)
                        if e == "pool":
                            r = eng.alloc_register("idma_bound")
                            eng.reg_mov(r, 8 * T_LAT - 1)
                            self.bc = eng.snap(r)
                    return body
                getattr(block, ENGOBJ[e])(make(e))

    def begin(self):
        self.ops = []
        self.lastw = {}
        self.readers = {}
        self.dkeys = {}

    def _add(self, op, reads, writes):
        deps = []
        for r in reads:
            w = self.lastw.get(r)
            if w is not None:
                deps.append(w)
        for wk in writes:
            w = self.lastw.get(wk)
            if w is not None:
                deps.append(w)
            deps.extend(self.readers.get(wk, ()))
        seen = set()
        for d in deps:
            if d is op or id(d) in seen:
                continue
            seen.add(id(d))
            if d.eng == "pe" and op.eng == "pe" and not d.is_dma and not op.is_dma:
                continue
            op.deps.append(d)
        for r in reads:
            self.readers.setdefault(r, []).append(op)
        for wk in writes:
            self.lastw[wk] = op
            self.readers[wk] = []
        self.ops.append(op)
        return op

    def op(self, eng, fn, reads=(), writes=(), cond=None):
        return self._add(_Op(eng, fn, cond=cond), list(reads), list(writes))

    def dma(self, queue, fn, reads=(), writes=(), key=None, n=1):
        if key not in self.dkeys:
            self.dkeys[key] = len(self.dkeys)
            assert len(self.dkeys) <= self.NDMA, "too many dma keys in phase"
        return self._add(_Op(queue, fn, True, key, n), list(reads), list(writes))

    def end(self):
        nc = self.nc
        ops = self.ops
        self.count += 1
        if self.limit is not None and self.count > self.limit:
            self.ops = None
            return
        if self.limit is not None and self.count == self.limit and getattr(self, "oplimit", None) is not None:
            ops = ops[:self.oplimit]
            print("phase ops total", len(self.ops), "emitting", len(ops))
        cur = self.phase_no % 2
        sems = self.sets[cur]
        other = self.sets[1 - cur]
        for o in ops:
            for d in o.deps:
                d.sig = True
        cnt = {e: 0 for e in ENGS}
        dcnt = {k: self.dtot.get(i, 0) for k, i in self.dkeys.items()}
        for o in ops:
            if o.is_dma:
                dcnt[o.dkey] = dcnt.get(o.dkey, 0) + 16 * o.ndma
                o.dcount = dcnt[o.dkey]
            elif o.sig:
                cnt[o.eng] += 1
                o.idx = cnt[o.eng]
        per = {e: [] for e in ENGS}
        for o in ops:
            per[o.eng].append(o)
        clear_other = self.dirty[1 - cur]
        dkeys = self.dkeys

        def dep_kv(d):
            if d.is_dma:
                return ("dma", dkeys[d.dkey]), d.dcount
            return d.eng, d.idx

        def do_waits(eng, need, waited):
            for k, v in need.items():
                s = self.sets[0]["dma"][k[1]] if isinstance(k, tuple) else sems[k]
                eng.wait_ge(s, v)
                waited[k] = v

        def emit_op(e, eng, o, waited):
            need = {}
            for d in o.deps:
                k, v = dep_kv(d)
                if waited.get(k, 0) >= v:
                    continue
                if need.get(k, 0) < v:
                    need[k] = v
            do_waits(eng, need, waited)
            ins = o.fn(eng)
            if o.is_dma:
                lst = ins if isinstance(ins, (list, tuple)) else [ins]
                assert len(lst) == o.ndma, (len(lst), o.ndma)
                for i_ in lst:
                    i_.then_inc(self.sets[0]["dma"][dkeys[o.dkey]], 16)
            elif o.sig:
                ins.then_inc(sems[e], 1)

        NLVL = self.nlvl

        def skip_regions(e, eng, regions, conds, snap):
            emitted = False
            for region in regions:
                need = {}
                nsig = 0
                for r_ in region:
                    if r_.sig:
                        nsig += 1
                    for d in r_.deps:
                        if d.cond in conds:
                            continue
                        k, v = dep_kv(d)
                        if snap.get(k, 0) >= v:
                            continue
                        if need.get(k, 0) < v:
                            need[k] = v
                do_waits(eng, need, snap)
                if nsig:
                    eng.sem_inc(sems[e], nsig)
                    emitted = True
            if not emitted:
                eng.nop()

        def emit_chain(e, eng, regions, waited):
            region = regions[0]
            lvl = region[0].cond[2]
            reg = self.regs[(e, lvl)]
            snap = dict(waited)
            conds = set(r[0].cond for r in regions)
            with eng.If_ne(reg, 0):
                w2 = dict(snap)
                for r_ in region:
                    emit_op(e, eng, r_, w2)
                if len(regions) > 1:
                    emit_chain(e, eng, regions[1:], w2)
            with eng.Else():
                skip_regions(e, eng, regions, conds, snap)
            return snap

        def emit(e, eng):
            waited = {}
            if clear_other:
                eng.sem_clear(other[e])
            lst = per[e]
            i = 0
            while i < len(lst):
                o = lst[i]
                if o.cond is None:
                    emit_op(e, eng, o, waited)
                    i += 1
                    continue
                eg = o.cond[:2]
                j = i
                while j < len(lst) and lst[j].cond is not None and lst[j].cond[:2] == eg:
                    j += 1
                chain = lst[i:j]
                i = j
                assert all(not r_.is_dma for r_ in chain)
                regions = []
                for r_ in chain:
                    if regions and regions[-1][0].cond == r_.cond:
                        regions[-1].append(r_)
                    else:
                        regions.append([r_])
                lv = [r[0].cond[2] for r in regions]
                assert lv == sorted(set(lv)), lv
                eidx = eg[0]
                if self.loaded.get(e) != eidx:
                    for lvl in range(1, NLVL + 1):
                        if (e, lvl) not in self.regs:
                            self.regs[(e, lvl)] = eng.alloc_register(f"fl_{e}_{lvl}")
                        c_ = eidx * NLVL + lvl - 1
                        eng.reg_load(self.regs[(e, lvl)], self.flags_ap[0:1, c_:c_ + 1])
                    self.loaded[e] = eidx
                waited = emit_chain(e, eng, regions, waited)
            last = {}
            for o in per[e]:
                if o.is_dma:
                    last[o.dkey] = o.dcount
            for k, v in last.items():
                if waited.get(("dma", dkeys[k]), 0) < v:
                    eng.wait_ge(self.sets[0]["dma"][dkeys[k]], v)

        with nc.Block() as block:
            for e in ENGS:
                def make(e):
                    def body(eng):
                        emit(e, eng)
                    return body
                getattr(block, ENGOBJ[e])(make(e))
        for k, i in self.dkeys.items():
            self.dtot[i] = dcnt[k]
        self.dirty[cur] = True
        self.phase_no += 1
        self.ops = None


def MM(out, lhsT, rhs, start=True, stop=True):
    return lambda e: e.matmul(out, lhsT=lhsT, rhs=rhs, start=start, stop=stop)


def TR(out, in_, ident):
    return lambda e: e.transpose(out, in_, ident)


def ACT(out, in_, func, bias=None, scale=None):
    kw = {}
    if bias is not None:
        kw["bias"] = bias
    if scale is not None:
        kw["scale"] = scale
    return lambda e: e.activation(out=out, in_=in_, func=func, **kw)


def TT(out, in0, in1, op):
    return lambda e: e.tensor_tensor(out=out, in0=in0, in1=in1, op=op)


def STT(out, in0, scalar, in1, op0, op1):
    return lambda e: e.scalar_tensor_tensor(out=out, in0=in0, scalar=scalar, in1=in1, op0=op0, op1=op1)


def TS(out, in0, s1, s2, op0, op1=None):
    if op1 is None:
        return lambda e: e.tensor_scalar(out=out, in0=in0, scalar1=s1, scalar2=None, op0=op0)
    return lambda e: e.tensor_scalar(out=out, in0=in0, scalar1=s1, scalar2=s2, op0=op0, op1=op1)


def CP(out, in_):
    return lambda e: e.tensor_copy(out=out, in_=in_)


def RECIP(out, in_):
    return lambda e: e.reciprocal(out=out, in_=in_)


def MEMSET(ap, v):
    return lambda e: e.memset(ap, v)


def RMAX(out, in_):
    return lambda e: e.reduce_max(out=out, in_=in_, axis=AX.X)


def DMA(out, in_):
    return lambda e: e.dma_start(out=out, in_=in_)


def RSUM(out, in_):
    return lambda e: e.reduce_sum(out=out, in_=in_, axis=AX.X)


def ISCAT(dram, idx_ap, src, bound):
    return lambda e: e.indirect_dma_start(out=dram, out_offset=bass.IndirectOffsetOnAxis(ap=idx_ap, axis=0), in_=src,
                                          in_offset=None, bounds_check=bound, oob_is_err=False)


def IGATH(dst, dram, idx_ap, bound):
    return lambda e: e.indirect_dma_start(out=dst, out_offset=None, in_=dram,
                                          in_offset=bass.IndirectOffsetOnAxis(ap=idx_ap, axis=0),
                                          bounds_check=bound, oob_is_err=False)


def DMAS(pairs):
    def f(e):
        return [e.dma_start(out=o, in_=i) for (o, i) in pairs]
    return f


def make_consts():
    c = {}
    c["ident_f"] = np.eye(128, dtype=np.float32)
    cb = np.zeros((128, 6, 128), np.float32)
    cb[:, 0, :] = np.eye(128)
    cb[:, 1, :] = 1.0
    cb[0:64, 2, 0:64] = 1.0
    cb[64:128, 2, 64:128] = 1.0
    for base in (0, 16):
        for i in range(8):
            a = 64 + base + i
            b = a + 8
            cb[b, 3, a] = -1.0
            cb[a, 3, b] = 1.0
    for hb in (0, 64):
        for base in (0, 32):
            for i in range(16):
                a = hb + base + i
                b = a + 16
                cb[b, 4, a] = -1.0
                cb[a, 4, b] = 1.0
    cb[:, 5, :] = np.triu(np.ones((128, 128), np.float32), 1)
    c["cb"] = cb
    c["ebase"] = np.ascontiguousarray(np.broadcast_to(np.tile(np.arange(8, dtype=np.float32) * 2048.0, 16)[None, :], (128, 128)))
    sel96 = np.zeros((32, 96), np.float32)
    for i in range(32):
        sel96[i, 64 + i] = 1.0
    c["sel96"] = sel96
    t = np.arange(T_LAT)
    row = (t // 64).astype(np.float64)
    col = (t % 64).astype(np.float64)
    tabs = np.zeros((128, 4, T_LAT), np.float32)
    tabs[:, 0, :] = 1.0
    tabs[:, 2, :] = 1.0
    fa = THETA ** (-np.arange(8, dtype=np.float64) / 8.0)
    fb = THETA ** (-np.arange(16, dtype=np.float64) / 16.0)
    fa32 = fa.astype(np.float32).astype(np.float64)
    fb32 = fb.astype(np.float32).astype(np.float64)
    for r in range(32):
        pos = row if r < 16 else col
        ang = (pos * fa32[r % 8]).astype(np.float32).astype(np.float64)
        tabs[64 + r, 0, :] = np.cos(ang)
        tabs[64 + r, 1, :] = np.sin(ang)
    for hb in (0, 64):
        for d in range(64):
            pos = row if d < 32 else col
            ang = (pos * fb32[d % 16]).astype(np.float32).astype(np.float64)
            tabs[hb + d, 2, :] = np.cos(ang)
            tabs[hb + d, 3, :] = np.sin(ang)
    c["tabs"] = tabs
    return c


LAYER_W = ["w_mod", "vecs", "w_in", "w_uq", "w_ukv", "mla_q_gain", "mla_k_gain", "gqa_q_gain",
           "gqa_k_gain", "w_out"]


def build_program(dbg=None, limit=None, dbg_fn=None, oplimit=None):
    nc = bass.Bass("TRN2", target_bir_lowering=False)

    def din(name, shape):
        return nc.dram_tensor(name, list(shape), F32, kind="ExternalInput").ap()

    x_d = din("x", [T_LAT, 1024])
    ctx_d = din("ctx", [T_CTX, 1024])
    cvec_d = din("cvec", [16, 128])
    identf_d = din("ident_f", [128, 128])
    cb_d = din("cb", [128, 6, 128])
    ebase_d = din("ebase", [128, 128])
    sel96_d = din("sel96", [32, 96])
    tabs_d = din("tabs", [128, 4, T_LAT])
    W = []
    for L in range(2):
        d = {}
        d["w_mod"] = din(f"l{L}_w_mod", [1024, 6144])
        d["vecs"] = din(f"l{L}_vecs", [67, 128])
        d["w_in"] = din(f"l{L}_w_in", [1024, 1184])
        d["w_uq"] = din(f"l{L}_w_uq", [256, 768])
        d["w_ukv"] = din(f"l{L}_w_ukv", [128, 1024])
        d["mla_q_gain"] = din(f"l{L}_mla_q_gain", [96, 1])
        d["mla_k_gain"] = din(f"l{L}_mla_k_gain", [96, 1])
        d["gqa_q_gain"] = din(f"l{L}_gqa_q_gain", [64, 1])
        d["gqa_k_gain"] = din(f"l{L}_gqa_k_gain", [64, 1])
        d["w_out"] = din(f"l{L}_w_out", [1024, 1024])
        if L == 0:
            d["wg"] = [din("l0_ffn_w_gate", [1024, FF])]
            d["wu"] = [din("l0_ffn_w_up", [1024, FF])]
            d["wd"] = [din("l0_ffn_w_down", [FF, 1024])]
        else:
            d["router"] = din("l1_router", [1024, 8])
            ne_ = 8 if (limit is None or limit > 12) else 1
            wg = din("l1_exp_w_gate", [ne_, 1024, FF])
            wu = din("l1_exp_w_up", [ne_, 1024, FF])
            wd = din("l1_exp_w_down", [ne_, FF, 1024])
            wg = [wg[min(e, ne_ - 1)] for e in range(8)]
            wu = [wu[min(e, ne_ - 1)] for e in range(8)]
            wd = [wd[min(e, ne_ - 1)] for e in range(8)]
            d["wg"] = wg
            d["wu"] = wu
            d["wd"] = wd
        W.append(d)
    out_d = nc.dram_tensor("out", [T_LAT, 1024], F32, kind="ExternalOutput").ap()
    xs = nc.dram_tensor("xs", [8, 128, T_ALL], F32, kind="Internal").ap()
    xs_v = xs.rearrange("k p t -> p k t")
    hs = nc.dram_tensor("hs", [8, 128, T_ALL], BF16, kind="Internal").ap()
    hs_v = hs.rearrange("k p t -> p k t")
    NSLOT = 8 * T_LAT
    Gd = nc.dram_tensor("Gd", [NSLOT, 1024], BF16, kind="Internal").ap()
    Yd = nc.dram_tensor("Yd", [NSLOT, 1024], F32, kind="Internal").ap()
    dbg_d = None
    if dbg is not None:
        dbg_d = nc.dram_tensor("dbg", [128, dbg], F32, kind="ExternalOutput").ap()

    P = Prog(nc)
    P.limit = limit
    P.oplimit = oplimit
    BLOCKS = [(0, 512), (512, 512), (1024, 512), (1536, 512), (2048, 256)]

    with ExitStack() as top:
        uid = [0]

        def sb(st, name, shape, dt):
            uid[0] += 1
            return st.enter_context(nc.sbuf_tensor(f"sb{uid[0]}_{name}", list(shape), dt))

        PS2 = [top.enter_context(nc.psum_tensor(f"ps{i}", [128, 1024], F32)) for i in range(4)]

        def bank(i):
            return PS2[i // 2][:, (i % 2) * 512:(i % 2) * 512 + 512]

        rr = [0]

        def nb():
            i = rr[0] % 8
            rr[0] += 1
            return i

        ident_f = sb(top, "ident_f", [128, 128], F32)
        cbt = sb(top, "cbt", [128, 6, 128], BF16)
        sel96 = sb(top, "sel96", [128, 96], BF16)
        epsc = sb(top, "epsc", [128, 1], F32)
        cols = sb(top, "cols", [128, 83], F32)
        modc = sb(top, "modc", [128, 48, 2], F32)
        A1 = sb(top, "A1", [128, 8, 2], F32)
        A2 = sb(top, "A2", [128, 8, 2], F32)
        gq = sb(top, "gq", [128, 4], F32)
        ident_b = cbt[:, 0, :]
        ones_b = cbt[:, 1, :]
        bonesB = cbt[:, 2, :]
        PA = cbt[:, 3, :]
        PB = cbt[:, 4, :]
        triU = cbt[:, 5, :]

        P.begin()
        P.dma("sp", DMA(ident_f[:], identf_d[:, :]), writes=["ident_f"], key="c0")
        P.dma("pool", DMA(cbt[:], cb_d[:, :, :]), writes=["cbt"], key="c1")
        P.dma("pool", DMA(sel96[0:32, :], sel96_d[:, :]), writes=["sel96"], key="c2")
        P.op("dve", MEMSET(epsc[:], EPS), writes=["epsc"])
        P.end()

        with ExitStack() as st:
            xin = [sb(st, f"xin{i}", [128, 1024], F32) for i in range(2)]
            xblk = sb(st, "xblk", [128, 8, 512], F32)
            P.begin()
            ti = 0
            for (c0, n) in BLOCKS:
                nt = n // 128
                for tt in range(nt):
                    tok0 = c0 + tt * 128
                    src = x_d[tok0:tok0 + 128, :] if tok0 < T_LAT else ctx_d[tok0 - T_LAT:tok0 - T_LAT + 128, :]
                    b_ = ti % 2
                    P.dma("sp", DMA(xin[b_][:], src), writes=[f"xin{b_}"], key=f"xin{b_}")
                    for k in range(8):
                        P.op("pe", TR(bank(k)[:, tt * 128:(tt + 1) * 128], xin[b_][:, k * 128:(k + 1) * 128], ident_f[:]),
                             reads=[f"xin{b_}", "ident_f"], writes=[f"B{k}"])
                    ti += 1
                for k in range(8):
                    eng = "act" if k % 2 == 0 else "dve"
                    fn = ACT(xblk[:, k, 0:n], bank(k)[:, 0:n], AF.Copy) if eng == "act" else CP(xblk[:, k, 0:n], bank(k)[:, 0:n])
                    P.op(eng, fn, reads=[f"B{k}"], writes=[f"xblk{k}"])
                P.dma("sp", DMA(xs_v[:, :, c0:c0 + n], xblk[:, :, 0:n]), reads=[f"xblk{k}" for k in range(8)], key="xo")
            P.end()

        for L in range(2):
            WL = W[L]
            last = (L == 1)
            with ExitStack() as st:
                stage = sb(st, "stage", [128, 128], F32)
                scb = sb(st, "scb", [128, 8, 2], BF16)
                wm = [sb(st, f"wm{i}", [128, 8, 1024], BF16) for i in range(2)]
                wmod_v = WL["w_mod"].rearrange("(k p) n -> p k n", p=128)
                P.begin()
                P.op("dve", MEMSET(stage[:], 0.0), writes=["stage"])
                P.dma("sp", DMA(stage[0:67, :], WL["vecs"][:, :]), reads=["stage"], writes=["stage"], key="st")
                P.dma("sp", DMA(stage[67:83, :], cvec_d[:, :]), writes=["stage"], key="st")
                P.dma("sp", DMAS([(gq[0:96, 0:1], WL["mla_q_gain"][:, :]), (gq[0:96, 1:2], WL["mla_k_gain"][:, :]),
                                  (gq[0:64, 2:3], WL["gqa_q_gain"][:, :]), (gq[64:128, 2:3], WL["gqa_q_gain"][:, :]),
                                  (gq[0:64, 3:4], WL["gqa_k_gain"][:, :]), (gq[64:128, 3:4], WL["gqa_k_gain"][:, :])]),
                      writes=["gq"], key="gq", n=6)
                b0 = nb()
                P.op("pe", TR(bank(b0)[:, 0:128], stage[:, :], ident_f[:, :]), reads=["stage", "ident_f"], writes=[f"B{b0}"])
                P.op("dve", CP(cols[:, :], bank(b0)[:, 0:83]), reads=[f"B{b0}"], writes=["cols"])
                P.op("act", ACT(scb[:, :, 0], cols[:, 67:75], AF.Silu), reads=["cols"], writes=["scb"])
                P.op("act", ACT(scb[:, :, 1], cols[:, 75:83], AF.Silu), reads=["scb", "cols"], writes=["scb"])
                bm = nb()
                psm = bank(bm)[:, 0:96].rearrange("p (m i) -> p m i", i=2)
                for sec in range(6):
                    b_ = sec % 2
                    P.dma("pool", DMAS([(wm[b_][:, k, :], wmod_v[:, k, sec * 1024:(sec + 1) * 1024]) for k in range(8)]),
                          writes=[f"wm{b_}"], key=f"wm{b_}", n=8)
                    for m in range(8):
                        for k in range(8):
                            P.op("pe", MM(psm[:, sec * 8 + m, :], wm[b_][:, k, m * 128:(m + 1) * 128], scb[:, k, :],
                                          start=(k == 0), stop=(k == 7)),
                                 reads=[f"wm{b_}", "scb"], writes=[f"B{bm}"])
                for i in range(2):
                    P.op("dve", TT(modc[:, :, i], psm[:, :, i], cols[:, 0:48], ALU.add), reads=[f"B{bm}", "cols"], writes=["modc"])
                for i in range(2):
                    P.op("dve", STT(A1[:, :, i], modc[:, 8:16, i], 1.0, cols[:, 48:56], ALU.add, ALU.mult),
                         reads=["modc", "cols"], writes=["A1"])
                    P.op("dve", STT(A2[:, :, i], modc[:, 32:40, i], 1.0, cols[:, 56:64], ALU.add, ALU.mult),
                         reads=["modc", "cols"], writes=["A2"])
                P.end()

            def Bcol(sec, k, i):
                return modc[:, sec * 8 + k, i:i + 1]

            def mk_sets(st, ns):
                sets = []
                for i_ in range(ns):
                    sets.append(dict(i=i_, sq=sb(st, f"sq{i_}", [128, 512], BF16), ms=sb(st, f"ms{i_}", [128, 512], F32),
                                     kn=sb(st, f"kn{i_}", [128, 512], BF16), t1=sb(st, f"t1{i_}", [128, 512], BF16),
                                     t2=sb(st, f"t2{i_}", [128, 512], BF16)))
                return sets

            job = [0]

            def next_set(SR):
                S = SR[job[0] % len(SR)]
                job[0] += 1
                return S

            def rstd_from(srcs, rows, n, inv_sqrt_d, ones_ap, sqs, ms):
                bi = nb()
                mst, msk = ms
                for i, (ap, rk) in enumerate(srcs):
                    s_, sk = sqs[i % len(sqs)]
                    P.op("act", ACT(s_[0:rows, 0:n], ap, AF.Square, scale=inv_sqrt_d), reads=rk, writes=[sk])
                    P.op("pe", MM(bank(bi)[0:rows, 0:n], ones_ap, s_[0:rows, 0:n], start=(i == 0), stop=(i == len(srcs) - 1)),
                         reads=[sk, "cbt"], writes=[f"B{bi}"])
                P.op("act", ACT(mst[0:rows, 0:n], bank(bi)[0:rows, 0:n], AF.Ln, bias=epsc[0:rows, 0:1], scale=1.0),
                     reads=[f"B{bi}", "epsc"], writes=[msk])
                P.op("act", ACT(mst[0:rows, 0:n], mst[0:rows, 0:n], AF.Exp, scale=-0.5), reads=[msk], writes=[msk])

            def norm_mod(xj, n, Acol, sec, i, hj, SR, xk="xj", hk="hj"):
                rstd_from([(xj[:, k, 0:n], [xk]) for k in range(8)], 128, n, 1.0 / 32.0, ones_b,
                          [(SR[0]["sq"], "sq0"), (SR[1]["sq"], "sq1")], (SR[2]["ms"], "ms2"))
                for k in range(8):
                    t_, tk = SR[k % 2]["ms"], f"ms{k % 2}"
                    P.op("dve", STT(t_[:, 0:n], xj[:, k, 0:n], Acol[:, k, i:i + 1], SR[2]["ms"][:, 0:n], ALU.mult, ALU.mult),
                         reads=[xk, "ms2", "A1", "A2"], writes=[tk])
                    if k % 2 == 0:
                        P.op("dve", TS(hj[:, k, 0:n], t_[:, 0:n], Bcol(sec, k, i), None, ALU.add),
                             reads=[tk, "modc"], writes=[f"{hk}{k}"])
                    else:
                        P.op("act", ACT(hj[:, k, 0:n], t_[:, 0:n], AF.Identity, bias=Bcol(sec, k, i), scale=1.0),
                             reads=[tk, "modc"], writes=[f"{hk}{k}"])

            with ExitStack() as att:
                tabs = sb(att, "tabs", [128, 4, T_LAT], BF16)
                KaT = sb(att, "KaT", [128, 8, T_ALL], BF16)
                KbT = sb(att, "KbT", [128, 2, T_ALL], BF16)
                Vst = sb(att, "Vst", [128, 18, 1152], BF16)
                cosA, sinA, cosB, sinB = tabs[:, 0, :], tabs[:, 1, :], tabs[:, 2, :], tabs[:, 3, :]

                def head_norm_rope(pre_bank, rows, n, inv_sqrt_d, ones_ap, gcol, perm, cos_t, sin_t, c0, dst, rope,
                                   S, dstkey):
                    si = S["i"]
                    rstdt, knt, t1t, t2t = S["ms"], S["kn"], S["t1"], S["t2"]
                    pre = bank(pre_bank)[0:rows, 0:n]
                    rstd_from([(pre, [f"B{pre_bank}"])], rows, n, inv_sqrt_d, ones_ap, [(S["sq"], f"sq{si}")], (rstdt, f"ms{si}"))
                    if not rope:
                        P.op("dve", STT(dst, pre, gcol, rstdt[0:rows, 0:n], ALU.mult, ALU.mult),
                             reads=[f"B{pre_bank}", f"ms{si}", "gq"], writes=[dstkey])
                        return
                    P.op("dve", STT(knt[0:rows, 0:n], pre, gcol, rstdt[0:rows, 0:n], ALU.mult, ALU.mult),
                         reads=[f"B{pre_bank}", f"ms{si}", "gq"], writes=[f"kn{si}"])
                    br = pre_bank
                    P.op("pe", MM(bank(br)[0:rows, 0:n], perm[0:rows, 0:rows], knt[0:rows, 0:n]),
                         reads=[f"kn{si}", "cbt"], writes=[f"B{br}"])
                    P.op("pool", TT(t1t[0:rows, 0:n], knt[0:rows, 0:n], cos_t[0:rows, c0:c0 + n], ALU.mult),
                         reads=[f"kn{si}", "tabs"], writes=[f"t1{si}"])
                    P.op("dve", TT(t2t[0:rows, 0:n], bank(br)[0:rows, 0:n], sin_t[0:rows, c0:c0 + n], ALU.mult),
                         reads=[f"B{br}", "tabs"], writes=[f"t2{si}"])
                    P.op("dve", TT(dst, t1t[0:rows, 0:n], t2t[0:rows, 0:n], ALU.add),
                         reads=[f"t1{si}", f"t2{si}"], writes=[dstkey])

                def head_job(SRl, pre_mm, pre_reads, rows, n, inv_sqrt_d, ones_ap, gcol, perm, cos_t, sin_t, c0, dst, rope, dstkey):
                    stt = {}

                    def A():
                        S = next_set(SRl)
                        bp = nb()
                        stt["S"], stt["bp"] = S, bp
                        pre_mm(bp)
                        P.op("act", ACT(S["sq"][0:rows, 0:n], bank(bp)[0:rows, 0:n], AF.Square, scale=inv_sqrt_d),
                             reads=[f"B{bp}"], writes=[f"sq{S['i']}"])

                    def B():
                        S, bp = stt["S"], stt["bp"]
                        si = S["i"]
                        pre = bank(bp)[0:rows, 0:n]
                        ms = S["ms"][0:rows, 0:n]
                        bi = nb()
                        P.op("pe", MM(bank(bi)[0:rows, 0:n], ones_ap, S["sq"][0:rows, 0:n]), reads=[f"sq{si}", "cbt"], writes=[f"B{bi}"])
                        P.op("act", ACT(ms, bank(bi)[0:rows, 0:n], AF.Ln, bias=epsc[0:rows, 0:1], scale=1.0),
                             reads=[f"B{bi}", "epsc"], writes=[f"ms{si}"])
                        P.op("act", ACT(ms, ms, AF.Exp, scale=-0.5), reads=[f"ms{si}"], writes=[f"ms{si}"])
                        if not rope:
                            P.op("dve", STT(dst, pre, gcol, ms, ALU.mult, ALU.mult), reads=[f"B{bp}", f"ms{si}", "gq"], writes=[dstkey])
                        else:
                            P.op("dve", STT(S["kn"][0:rows, 0:n], pre, gcol, ms, ALU.mult, ALU.mult),
                                 reads=[f"B{bp}", f"ms{si}", "gq"], writes=[f"kn{si}"])

                    def C():
                        if not rope:
                            return
                        S, bp = stt["S"], stt["bp"]
                        si = S["i"]
                        knt, t1t, t2t = S["kn"], S["t1"], S["t2"]
                        P.op("pe", MM(bank(bp)[0:rows, 0:n], perm[0:rows, 0:rows], knt[0:rows, 0:n]),
                             reads=[f"kn{si}", "cbt"], writes=[f"B{bp}"])
                        P.op("pool", TT(t1t[0:rows, 0:n], knt[0:rows, 0:n], cos_t[0:rows, c0:c0 + n], ALU.mult),
                             reads=[f"kn{si}", "tabs"], writes=[f"t1{si}"])
                        P.op("dve", TT(t2t[0:rows, 0:n], bank(bp)[0:rows, 0:n], sin_t[0:rows, c0:c0 + n], ALU.mult),
                             reads=[f"B{bp}", "tabs"], writes=[f"t2{si}"])
                        P.op("dve", TT(dst, t1t[0:rows, 0:n], t2t[0:rows, 0:n], ALU.add),
                             reads=[f"t1{si}", f"t2{si}"], writes=[dstkey])
                    return [A, B, C]

                def run_pipeline(jobs):
                    ns = 3
                    for t in range(len(jobs) + ns - 1):
                        for s in range(ns):
                            j = t - s
                            if 0 <= j < len(jobs):
                                jobs[j][s]()

                with ExitStack() as st:
                    w_in = sb(st, "w_in", [128, 8, 160], BF16)
                    WKB = sb(st, "WKB", [128, 8, 2, 128], BF16)
                    WVB = sb(st, "WVB", [128, 8, 128], BF16)
                    WN = sb(st, "WN", [128, 8, 96], BF16)
                    WV = sb(st, "WV", [128, 8, 64], BF16)
                    xjs = [sb(st, f"xj{i}", [128, 8, 512], F32) for i in range(2)]
                    hjs = [sb(st, f"hj{i}", [128, 8, 512], BF16) for i in range(2)]
                    ckvn2 = [sb(st, f"ckvn{i}", [128, 512], BF16) for i in range(2)]
                    krope2 = [sb(st, f"krope{i}", [128, 512], BF16) for i in range(2)]
                    SR = mk_sets(st, 4)
                    win_v = WL["w_in"].rearrange("(k p) n -> p k n", p=128)
                    wukv_v = WL["w_ukv"].rearrange("p (h c) -> p h c", c=128)
                    P.begin()
                    P.dma("pool", DMAS([(w_in[:, k, 0:160], win_v[:, k, 256:416]) for k in range(8)]), writes=["w_in"], key="w_in", n=8)
                    P.op("dve", MEMSET(WN[:], 0.0), writes=["WN"])
                    P.dma("pool", DMA(WN[:, :, 0:64], wukv_v[:, :, 0:64]), reads=["WN"], writes=["WN"], key="WN")
                    P.dma("pool", DMA(WV[:, :, :], wukv_v[:, :, 64:128]), writes=["WV"], key="WV")
                    P.dma("pool", DMA(tabs[:], tabs_d[:, :, :]), writes=["tabs"], key="tabs")
                    P.dma("pool", DMAS([(WKB[:, k, g, h_ * 64:(h_ + 1) * 64], win_v[:, k, 928 + g * 64:928 + (g + 1) * 64])
                                        for k in range(8) for g in range(2) for h_ in range(2)]), writes=["WKB"], key="WKB", n=32)
                    P.dma("pool", DMAS([(WVB[:, k, :], win_v[:, k, 1056:1184]) for k in range(8)]), writes=["WVB"], key="WVB", n=8)
                    P.op("dve", MEMSET(Vst[:, :, :].rearrange("p k (a s c) -> p k a s c", s=3, c=64)[:, :, :, 1, :], 1.0), writes=["Vst"])
                    if L == 0:
                        zt = sb(st, "zt", [128, 4, 1024], BF16)
                        P.op("pool", MEMSET(zt[:], 0.0), writes=["zt"])
                    def kv_block(bj_, c0, n):
                        i_mod = 0 if c0 < T_LAT else 1
                        rope = c0 < T_LAT
                        p2 = bj_ % 2
                        xj = xjs[p2]
                        hj = hjs[p2]
                        ckvn = ckvn2[p2]
                        krope = krope2[p2]
                        xk_ = f"xj{p2}"
                        hk_ = f"hj{p2}_"
                        ck_ = f"ckvn{p2}"
                        kr_ = f"krope{p2}"

                        def preA():
                            P.dma("sp", DMA(xj[:, :, 0:n], xs_v[:, :, c0:c0 + n]), writes=[xk_], key=xk_)
                            if L == 0 and bj_ >= 1:
                                for zi in range((bj_ - 1) * 8, bj_ * 8):
                                    P.dma("sp", DMA(Gd[zi * 512:(zi + 1) * 512, :].rearrange("(s p) d -> p s d", p=128), zt[:, :, :]),
                                          reads=["zt"], key="zf")
                            norm_mod(xj, n, A1, 0, i_mod, hj, SR, xk=xk_, hk=hk_)
                            P.dma("sp", DMA(hs_v[:, :, c0:c0 + n], hj[:, :, 0:n]), reads=[f"{hk_}{k}" for k in range(8)], key="hso")

                        def preB():
                            bc = nb()
                            for k in range(8):
                                P.op("pe", MM(bank(bc)[:, 0:n], w_in[:, k, 0:128], hj[:, k, 0:n], start=(k == 0), stop=(k == 7)),
                                     reads=["w_in", f"{hk_}{k}"], writes=[f"B{bc}"])
                            S_ = next_set(SR)
                            rstd_from([(bank(bc)[:, 0:n], [f"B{bc}"])], 128, n, 128 ** -0.5, ones_b, [(S_["sq"], f"sq{S_['i']}")], (S_["ms"], f"ms{S_['i']}"))
                            P.op("dve", STT(ckvn[:, 0:n], bank(bc)[:, 0:n], cols[:, 66:67], S_["ms"][:, 0:n], ALU.mult, ALU.mult),
                                 reads=[f"B{bc}", f"ms{S_['i']}", "cols"], writes=[ck_])
                            bk = nb()
                            for k in range(8):
                                P.op("pe", MM(bank(bk)[0:32, 0:n], w_in[:, k, 128:160], hj[:, k, 0:n], start=(k == 0), stop=(k == 7)),
                                     reads=["w_in", f"{hk_}{k}"], writes=[f"B{bk}"])
                            P.op("act", ACT(krope[0:32, 0:n], bank(bk)[0:32, 0:n], AF.Copy), reads=[f"B{bk}"], writes=[kr_])

                        def main():
                            jobs = []
                            for h in range(8):
                                def pre_mla(bp, h=h):
                                    P.op("pe", MM(bank(bp)[0:96, 0:n], WN[:, h, :], ckvn[:, 0:n], start=True, stop=False),
                                         reads=["WN", ck_], writes=[f"B{bp}"])
                                    P.op("pe", MM(bank(bp)[0:96, 0:n], sel96[0:32, :], krope[0:32, 0:n], start=False, stop=True),
                                         reads=["sel96", kr_], writes=[f"B{bp}"])
                                jobs.append(head_job(SR, pre_mla, None, 96, n, 96 ** -0.5, ones_b[0:96, 0:96], gq[0:96, 1:2], PA, cosA, sinA, c0,
                                                     KaT[0:96, h, c0:c0 + n], rope, "KaT"))
                            for g in range(2):
                                def pre_gqa(bp, g=g):
                                    for k in range(8):
                                        P.op("pe", MM(bank(bp)[:, 0:n], WKB[:, k, g, :], hj[:, k, 0:n], start=(k == 0), stop=(k == 7)),
                                             reads=["WKB", f"{hk_}{k}"], writes=[f"B{bp}"])
                                jobs.append(head_job(SR, pre_gqa, None, 128, n, 0.125, bonesB, gq[:, 3:4], PB, cosB, sinB, c0,
                                                     KbT[:, g, c0:c0 + n], rope, "KbT"))
                            run_pipeline(jobs)

                        def vals():
                            for tt in range(n // 128):
                                kc = (c0 + tt * 128) // 128
                                bv = nb()
                                P.op("pe", MM(bank(bv)[:, 0:512], ckvn[:, tt * 128:(tt + 1) * 128], WV[:, :, :].rearrange("p h c -> p (h c)"),
                                              start=True, stop=True), reads=[ck_, "WV"], writes=[f"B{bv}"])
                                src = bank(bv)[:, 0:512].rearrange("p (a s c) -> p a s c", s=2, c=64)
                                dst = Vst[:, kc, 0:768].rearrange("p (a s c) -> p a s c", s=3, c=64)[:, :, 0:3:2, :]
                                P.op("act", ACT(dst, src, AF.Copy), reads=[f"B{bv}"], writes=["Vst"])
                                bw = nb()
                                for k in range(8):
                                    P.op("pe", MM(bank(bw)[:, 0:128], hj[:, k, tt * 128:(tt + 1) * 128], WVB[:, k, :], start=(k == 0), stop=(k == 7)),
                                         reads=["WVB", f"{hk_}{k}"], writes=[f"B{bw}"])
                                srcb = bank(bw)[:, 0:128].rearrange("p (g c) -> p g c", c=64)
                                for s_ in (0, 2):
                                    dstb = Vst[:, kc, 768:1152].rearrange("p (g s c) -> p g s c", s=3, c=64)[:, :, s_, :]
                                    P.op("dve", CP(dstb, srcb), reads=[f"B{bw}"], writes=["Vst"])
                        return preA, preB, main, vals

                    kvb = [kv_block(bj_, c0, n) for bj_, (c0, n) in enumerate(BLOCKS)]
                    kvb[0][0]()
                    kvb[0][1]()
                    for bj_ in range(len(BLOCKS)):
                        if bj_ + 1 < len(BLOCKS):
                            kvb[bj_ + 1][0]()
                        kvb[bj_][2]()
                        if bj_ + 1 < len(BLOCKS):
                            kvb[bj_ + 1][1]()
                        kvb[bj_][3]()
                    P.end()

                with ExitStack() as st:
                    wq = sb(st, "wq", [128, 8, 768], BF16)
                    wuq = sb(st, "wuq", [128, 2, 768], BF16)
                    wout = sb(st, "wout", [128, 8, 1024], BF16)
                    xj = sb(st, "xj", [128, 8, 512], F32)
                    hjs2 = [sb(st, f"hjq{i}", [128, 8, 512], BF16) for i in range(2)]
                    QaT = sb(st, "QaT", [128, 8, 512], BF16)
                    QbT = sb(st, "QbT", [128, 4, 512], BF16)
                    cqn = sb(st, "cqn", [128, 2, 512], BF16)
                    PT = [sb(st, f"PT{i}", [128, 2, 512], BF16) for i in range(3)]
                    SR = mk_sets(st, 3)
                    rden = [SR[0]["ms"], SR[1]["ms"]]
                    win_v = WL["w_in"].rearrange("(k p) n -> p k n", p=128)
                    wuq_v = WL["w_uq"].rearrange("(k p) n -> p k n", p=128)
                    wout_v = WL["w_out"].rearrange("(k p) n -> p k n", p=128)
                    P.begin()
                    P.dma("pool", DMAS([(wq[:, k, 0:256], win_v[:, k, 0:256]) for k in range(8)]), writes=["wq"], key="wq", n=8)
                    P.dma("pool", DMAS([(wq[:, k, 256:768], win_v[:, k, 416:928]) for k in range(8)]), reads=["wq"], writes=["wq"], key="wq", n=8)
                    P.dma("pool", DMAS([(wuq[:, k, :], wuq_v[:, k, :]) for k in range(2)]), writes=["wuq"], key="wuq", n=2)
                    P.dma("pool", DMAS([(wout[:, k, :], wout_v[:, k, :]) for k in range(8)]), writes=["wout"], key="wout", n=8)
                    qblocks = BLOCKS[:4] if last else BLOCKS

                    def load_h(jb_):
                        c0_, n_ = qblocks[jb_]
                        p_ = jb_ % 2
                        P.dma("sp", DMA(hjs2[p_][:, :, 0:n_], hs_v[:, :, c0_:c0_ + n_]), writes=[f"hj{p_}_{k}" for k in range(8)], key=f"hjl{p_}")

                    load_h(0)
                    for jb, (c0, n) in enumerate(qblocks):
                        lat = c0 < T_LAT
                        i_mod = 0 if lat else 1
                        hj = hjs2[jb % 2]
                        hq = f"hj{jb % 2}_"
                        P.dma("sp", DMA(xj[:, :, 0:n], xs_v[:, :, c0:c0 + n]), writes=["xj"], key="xj")
                        bq = [nb(), nb()]
                        for c_ in range(2):
                            for k in range(8):
                                P.op("pe", MM(bank(bq[c_])[:, 0:n], wq[:, k, c_ * 128:(c_ + 1) * 128], hj[:, k, 0:n], start=(k == 0), stop=(k == 7)),
                                     reads=["wq", f"{hq}{k}"], writes=[f"B{bq[c_]}"])
                        S_ = next_set(SR)
                        S2_ = next_set(SR)
                        rstd_from([(bank(bq[c_])[:, 0:n], [f"B{bq[c_]}"]) for c_ in range(2)], 128, n, 1.0 / 16.0, ones_b,
                                  [(S_["sq"], f"sq{S_['i']}"), (S2_["sq"], f"sq{S2_['i']}")], (S_["ms"], f"ms{S_['i']}"))
                        for c_ in range(2):
                            P.op("dve", STT(cqn[:, c_, 0:n], bank(bq[c_])[:, 0:n], cols[:, 64 + c_:65 + c_], S_["ms"][:, 0:n], ALU.mult, ALU.mult),
                                 reads=[f"B{bq[c_]}", f"ms{S_['i']}", "cols"], writes=["cqn"])
                        jobs = []
                        for h in range(8):
                            def pre_q(bp, h=h, n=n):
                                for c_ in range(2):
                                    P.op("pe", MM(bank(bp)[0:96, 0:n], wuq[:, c_, h * 96:(h + 1) * 96], cqn[:, c_, 0:n], start=(c_ == 0), stop=(c_ == 1)),
                                         reads=["wuq", "cqn"], writes=[f"B{bp}"])
                            jobs.append(head_job(SR, pre_q, None, 96, n, 96 ** -0.5, ones_b[0:96, 0:96], gq[0:96, 0:1], PA, cosA, sinA, c0,
                                                 QaT[0:96, h, 0:n], lat, f"QaT{h}"))
                        for m in range(4):
                            def pre_qb(bp, m=m, n=n):
                                for k in range(8):
                                    P.op("pe", MM(bank(bp)[:, 0:n], wq[:, k, 256 + m * 128:256 + (m + 1) * 128], hj[:, k, 0:n], start=(k == 0), stop=(k == 7)),
                                         reads=["wq", f"{hq}{k}"], writes=[f"B{bp}"])
                            jobs.append(head_job(SR, pre_qb, None, 128, n, 0.125, bonesB, gq[:, 2:3], PB, cosB, sinB, c0,
                                                 QbT[:, m, 0:n], lat, f"QbT{m}"))
                        run_pipeline(jobs)
                        if jb + 1 < len(qblocks):
                            load_h(jb + 1)
                        kchunks = list(range(18)) if lat else [16, 17]
                        items = []
                        for hh in range(16):
                            for pi in range(0, len(kchunks), 2):
                                items.append((hh, kchunks[pi:pi + 2], pi == 0, pi + 2 >= len(kchunks)))

                        def head_ops(hh):
                            if hh < 8:
                                return (lambda kc: KaT[0:96, hh, kc * 128:(kc + 1) * 128], QaT[0:96, hh, 0:n], f"QaT{hh}", "KaT",
                                        96 ** -0.5, (hh // 2) * 192 + (hh % 2) * 64, hh // 2, hh % 2)
                            q = hh - 8
                            kv = q // 4
                            r0 = (q % 2) * 64
                            return (lambda kc: KbT[r0:r0 + 64, kv, kc * 128:(kc + 1) * 128], QbT[r0:r0 + 64, q // 2, 0:n], f"QbT{q // 2}", "KbT",
                                    0.125, 768 + kv * 192 + (q % 2) * 64, 4 + q // 2, q % 2)

                        SB = [(0, 1), (2, 3), (4, 5)]
                        OB = [6, 7]
                        pend = []
                        for it, (hh, kcs, first, lastp) in enumerate(items):
                            kfn, qap, qkey, kkey, scl, voff, ochunk, par = head_ops(hh)
                            sp_ = it % 3
                            ps2 = PS2[sp_]
                            for i_, kc in enumerate(kcs):
                                P.op("pe", MM(ps2[:, i_ * 512:i_ * 512 + n], kfn(kc), qap, start=True, stop=True),
                                     reads=[kkey, qkey], writes=[f"B{SB[sp_][i_]}"])
                            if len(pend) >= 2:
                                pend.pop(0)()
                            ptv = PT[sp_]
                            nk = len(kcs)
                            P.op("act", ACT(ptv[:, 0:nk, 0:n], ps2[:, 0:nk * 512].rearrange("p (a c) -> p a c", c=512)[:, :, 0:n], AF.Exp, scale=scl),
                                 reads=[f"B{SB[sp_][i_]}" for i_ in range(nk)], writes=[f"PT{sp_}"])

                            def mk(hh=hh, kcs=kcs, first=first, lastp=lastp, sp_=sp_, voff=voff, ochunk=ochunk, par=par):
                                def run():
                                    ob = OB[hh % 2]
                                    for i_, kc in enumerate(kcs):
                                        P.op("pe", MM(bank(ob)[:, 0:n], Vst[:, kc, voff:voff + 128], PT[sp_][:, i_, 0:n],
                                                      start=(first and i_ == 0), stop=(lastp and i_ == len(kcs) - 1)),
                                             reads=["Vst", f"PT{sp_}"], writes=[f"B{ob}"])
                                    if lastp:
                                        o0 = par * 64
                                        d0 = 64 - o0
                                        rd = rden[hh % 2]
                                        P.op("dve", RECIP(rd[o0:o0 + 64, 0:n], bank(ob)[d0:d0 + 64, 0:n]), reads=[f"B{ob}"], writes=[f"ms{hh % 2}"])
                                        P.op("dve", TT(hj[o0:o0 + 64, ochunk, 0:n], bank(ob)[o0:o0 + 64, 0:n], rd[o0:o0 + 64, 0:n], ALU.mult),
                                             reads=[f"B{ob}", f"ms{hh % 2}"], writes=[f"{hq}{ochunk}"])
                                return run
                            pend.append(mk())
                        while pend:
                            pend.pop(0)()
                        rr[0] = 0
                        for m in range(8):
                            bo = m % 6
                            for c_ in range(8):
                                P.op("pe", MM(bank(bo)[:, 0:n], wout[:, c_, m * 128:(m + 1) * 128], hj[:, c_, 0:n], start=(c_ == 0), stop=(c_ == 7)),
                                     reads=["wout", f"{hq}{c_}"], writes=[f"B{bo}"])
                            P.op("dve", STT(xj[:, m, 0:n], bank(bo)[:, 0:n], Bcol(2, m, i_mod), xj[:, m, 0:n], ALU.mult, ALU.add),
                                 reads=[f"B{bo}", "xj", "modc"], writes=["xj"])
                        P.dma("sp", DMA(xs_v[:, :, c0:c0 + n], xj[:, :, 0:n]), reads=["xj"], key="xo")
                    P.end()

            if last:
                with ExitStack() as ffn:
                    ridx = sb(ffn, "ridx", [128, 2, 16], I32)
                    wts = sb(ffn, "wts", [128, 2, 16], F32)
                    flags = sb(ffn, "flags", [128, 48], I32)
                    P.flags_ap = flags
                    with ExitStack() as st:
                        SR = mk_sets(st, 4)
                        xT = sb(st, "xT", [128, 8, T_LAT], F32)
                        h2T = sb(st, "h2T", [128, 8, T_LAT], BF16)
                        hf = sb(st, "hf", [128, 8, 512], F32)
                        rt = sb(st, "rt", [128, 8, 8], F32)
                        Lg = sb(st, "Lg", [128, 16, 8], F32)
                        L2 = sb(st, "L2", [128, 16, 8], F32)
                        mk1 = sb(st, "mk1", [128, 16, 8], F32)
                        mk2 = sb(st, "mk2", [128, 16, 8], F32)
                        sm = sb(st, "sm", [128, 4, 16], F32)
                        maskb = sb(st, "maskb", [128, 16, 8], BF16)
                        tot_s = sb(st, "tot_s", [128, 16, 8], F32)
                        cum = sb(st, "cum", [128, 17, 8], F32)
                        val = sb(st, "val", [128, 16, 8], F32)
                        tmp = sb(st, "tmp", [128, 16, 8], F32)
                        rf = sb(st, "rf", [128, 2, 16], F32)
                        flagsf = sb(st, "flagsf", [128, 8, 6], F32)
                        ebase = sb(st, "ebase", [128, 16, 8], F32)
                        htok = [sb(st, f"htok{i}", [128, 1024], BF16) for i in range(2)]
                        fblocks = BLOCKS[:4]
                        P.begin()
                        for bi_, (c0, n) in enumerate(fblocks):
                            P.dma("sp", DMA(xT[:, :, c0:c0 + n], xs_v[:, :, c0:c0 + n]), writes=[f"xT{bi_}"], key=f"xT{bi_}")
                        P.dma("sp", DMA(rt[:], WL["router"].rearrange("(k p) e -> p k e", p=128)), writes=["rt"], key="rt")
                        P.dma("sp", DMA(ebase[:].rearrange("p a b -> p (a b)"), ebase_d[:, :]), writes=["ebase"], key="eb")
                        for bi_, (c0, n) in enumerate(fblocks):
                            r3 = bi_ % 2
                            rstd_from([(xT[:, k, c0:c0 + n], [f"xT{bi_}"]) for k in range(8)], 128, n, 1.0 / 32.0, ones_b,
                                      [(SR[0]["sq"], "sq0"), (SR[1]["sq"], "sq1")], (SR[2 + r3]["ms"], f"ms{2 + r3}"))
                            rst_ = SR[2 + r3]["ms"]
                            for k in range(8):
                                t_, tk = SR[k % 2]["ms"], f"ms{k % 2}"
                                P.op("dve", STT(t_[:, 0:n], xT[:, k, c0:c0 + n], A2[:, k, 0:1], rst_[:, 0:n], ALU.mult, ALU.mult),
                                     reads=[f"xT{bi_}", f"ms{2 + r3}", "A2"], writes=[tk])
                                P.op("act", ACT(hf[:, k, 0:n], t_[:, 0:n], AF.Identity, bias=Bcol(3, k, 0), scale=1.0),
                                     reads=[tk, "modc"], writes=[f"hf{k}"])
                                P.op("dve", CP(h2T[:, k, c0:c0 + n], hf[:, k, 0:n]), reads=[f"hf{k}"], writes=[f"h2T{k}"])
                            for tt in range(n // 128):
                                tg = (c0 // 128) + tt
                                bl = nb()
                                for k in range(8):
                                    P.op("pe", MM(bank(bl)[:, 0:8], hf[:, k, tt * 128:(tt + 1) * 128], rt[:, k, :], start=(k == 0), stop=(k == 7)),
                                         reads=[f"hf{k}", "rt"], writes=[f"B{bl}"])
                                P.op("dve", CP(Lg[:, tg, :], bank(bl)[:, 0:8]), reads=[f"B{bl}"], writes=["Lg"])
                        m1, m2, df, e2 = (sm[:, i, :] for i in range(4))
                        g1, g2 = wts[:, 0, :], wts[:, 1, :]
                        P.op("dve", RMAX(m1, Lg[:, :, :]), reads=["Lg"], writes=["m1"])
                        for e_ in range(8):
                            P.op("dve", TT(mk1[:, :, e_], Lg[:, :, e_], m1, ALU.is_equal), reads=["Lg", "m1"], writes=["mk1"])
                        P.op("dve", STT(L2[:, :, :], mk1[:, :, :], -1e30, Lg[:, :, :], ALU.mult, ALU.add), reads=["mk1", "Lg"], writes=["L2"])
                        P.op("dve", RMAX(m2, L2[:, :, :]), reads=["L2"], writes=["m2"])
                        for e_ in range(8):
                            P.op("dve", TT(mk2[:, :, e_], L2[:, :, e_], m2, ALU.is_equal), reads=["L2", "m2"], writes=["mk2"])
                        P.op("dve", TT(df, m2, m1, ALU.subtract), reads=["m1", "m2"], writes=["df"])
                        P.op("act", ACT(e2, df, AF.Exp), reads=["df"], writes=["e2"])
                        P.op("dve", TS(g1, e2, 1.0, None, ALU.add), reads=["e2"], writes=["wts"])
                        P.op("dve", RECIP(g1, g1), reads=["wts"], writes=["wts"])
                        P.op("dve", TT(g2, e2, g1, ALU.mult), reads=["e2", "wts"], writes=["wts"])
                        P.op("dve", TT(maskb[:, :, :], mk1[:, :, :], mk2[:, :, :], ALU.add), reads=["mk1", "mk2"], writes=["maskb"])
                        mflat = maskb[:, :, :].rearrange("p a b -> p (a b)")
                        bw_, bt_ = nb(), nb()
                        P.op("pe", MM(bank(bw_)[:, 0:128], triU, mflat), reads=["cbt", "maskb"], writes=[f"B{bw_}"])
                        P.op("pe", MM(bank(bt_)[:, 0:128], ones_b, mflat), reads=["cbt", "maskb"], writes=[f"B{bt_}"])
                        P.op("dve", CP(tot_s[:, :, :].rearrange("p a b -> p (a b)"), bank(bt_)[:, 0:128]), reads=[f"B{bt_}"], writes=["tot_s"])
                        P.op("dve", MEMSET(cum[:, 0, :], 0.0), writes=["cum"])
                        for tg in range(16):
                            P.op("dve", TT(cum[:, tg + 1, :], cum[:, tg, :], tot_s[:, tg, :], ALU.add), reads=["cum", "tot_s"], writes=["cum"])
                        P.op("dve", TT(val[:, :, :].rearrange("p a b -> p (a b)"), bank(bw_)[:, 0:128],
                                       cum[:, 0:16, :].rearrange("p a b -> p (a b)"), ALU.add), reads=[f"B{bw_}", "cum"], writes=["val"])
                        P.op("dve", TT(val[:, :, :], val[:, :, :], ebase[:, :, :], ALU.add), reads=["val", "ebase"], writes=["val"])
                        for j, mk_ in enumerate((mk1, mk2)):
                            P.op("dve", TT(tmp[:, :, :], mk_[:, :, :], val[:, :, :], ALU.mult), reads=["val", f"mk{j + 1}"], writes=["tmp"])
                            P.op("dve", RSUM(rf[:, j, :], tmp[:, :, :]), reads=["tmp"], writes=["rf"])
                        P.op("dve", CP(ridx[:, :, :], rf[:, :, :]), reads=["rf"], writes=["ridx"])
                        for b_ in range(6):
                            P.op("dve", TS(flagsf[:, :, b_], cum[:, 16, :], 512.0 + 256.0 * b_, None, ALU.is_gt), reads=["cum"], writes=["flagsf"])
                        P.op("dve", CP(flags[:, :], flagsf[:, :, :].rearrange("p a b -> p (a b)")), reads=["flagsf"], writes=["flags"])
                        for tg in range(16):
                            pz = PS2[tg % 2]
                            bk0 = 2 * (tg % 2)
                            for k in range(8):
                                P.op("pe", MM(pz[:, k * 128:(k + 1) * 128], h2T[:, k, tg * 128:(tg + 1) * 128], ident_b),
                                     reads=[f"h2T{k}", "cbt"], writes=[f"B{bk0 + k // 4}"])
                            ht = htok[tg % 2]
                            P.op("act", ACT(ht[:, 0:512], pz[:, 0:512], AF.Copy), reads=[f"B{bk0}"], writes=[f"htok{tg % 2}a"])
                            P.op("dve", CP(ht[:, 512:1024], pz[:, 512:1024]), reads=[f"B{bk0 + 1}"], writes=[f"htok{tg % 2}b"])
                            for j in range(2):
                                P.dma("pool", ISCAT(Gd[:, :], ridx[:, j, tg:tg + 1], ht[:, :], P.bc),
                                      reads=[f"htok{tg % 2}a", f"htok{tg % 2}b", "ridx"], key=f"sc{tg % 2}")
                        P.end()

                    GS = 4
                    groups = []
                    f0 = 0
                    while f0 < NFC:
                        gs = min(GS, NFC - f0)
                        groups.append((f0, gs))
                        f0 += gs
                    NRING = 4
                    with ExitStack() as st:
                        wgt = [sb(st, f"wgt{i}", [128, 8, GS * 128], BF16) for i in range(2)]
                        wut = [sb(st, f"wut{i}", [128, 8, GS * 128], BF16) for i in range(2)]
                        wdt = [sb(st, f"wdt{i}", [128, GS, 1024], BF16) for i in range(2)]
                        sgt = [sb(st, f"sgt{i}", [128, 512], F32) for i in range(2)]
                        actt = [sb(st, f"actt{i}", [128, GS, 512], BF16) for i in range(2)]
                        hT0 = [sb(st, f"hT0_{i}", [128, 8, 512], BF16) for i in range(2)]
                        hTx = [sb(st, f"hTx{i}", [128, 8, 256], BF16) for i in range(6)]
                        ya0 = sb(st, "ya0", [128, 4, 1024], F32)
                        yax = sb(st, "yax", [128, 12, 1024], F32)

                        def geom(blk):
                            return (0, 512) if blk == 0 else (512 + 256 * (blk - 1), 256)

                        def yslice(blk, s4, half):
                            if blk == 0:
                                return ya0[:, s4, half * 512:(half + 1) * 512]
                            return yax[:, (blk - 1) * 2 + s4, half * 512:(half + 1) * 512]
                        gtok = [sb(st, f"gtok{i}", [128, 1024], BF16) for i in range(NRING)]
                        P.begin()
                        ring = [0]
                        evq = [0]

                        def hbuf(e_, blk):
                            if blk == 0:
                                return hT0[e_ % 2], f"hT0_{e_ % 2}"
                            return hTx[blk - 1], f"hTx{blk - 1}"

                        def prep(e_, blk, cond):
                            hT, hkey = hbuf(e_, blk)
                            s0, W = geom(blk)
                            nst = W // 128
                            bufs = []
                            for s4 in range(nst):
                                r = ring[0] % NRING
                                ring[0] += 1
                                row0 = e_ * T_LAT + s0 + s4 * 128
                                P.dma("sp", DMA(gtok[r][:, :], Gd[row0:row0 + 128, :]), writes=[f"gt{r}"], key=f"gl{r}")
                                bufs.append(r)
                            for k in range(8):
                                bi = k % 4
                                for s4 in range(nst):
                                    P.op("pe", MM(bank(bi)[:, s4 * 128:(s4 + 1) * 128], gtok[bufs[s4]][:, k * 128:(k + 1) * 128], ident_b),
                                         reads=[f"gt{bufs[s4]}", "cbt"], writes=[f"B{bi}"], cond=cond)
                                evq[0] += 1
                                if evq[0] % 2 == 0:
                                    P.op("act", ACT(hT[:, k, 0:W], bank(bi)[:, 0:W], AF.Copy), reads=[f"B{bi}"], writes=[f"{hkey}_{k}"], cond=cond)
                                else:
                                    P.op("dve", CP(hT[:, k, 0:W], bank(bi)[:, 0:W]), reads=[f"B{bi}"], writes=[f"{hkey}_{k}"], cond=cond)

                        gi = 0
                        ai = 0
                        cq = [0]

                        def block_compute(e_, gidx, gs, b_, blk, cond):
                            nonlocal ai
                            hT, hkey = hbuf(e_, blk)
                            s0, W = geom(blk)
                            nst = W // 128
                            a_ = ai % 2
                            ai += 1
                            at = actt[a_]
                            for f in range(gs):
                                pg = (f % 2) * 2
                                pu = pg + 1
                                for k in range(8):
                                    P.op("pe", MM(bank(pg)[:, 0:W], wgt[b_][:, k, f * 128:(f + 1) * 128], hT[:, k, 0:W], start=(k == 0), stop=(k == 7)),
                                         reads=[f"wgt{b_}", f"{hkey}_{k}"], writes=[f"B{pg}"], cond=cond)
                                for k in range(8):
                                    P.op("pe", MM(bank(pu)[:, 0:W], wut[b_][:, k, f * 128:(f + 1) * 128], hT[:, k, 0:W], start=(k == 0), stop=(k == 7)),
                                         reads=[f"wut{b_}", f"{hkey}_{k}"], writes=[f"B{pu}"], cond=cond)
                                sg = sgt[f % 2]
                                P.op("act", ACT(sg[:, 0:W], bank(pg)[:, 0:W], AF.Silu), reads=[f"B{pg}"], writes=[f"sg{f % 2}"], cond=cond)
                                P.op("dve", TT(at[:, f, 0:W], sg[:, 0:W], bank(pu)[:, 0:W], ALU.mult),
                                     reads=[f"sg{f % 2}", f"B{pu}"], writes=[f"act{a_}_{f}"], cond=cond)
                            for half in range(2):
                                for s4 in range(nst):
                                    bd = 4 + (half * nst + s4) % 4
                                    for f in range(gs):
                                        P.op("pe", MM(bank(bd)[:, 0:512], at[:, f, s4 * 128:(s4 + 1) * 128], wdt[b_][:, f, half * 512:(half + 1) * 512],
                                                      start=(f == 0), stop=(f == gs - 1)),
                                             reads=[f"wdt{b_}", f"act{a_}_{f}"], writes=[f"B{bd}"], cond=cond)
                                    ya = yslice(blk, s4, half)
                                    yk = f"yacc{blk}_{s4}_{half}"
                                    if gidx == 0:
                                        cq[0] += 1
                                        if cq[0] % 2 == 0:
                                            P.op("act", ACT(ya, bank(bd)[:, 0:512], AF.Copy), reads=[f"B{bd}"], writes=[yk], cond=cond)
                                        else:
                                            P.op("dve", CP(ya, bank(bd)[:, 0:512]), reads=[f"B{bd}"], writes=[yk], cond=cond)
                                    else:
                                        P.op("dve", TT(ya, bank(bd)[:, 0:512], ya, ALU.add), reads=[f"B{bd}", yk], writes=[yk], cond=cond)

                        prep(0, 0, None)
                        for e_ in range(8):
                            wg_v = WL["wg"][e_].rearrange("(k p) f -> p k f", p=128)
                            wu_v = WL["wu"][e_].rearrange("(k p) f -> p k f", p=128)
                            wd_v = WL["wd"][e_].rearrange("(f p) d -> p f d", p=128)
                            for gidx, (f0, gs) in enumerate(groups):
                                b_ = gi % 2
                                gi += 1
                                P.dma("pool", DMAS([(wgt[b_][:, k, 0:gs * 128], wg_v[:, k, f0 * 128:(f0 + gs) * 128]) for k in range(8)]),
                                      writes=[f"wgt{b_}"], key=f"wg{b_}", n=8)
                                P.dma("pool", DMAS([(wut[b_][:, k, 0:gs * 128], wu_v[:, k, f0 * 128:(f0 + gs) * 128]) for k in range(8)]),
                                      writes=[f"wut{b_}"], key=f"wu{b_}", n=8)
                                P.dma("pool", DMAS([(wdt[b_][:, f, :], wd_v[:, f0 + f, :]) for f in range(gs)]),
                                      writes=[f"wdt{b_}"], key=f"wd{b_}", n=gs)
                                for blk in range(7):
                                    cond = None if blk == 0 else (e_, gidx, blk)
                                    if gidx == 0 and blk > 0:
                                        prep(e_, blk, cond)
                                    block_compute(e_, gidx, gs, b_, blk, cond)
                                if gidx == 3 and e_ < 7:
                                    prep(e_ + 1, 0, None)
                            row0 = e_ * T_LAT
                            P.dma("sp", DMA(Yd[row0:row0 + 512, :].rearrange("(s p) d -> p s d", p=128), ya0[:, :, :]),
                                  reads=[f"yacc0_{s4}_{half}" for s4 in range(4) for half in range(2)], key="yw0")
                            P.dma("sp", DMA(Yd[row0 + 512:row0 + T_LAT, :].rearrange("(s p) d -> p s d", p=128), yax[:, :, :]),
                                  reads=[f"yacc{blk}_{s4}_{half}" for blk in range(1, 7) for s4 in range(2) for half in range(2)], key="ywx")
                        P.end()

                    with ExitStack() as st:
                        xo = [sb(st, f"xo{i}", [128, 8, 512], F32) for i in range(2)]
                        yg = [[sb(st, f"yg{i}_{j}", [128, 1024], F32) for j in range(2)] for i in range(3)]
                        ttl = [sb(st, f"ttl{i}", [128, 1024], F32) for i in range(2)]
                        ot = [sb(st, f"ot{i}", [128, 1024], F32) for i in range(2)]
                        g2row = sb(st, "g2row", [128, 1024], F32)
                        dg = [sb(st, f"dg{i}", [128, 128], F32) for i in range(2)]
                        onesf = sb(st, "onesf", [128, 128], F32)
                        P.begin()
                        P.op("dve", MEMSET(onesf[:], 1.0), writes=["onesf"])
                        for m in range(8):
                            d_ = dg[m % 2]
                            P.op("dve", TS(d_[:, :], ident_f[:, :], Bcol(5, m, 0), None, ALU.mult), reads=["ident_f", "modc"], writes=[f"dg{m % 2}"])
                            bq_ = 6 + (m % 2)
                            P.op("pe", MM(bank(bq_)[:, 0:128], onesf[:, :], d_[:, :]), reads=["onesf", f"dg{m % 2}"], writes=[f"B{bq_}"])
                            P.op("act", ACT(g2row[:, m * 128:(m + 1) * 128], bank(bq_)[:, 0:128], AF.Copy), reads=[f"B{bq_}"], writes=["g2row"])
                        for tg in range(16):
                            b4 = tg // 4
                            xb = xo[b4 % 2]
                            if tg % 4 == 0:
                                P.dma("sp", DMA(xb[:, :, :], xs_v[:, :, b4 * 512:(b4 + 1) * 512]), writes=[f"xo{b4 % 2}"], key=f"xo{b4 % 2}")
                            pz = PS2[tg % 2]
                            bk0 = 2 * (tg % 2)
                            for k in range(8):
                                P.op("pe", TR(pz[:, k * 128:(k + 1) * 128], xb[:, k, (tg % 4) * 128:(tg % 4 + 1) * 128], ident_f[:, :]),
                                     reads=[f"xo{b4 % 2}", "ident_f"], writes=[f"B{bk0 + k // 4}"])
                            y0, y1 = yg[tg % 3]
                            for j in range(2):
                                P.dma("pool", IGATH(yg[tg % 3][j][:, :], Yd[:, :], ridx[:, j, tg:tg + 1], P.bc),
                                      reads=["ridx"], writes=[f"yg{tg % 3}_{j}"], key=f"yg{tg % 3}_{j}")
                            t_ = ttl[tg % 2]
                            tk = f"ttl{tg % 2}"
                            P.op("act", ACT(t_[:, :], y1[:, :], AF.Identity, scale=wts[:, 1, tg:tg + 1]), reads=[f"yg{tg % 3}_1", "wts"], writes=[tk])
                            P.op("dve", STT(t_[:, :], y0[:, :], wts[:, 0, tg:tg + 1], t_[:, :], ALU.mult, ALU.add),
                                 reads=[f"yg{tg % 3}_0", "wts", tk], writes=[tk])
                            P.op("dve", TT(t_[:, :], t_[:, :], g2row[:, :], ALU.mult), reads=[tk, "g2row"], writes=[tk])
                            o_ = ot[tg % 2]
                            P.op("dve", TT(o_[:, 0:512], t_[:, 0:512], pz[:, 0:512], ALU.add), reads=[tk, f"B{bk0}"], writes=[f"ot{tg % 2}a"])
                            P.op("dve", TT(o_[:, 512:1024], t_[:, 512:1024], pz[:, 512:1024], ALU.add), reads=[tk, f"B{bk0 + 1}"], writes=[f"ot{tg % 2}b"])
                            P.dma("sp", DMA(out_d[tg * 128:(tg + 1) * 128, :], o_[:, :]), reads=[f"ot{tg % 2}a", f"ot{tg % 2}b"], key=f"out{tg % 2}")
                        P.end()
                continue

            with ExitStack() as ffn:
                ntok = T_LAT if last else T_ALL
                fblocks = BLOCKS[:4] if last else BLOCKS
                xT = sb(ffn, "xT", [128, 8, ntok], F32)
                h2T = sb(ffn, "h2T", [128, 8, ntok], BF16)
                if last:
                    gT = sb(ffn, "gT", [128, T_LAT], F32)
                with ExitStack() as st:
                    SR = mk_sets(ffn, 4)
                    if last:
                        hf = sb(st, "hf", [128, 8, 512], F32)
                        rt = sb(st, "rt", [128, 8, 8], F32)
                        Lg = sb(st, "Lg", [128, 16, 8], F32)
                        L2 = sb(st, "L2", [128, 16, 8], F32)
                        mk1 = sb(st, "mk1", [128, 16, 8], F32)
                        mk2 = sb(st, "mk2", [128, 16, 8], F32)
                        gts = sb(st, "gts", [128, 16, 8], F32)
                        sm = sb(st, "sm", [128, 6, 16], F32)
                    P.begin()
                    for bi_, (c0, n) in enumerate(fblocks):
                        P.dma("sp", DMA(xT[:, :, c0:c0 + n], xs_v[:, :, c0:c0 + n]), writes=[f"xT{bi_}"], key=f"xT{bi_}")
                    if last:
                        P.dma("sp", DMA(rt[:], WL["router"].rearrange("(k p) e -> p k e", p=128)), writes=["rt"], key="rt")

                    norm_fns = []
                    for bi_, (c0, n) in enumerate(fblocks):
                        def norm_blk(bi_=bi_, c0=c0, n=n):
                            i_mod = 0 if c0 < T_LAT else 1

                            r3 = bi_ % 2
                            rstd_from([(xT[:, k, c0:c0 + n], [f"xT{bi_}"]) for k in range(8)], 128, n, 1.0 / 32.0, ones_b,
                                      [(SR[0]["sq"], "sq0"), (SR[1]["sq"], "sq1")], (SR[2 + r3]["ms"], f"ms{2 + r3}"))
                            rst_ = SR[2 + r3]["ms"]
                            for k in range(8):
                                t_, tk = SR[k % 2]["ms"], f"ms{k % 2}"
                                P.op("dve", STT(t_[:, 0:n], xT[:, k, c0:c0 + n], A2[:, k, i_mod:i_mod + 1], rst_[:, 0:n], ALU.mult, ALU.mult),
                                     reads=[f"xT{bi_}", f"ms{2 + r3}", "A2"], writes=[tk])
                                if not last:
                                    if k % 2 == 0:
                                        P.op("dve", TS(h2T[:, k, c0:c0 + n], t_[:, 0:n], Bcol(3, k, i_mod), None, ALU.add),
                                             reads=[tk, "modc"], writes=[f"h2T{k}_{bi_}"])
                                    else:
                                        P.op("act", ACT(h2T[:, k, c0:c0 + n], t_[:, 0:n], AF.Identity, bias=Bcol(3, k, i_mod), scale=1.0),
                                             reads=[tk, "modc"], writes=[f"h2T{k}_{bi_}"])
                                else:
                                    P.op("act", ACT(hf[:, k, 0:n], t_[:, 0:n], AF.Identity, bias=Bcol(3, k, i_mod), scale=1.0),
                                         reads=[tk, "modc"], writes=[f"hf{k}"])
                                    P.op("pool", CP(h2T[:, k, c0:c0 + n], hf[:, k, 0:n]), reads=[f"hf{k}"], writes=[f"h2T{k}"])
                            if last:
                                for tt in range(n // 128):
                                    tg = (c0 // 128) + tt
                                    bl = nb()
                                    for k in range(8):
                                        P.op("pe", MM(bank(bl)[:, 0:8], hf[:, k, tt * 128:(tt + 1) * 128], rt[:, k, :], start=(k == 0), stop=(k == 7)),
                                             reads=[f"hf{k}", "rt"], writes=[f"B{bl}"])
                                    P.op("dve", CP(Lg[:, tg, :], bank(bl)[:, 0:8]), reads=[f"B{bl}"], writes=["Lg"])
                        norm_fns.append(norm_blk)
                    norm_fns[0]()
                    if last:
                        m1, m2, df, e2, g1, g2 = (sm[:, i, :] for i in range(6))
                        P.op("dve", RMAX(m1, Lg[:, :, :]), reads=["Lg"], writes=["m1"])
                        for e_ in range(8):
                            P.op("dve", TT(mk1[:, :, e_], Lg[:, :, e_], m1, ALU.is_equal), reads=["Lg", "m1"], writes=["mk1"])
                        P.op("dve", STT(L2[:, :, :], mk1[:, :, :], -1e30, Lg[:, :, :], ALU.mult, ALU.add), reads=["mk1", "Lg"], writes=["L2"])
                        P.op("dve", RMAX(m2, L2[:, :, :]), reads=["L2"], writes=["m2"])
                        for e_ in range(8):
                            P.op("dve", TT(mk2[:, :, e_], L2[:, :, e_], m2, ALU.is_equal), reads=["L2", "m2"], writes=["mk2"])
                        P.op("dve", TT(df, m2, m1, ALU.subtract), reads=["m1", "m2"], writes=["df"])
                        P.op("act", ACT(e2, df, AF.Exp), reads=["df"], writes=["e2"])
                        P.op("dve", TS(g1, e2, 1.0, None, ALU.add), reads=["e2"], writes=["g1"])
                        P.op("dve", RECIP(g1, g1), reads=["g1"], writes=["g1"])
                        P.op("dve", TT(g2, e2, g1, ALU.mult), reads=["e2", "g1"], writes=["g2"])
                        for e_ in range(8):
                            P.op("dve", TT(gts[:, :, e_], mk1[:, :, e_], g1, ALU.mult), reads=["mk1", "g1"], writes=["gts"])
                            P.op("dve", TT(mk2[:, :, e_], mk2[:, :, e_], g2, ALU.mult), reads=["mk2", "g2"], writes=["mk2"])
                        P.op("dve", TT(gts[:, :, :], gts[:, :, :], mk2[:, :, :], ALU.add), reads=["gts", "mk2"], writes=["gts"])
                        for q4 in range(4):
                            bg = nb()
                            for tt in range(4):
                                tg = q4 * 4 + tt
                                P.op("pe", TR(bank(bg)[0:8, tt * 128:(tt + 1) * 128], gts[:, tg, :], ident_f[:, :]),
                                     reads=["gts", "ident_f"], writes=[f"B{bg}"])
                            P.op("dve", CP(gT[0:8, q4 * 512:(q4 + 1) * 512], bank(bg)[0:8, 0:512]), reads=[f"B{bg}"], writes=["gT"])

                GS = 4
                groups = []
                f0 = 0
                while f0 < NFC:
                    gs = min(GS, NFC - f0)
                    groups.append((f0, gs))
                    f0 += gs
                with ExitStack() as st:
                    wgt = [sb(st, f"wgt{i}", [128, 8, GS * 128], BF16) for i in range(2)]
                    wut = [sb(st, f"wut{i}", [128, 8, GS * 128], BF16) for i in range(2)]
                    wdt = [sb(st, f"wdt{i}", [128, GS, 1024], BF16) for i in range(2)]
                    sgt = [sb(st, f"sgt{i}", [128, 512], F32) for i in range(2)]
                    actt = [sb(st, f"actt{i}", [128, GS, 512], BF16) for i in range(2)]
                    if last:
                        Ge = [sb(st, f"Ge{i}", [128, T_LAT], BF16) for i in range(2)]
                        selE = sb(st, "selE", [128, 8, 128], F32)
                    nexp = 8 if last else 1
                    if last:
                        P.dma("sp", DMA(selE[0:8, :, :], selE_d[:, :, :]), writes=["selE"], key="selE")
                    gi = 0
                    ai = 0
                    rr[0] = 0
                    for e_ in range(nexp):
                        wg_v = WL["wg"][e_].rearrange("(k p) f -> p k f", p=128)
                        wu_v = WL["wu"][e_].rearrange("(k p) f -> p k f", p=128)
                        wd_v = WL["wd"][e_].rearrange("(f p) d -> p f d", p=128)
                        if last:
                            ge = Ge[e_ % 2]
                            for q4 in range(4):
                                bg = 6 + (q4 % 2)
                                P.op("pe", MM(bank(bg)[:, 0:512], selE[0:8, e_, :], gT[0:8, q4 * 512:(q4 + 1) * 512]),
                                     reads=["selE", "gT"], writes=[f"B{bg}"])
                                P.op("act", ACT(ge[:, q4 * 512:(q4 + 1) * 512], bank(bg)[:, 0:512], AF.Copy), reads=[f"B{bg}"], writes=[f"Ge{e_ % 2}"])
                        for (f0, gs) in groups:
                            b_ = gi % 2
                            gi += 1
                            P.dma("pool", DMAS([(wgt[b_][:, k, 0:gs * 128], wg_v[:, k, f0 * 128:(f0 + gs) * 128]) for k in range(8)]),
                                  writes=[f"wgt{b_}"], key=f"wg{b_}", n=8)
                            P.dma("pool", DMAS([(wut[b_][:, k, 0:gs * 128], wu_v[:, k, f0 * 128:(f0 + gs) * 128]) for k in range(8)]),
                                  writes=[f"wut{b_}"], key=f"wu{b_}", n=8)
                            P.dma("pool", DMAS([(wdt[b_][:, f, :], wd_v[:, f0 + f, :]) for f in range(gs)]),
                                  writes=[f"wdt{b_}"], key=f"wd{b_}", n=gs)
                            for bidx, (c0, n) in enumerate(fblocks):
                                if e_ == 0 and f0 == 0 and bidx + 1 < len(fblocks):
                                    norm_fns[bidx + 1]()
                                i_mod = 0 if c0 < T_LAT else 1
                                a_ = ai % 2
                                ai += 1
                                at = actt[a_]
                                for f in range(gs):
                                    pg = (f % 2) * 2
                                    pu = pg + 1
                                    for k in range(8):
                                        P.op("pe", MM(bank(pg)[:, 0:n], wgt[b_][:, k, f * 128:(f + 1) * 128], h2T[:, k, c0:c0 + n], start=(k == 0), stop=(k == 7)),
                                             reads=[f"wgt{b_}", f"h2T{k}_{bidx}"], writes=[f"B{pg}"])
                                    for k in range(8):
                                        P.op("pe", MM(bank(pu)[:, 0:n], wut[b_][:, k, f * 128:(f + 1) * 128], h2T[:, k, c0:c0 + n], start=(k == 0), stop=(k == 7)),
                                             reads=[f"wut{b_}", f"h2T{k}_{bidx}"], writes=[f"B{pu}"])
                                    sg = sgt[f % 2]
                                    P.op("act", ACT(sg[:, 0:n], bank(pg)[:, 0:n], AF.Silu), reads=[f"B{pg}"], writes=[f"sg{f % 2}"])
                                    if last:
                                        P.op("dve", TT(sg[:, 0:n], sg[:, 0:n], Ge[e_ % 2][:, c0:c0 + n], ALU.mult),
                                             reads=[f"sg{f % 2}", f"Ge{e_ % 2}"], writes=[f"sg{f % 2}"])
                                    P.op("dve", TT(at[:, f, 0:n], sg[:, 0:n], bank(pu)[:, 0:n], ALU.mult),
                                         reads=[f"sg{f % 2}", f"B{pu}"], writes=[f"act{a_}_{f}"])
                                for m in range(8):
                                    bd = 4 + (m % 4)
                                    for f in range(gs):
                                        P.op("pe", MM(bank(bd)[:, 0:n], wdt[b_][:, f, m * 128:(m + 1) * 128], at[:, f, 0:n], start=(f == 0), stop=(f == gs - 1)),
                                             reads=[f"wdt{b_}", f"act{a_}_{f}"], writes=[f"B{bd}"])
                                    P.op("dve", STT(xT[:, m, c0:c0 + n], bank(bd)[:, 0:n], Bcol(5, m, i_mod), xT[:, m, c0:c0 + n], ALU.mult, ALU.add),
                                         reads=[f"B{bd}", "modc", f"xT{m}_{c0}"], writes=[f"xT{m}_{c0}"])
                                    if (not last) and e_ == nexp - 1 and f0 + gs == NFC:
                                        P.dma("sp", DMA(xs_v[:, m, c0:c0 + n], xT[:, m, c0:c0 + n]), reads=[f"xT{m}_{c0}"], key="xo")
                    P.end()

                if not last:
                    pass
                else:
                    with ExitStack() as st:
                        ot = [sb(st, f"ot{i}", [128, 1024], F32) for i in range(2)]
                        P.begin()
                        for tt in range(16):
                            o_ = ot[tt % 2]
                            pz = PS2[tt % 2]
                            for k in range(8):
                                P.op("pe", TR(pz[:, k * 128:(k + 1) * 128], xT[:, k, tt * 128:(tt + 1) * 128], ident_f[:, :]),
                                     reads=["ident_f"], writes=[f"PZ{tt % 2}"])
                            P.op("act", ACT(o_[:, 0:512], pz[:, 0:512], AF.Copy), reads=[f"PZ{tt % 2}"], writes=[f"ot{tt % 2}a"])
                            P.op("dve", CP(o_[:, 512:1024], pz[:, 512:1024]), reads=[f"PZ{tt % 2}"], writes=[f"ot{tt % 2}b"])
                            P.dma("sp", DMA(out_d[tt * 128:(tt + 1) * 128, :], o_[:, :]), reads=[f"ot{tt % 2}a", f"ot{tt % 2}b"], key=f"out{tt % 2}")
                        P.end()
    return nc


_CACHE = {}


def _prep_inputs(inputs):
    f = lambda a: np.ascontiguousarray(np.asarray(a, dtype=np.float32))
    consts = make_consts()
    shared = dict(consts)
    for L in range(2):
        p = f"l{L}_"
        shared[p + "w_mod"] = f(inputs[p + "w_mod"])
        vecs = np.concatenate([f(inputs[p + "b_mod"]).reshape(48, 128), f(inputs[p + "norm_attn"]).reshape(8, 128),
                               f(inputs[p + "norm_ffn"]).reshape(8, 128), f(inputs[p + "q_lat_norm"]).reshape(2, 128),
                               f(inputs[p + "kv_lat_norm"]).reshape(1, 128)], axis=0)
        shared[p + "vecs"] = np.ascontiguousarray(vecs)
        for nm in ("w_in", "w_uq", "w_ukv", "w_out"):
            shared[p + nm] = f(inputs[p + nm])
        for nm in ("mla_q_gain", "mla_k_gain", "gqa_q_gain", "gqa_k_gain"):
            shared[p + nm] = f(inputs[p + nm]).reshape(-1, 1)
    for nm in ("l0_ffn_w_gate", "l0_ffn_w_up", "l0_ffn_w_down", "l1_router", "l1_exp_w_gate", "l1_exp_w_up", "l1_exp_w_down"):
        shared[nm] = f(inputs[nm])
    x = f(inputs["x"])
    ctx = f(inputs["ctx"])
    c = f(inputs["c"])
    cc = f(inputs["c_ctx"]).reshape(8, 128)
    in_maps = []
    for b in range(8):
        m = dict(shared)
        m["x"] = x[b]
        m["ctx"] = ctx[b]
        m["cvec"] = np.ascontiguousarray(np.concatenate([c[b].reshape(8, 128), cc], axis=0))
        in_maps.append(m)
    return in_maps


def kernel(**inputs):
    if "nc" not in _CACHE:
        _CACHE["nc"] = build_program()
    nc = _CACHE["nc"]
    in_maps = _prep_inputs(inputs)
    res = run_bass_kernel_spmd(nc, in_maps, core_ids=list(range(8)))
    out = np.stack([np.asarray(res.results[b]["out"], dtype=np.float32) for b in range(8)], axis=0)
    return out
```

```python
import numpy as np
from contextlib import ExitStack
import concourse.bass as bass
import concourse.mybir as mybir
from concourse.bass_utils import run_bass_kernel_spmd

F32 = mybir.dt.float32
BF16 = mybir.dt.bfloat16
I32 = mybir.dt.int32
AF = mybir.ActivationFunctionType
ALU = mybir.AluOpType
AX = mybir.AxisListType

ENGS = ("pe", "act", "dve", "pool", "sp")
ENGOBJ = {"pe": "tensor", "act": "scalar", "dve": "vector", "pool": "gpsimd", "sp": "sync"}

T_LAT, T_CTX, T_ALL = 2048, 256, 2304
EPS = 1e-6
THETA = 10000.0
FF = 2816
NFC = FF // 128


class _Op:
    __slots__ = ("eng", "fn", "deps", "is_dma", "dkey", "sig", "idx", "dcount", "ndma", "cond")

    def __init__(self, eng, fn, is_dma=False, dkey=None, ndma=1, cond=None):
        self.cond = cond
        self.eng = eng
        self.fn = fn
        self.deps = []
        self.is_dma = is_dma
        self.dkey = dkey
        self.sig = False
        self.idx = None
        self.dcount = None
        self.ndma = ndma


class Prog:
    NDMA = 14

    def __init__(self, nc):
        self.nc = nc
        self.sets = []
        for s in range(2):
            d = {e: nc.alloc_semaphore(name=f"s{s}_{e}") for e in ENGS}
            d["dma"] = [nc.alloc_semaphore(name=f"s{s}_d{i}") for i in range(self.NDMA)]
            self.sets.append(d)
        self.phase_no = 0
        self.count = 0
        self.dtot = {}
        self.limit = None
        self.ops = None
        self.dirty = [False, False]
        self.flags_ap = None
        self.nlvl = 6
        self.regs = {}
        self.loaded = {}
        with nc.Block() as block:
            for e in ENGS:
                def make(e):
                    def body(eng):
                        for st in self.sets:
                            eng.sem_clear(st[e])
                            if e == "pool":
                                for sm in st["dma"]:
                                    eng.sem_clear(sm)
                        if e == "pool":
                            r = eng.alloc_register("idma_bound")
                            eng.reg_mov(r, 8 * T_LAT - 1)
                            self.bc = eng.snap(r)
                    return body
                getattr(block, ENGOBJ[e])(make(e))

    def begin(self):
        self.ops = []
        self.lastw = {}
        self.readers = {}
        self.dkeys = {}

    def _add(self, op, reads, writes):
        deps = []
        for r in reads:
            w = self.lastw.get(r)
            if w is not None:
                deps.append(w)
        for wk in writes:
            w = self.lastw.get(wk)
            if w is not None:
                deps.append(w)
            deps.extend(self.readers.get(wk, ()))
        seen = set()
        for d in deps:
            if d is op or id(d) in seen:
                continue
            seen.add(id(d))
            if d.eng == "pe" and op.eng == "pe" and not d.is_dma and not op.is_dma:
                continue
            op.deps.append(d)
        for r in reads:
            self.readers.setdefault(r, []).append(op)
        for wk in writes:
            self.lastw[wk] = op
            self.readers[wk] = []
        self.ops.append(op)
        return op

    def op(self, eng, fn, reads=(), writes=(), cond=None):
        return self._add(_Op(eng, fn, cond=cond), list(reads), list(writes))

    def dma(self, queue, fn, reads=(), writes=(), key=None, n=1):
        if key not in self.dkeys:
            self.dkeys[key] = len(self.dkeys)
            assert len(self.dkeys) <= self.NDMA, "too many dma keys in phase"
        return self._add(_Op(queue, fn, True, key, n), list(reads), list(writes))

    def end(self):
        nc = self.nc
        ops = self.ops
        self.count += 1
        if self.limit is not None and self.count > self.limit:
            self.ops = None
            return
        if self.limit is not None and self.count == self.limit and getattr(self, "oplimit", None) is not None:
            ops = ops[:self.oplimit]
            print("phase ops total", len(self.ops), "emitting", len(ops))
        cur = self.phase_no % 2
        sems = self.sets[cur]
        other = self.sets[1 - cur]
        for o in ops:
            for d in o.deps:
                d.sig = True
        cnt = {e: 0 for e in ENGS}
        dcnt = {k: self.dtot.get(i, 0) for k, i in self.dkeys.items()}
        for o in ops:
            if o.is_dma:
                dcnt[o.dkey] = dcnt.get(o.dkey, 0) + 16 * o.ndma
                o.dcount = dcnt[o.dkey]
            elif o.sig:
                cnt[o.eng] += 1
                o.idx = cnt[o.eng]
        per = {e: [] for e in ENGS}
        for o in ops:
            per[o.eng].append(o)
        clear_other = self.dirty[1 - cur]
        dkeys = self.dkeys

        def dep_kv(d):
            if d.is_dma:
                return ("dma", dkeys[d.dkey]), d.dcount
            return d.eng, d.idx

        def do_waits(eng, need, waited):
            for k, v in need.items():
                s = self.sets[0]["dma"][k[1]] if isinstance(k, tuple) else sems[k]
                eng.wait_ge(s, v)
                waited[k] = v

        def emit_op(e, eng, o, waited):
            need = {}
            for d in o.deps:
                k, v = dep_kv(d)
                if waited.get(k, 0) >= v:
                    continue
                if need.get(k, 0) < v:
                    need[k] = v
            do_waits(eng, need, waited)
            ins = o.fn(eng)
            if o.is_dma:
                lst = ins if isinstance(ins, (list, tuple)) else [ins]
                assert len(lst) == o.ndma, (len(lst), o.ndma)
                for i_ in lst:
                    i_.then_inc(self.sets[0]["dma"][dkeys[o.dkey]], 16)
            elif o.sig:
                ins.then_inc(sems[e], 1)

        NLVL = self.nlvl

        def skip_regions(e, eng, regions, conds, snap):
            emitted = False
            for region in regions:
                need = {}
                nsig = 0
                for r_ in region:
                    if r_.sig:
                        nsig += 1
                    for d in r_.deps:
                        if d.cond in conds:
                            continue
                        k, v = dep_kv(d)
                        if snap.get(k, 0) >= v:
                            continue
                        if need.get(k, 0) < v:
                            need[k] = v
                do_waits(eng, need, snap)
                if nsig:
                    eng.sem_inc(sems[e], nsig)
                    emitted = True
            if not emitted:
                eng.nop()

        def emit_chain(e, eng, regions, waited):
            region = regions[0]
            lvl = region[0].cond[2]
            reg = self.regs[(e, lvl)]
            snap = dict(waited)
            conds = set(r[0].cond for r in regions)
            with eng.If_ne(reg, 0):
                w2 = dict(snap)
                for r_ in region:
                    emit_op(e, eng, r_, w2)
                if len(regions) > 1:
                    emit_chain(e, eng, regions[1:], w2)
            with eng.Else():
                skip_regions(e, eng, regions, conds, snap)
            return snap

        def emit(e, eng):
            waited = {}
            if clear_other:
                eng.sem_clear(other[e])
            lst = per[e]
            i = 0
            while i < len(lst):
                o = lst[i]
                if o.cond is None:
                    emit_op(e, eng, o, waited)
                    i += 1
                    continue
                eg = o.cond[:2]
                j = i
                while j < len(lst) and lst[j].cond is not None and lst[j].cond[:2] == eg:
                    j += 1
                chain = lst[i:j]
                i = j
                assert all(not r_.is_dma for r_ in chain)
                regions = []
                for r_ in chain:
                    if regions and regions[-1][0].cond == r_.cond:
                        regions[-1].append(r_)
                    else:
                        regions.append([r_])
                lv = [r[0].cond[2] for r in regions]
                assert lv == sorted(set(lv)), lv
                eidx = eg[0]
                if self.loaded.get(e) != eidx:
                    for lvl in range(1, NLVL + 1):
                        if (e, lvl) not in self.regs:
                            self.regs[(e, lvl)] = eng.alloc_register(f"fl_{e}_{lvl}")
                        c_ = eidx * NLVL + lvl - 1
                        eng.reg_load(self.regs[(e, lvl)], self.flags_ap[0:1, c_:c_ + 1])
                    self.loaded[e] = eidx
                waited = emit_chain(e, eng, regions, waited)
            last = {}
            for o in per[e]:
                if o.is_dma:
                    last[o.dkey] = o.dcount
            for k, v in last.items():
                if waited.get(("dma", dkeys[k]), 0) < v:
                    eng.wait_ge(self.sets[0]["dma"][dkeys[k]], v)

        with nc.Block() as block:
            for e in ENGS:
                def make(e):
                    def body(eng):
                        emit(e, eng)
                    return body
                getattr(block, ENGOBJ[e])(make(e))
        for k, i in self.dkeys.items():
            self.dtot[i] = dcnt[k]
        self.dirty[cur] = True
        self.phase_no += 1
        self.ops = None


def MM(out, lhsT, rhs, start=True, stop=True):
    return lambda e: e.matmul(out, lhsT=lhsT, rhs=rhs, start=start, stop=stop)


def TR(out, in_, ident):
    return lambda e: e.transpose(out, in_, ident)


def ACT(out, in_, func, bias=None, scale=None):
    kw = {}
    if bias is not None:
        kw["bias"] = bias
    if scale is not None:
        kw["scale"] = scale
    return lambda e: e.activation(out=out, in_=in_, func=func, **kw)


def TT(out, in0, in1, op):
    return lambda e: e.tensor_tensor(out=out, in0=in0, in1=in1, op=op)


def STT(out, in0, scalar, in1, op0, op1):
    return lambda e: e.scalar_tensor_tensor(out=out, in0=in0, scalar=scalar, in1=in1, op0=op0, op1=op1)


def TS(out, in0, s1, s2, op0, op1=None):
    if op1 is None:
        return lambda e: e.tensor_scalar(out=out, in0=in0, scalar1=s1, scalar2=None, op0=op0)
    return lambda e: e.tensor_scalar(out=out, in0=in0, scalar1=s1, scalar2=s2, op0=op0, op1=op1)


def CP(out, in_):
    return lambda e: e.tensor_copy(out=out, in_=in_)


def RECIP(out, in_):
    return lambda e: e.reciprocal(out=out, in_=in_)


def MEMSET(ap, v):
    return lambda e: e.memset(ap, v)


def RMAX(out, in_):
    return lambda e: e.reduce_max(out=out, in_=in_, axis=AX.X)


def DMA(out, in_):
    return lambda e: e.dma_start(out=out, in_=in_)


def RSUM(out, in_):
    return lambda e: e.reduce_sum(out=out, in_=in_, axis=AX.X)


def ISCAT(dram, idx_ap, src, bound):
    return lambda e: e.indirect_dma_start(out=dram, out_offset=bass.IndirectOffsetOnAxis(ap=idx_ap, axis=0), in_=src,
                                          in_offset=None, bounds_check=bound, oob_is_err=False)


def IGATH(dst, dram, idx_ap, bound):
    return lambda e: e.indirect_dma_start(out=dst, out_offset=None, in_=dram,
                                          in_offset=bass.IndirectOffsetOnAxis(ap=idx_ap, axis=0),
                                          bounds_check=bound, oob_is_err=False)


def DMAS(pairs):
    def f(e):
        return [e.dma_start(out=o, in_=i) for (o, i) in pairs]
    return f


def make_consts():
    c = {}
    c["ident_f"] = np.eye(128, dtype=np.float32)
    cb = np.zeros((128, 6, 128), np.float32)
    cb[:, 0, :] = np.eye(128)
    cb[:, 1, :] = 1.0
    cb[0:64, 2, 0:64] = 1.0
    cb[64:128, 2, 64:128] = 1.0
    for base in (0, 16):
        for i in range(8):
            a = 64 + base + i
            b = a + 8
            cb[b, 3, a] = -1.0
            cb[a, 3, b] = 1.0
    for hb in (0, 64):
        for base in (0, 32):
            for i in range(16):
                a = hb + base + i
                b = a + 16
                cb[b, 4, a] = -1.0
                cb[a, 4, b] = 1.0
    cb[:, 5, :] = np.triu(np.ones((128, 128), np.float32), 1)
    c["cb"] = cb
    c["ebase"] = np.ascontiguousarray(np.broadcast_to(np.tile(np.arange(8, dtype=np.float32) * 2048.0, 16)[None, :], (128, 128)))
    sel96 = np.zeros((32, 96), np.float32)
    for i in range(32):
        sel96[i, 64 + i] = 1.0
    c["sel96"] = sel96
    t = np.arange(T_LAT)
    row = (t // 64).astype(np.float64)
    col = (t % 64).astype(np.float64)
    tabs = np.zeros((128, 4, T_LAT), np.float32)
    tabs[:, 0, :] = 1.0
    tabs[:, 2, :] = 1.0
    fa = THETA ** (-np.arange(8, dtype=np.float64) / 8.0)
    fb = THETA ** (-np.arange(16, dtype=np.float64) / 16.0)
    fa32 = fa.astype(np.float32).astype(np.float64)
    fb32 = fb.astype(np.float32).astype(np.float64)
    for r in range(32):
        pos = row if r < 16 else col
        ang = (pos * fa32[r % 8]).astype(np.float32).astype(np.float64)
        tabs[64 + r, 0, :] = np.cos(ang)
        tabs[64 + r, 1, :] = np.sin(ang)
    for hb in (0, 64):
        for d in range(64):
            pos = row if d < 32 else col
            ang = (pos * fb32[d % 16]).astype(np.float32).astype(np.float64)
            tabs[hb + d, 2, :] = np.cos(ang)
            tabs[hb + d, 3, :] = np.sin(ang)
    c["tabs"] = tabs
    return c


LAYER_W = ["w_mod", "vecs", "w_in", "w_uq", "w_ukv", "mla_q_gain", "mla_k_gain", "gqa_q_gain",
           "gqa_k_gain", "w_out"]


def build_program(dbg=None, limit=None, dbg_fn=None, oplimit=None):
    nc = bass.Bass("TRN2", target_bir_lowering=False)

    def din(name, shape):
        return nc.dram_tensor(name, list(shape), F32, kind="ExternalInput").ap()

    x_d = din("x", [T_LAT, 1024])
    ctx_d = din("ctx", [T_CTX, 1024])
    cvec_d = din("cvec", [16, 128])
    identf_d = din("ident_f", [128, 128])
    cb_d = din("cb", [128, 6, 128])
    ebase_d = din("ebase", [128, 128])
    sel96_d = din("sel96", [32, 96])
    tabs_d = din("tabs", [128, 4, T_LAT])
    W = []
    for L in range(2):
        d = {}
        d["w_mod"] = din(f"l{L}_w_mod", [1024, 6144])
        d["vecs"] = din(f"l{L}_vecs", [67, 128])
        d["w_in"] = din(f"l{L}_w_in", [1024, 1184])
        d["w_uq"] = din(f"l{L}_w_uq", [256, 768])
        d["w_ukv"] = din(f"l{L}_w_ukv", [128, 1024])
        d["mla_q_gain"] = din(f"l{L}_mla_q_gain", [96, 1])
        d["mla_k_gain"] = din(f"l{L}_mla_k_gain", [96, 1])
        d["gqa_q_gain"] = din(f"l{L}_gqa_q_gain", [64, 1])
        d["gqa_k_gain"] = din(f"l{L}_gqa_k_gain", [64, 1])
        d["w_out"] = din(f"l{L}_w_out", [1024, 1024])
        if L == 0:
            d["wg"] = [din("l0_ffn_w_gate", [1024, FF])]
            d["wu"] = [din("l0_ffn_w_up", [1024, FF])]
            d["wd"] = [din("l0_ffn_w_down", [FF, 1024])]
        else:
            d["router"] = din("l1_router", [1024, 8])
            ne_ = 8 if (limit is None or limit > 12) else 1
            wg = din("l1_exp_w_gate", [ne_, 1024, FF])
            wu = din("l1_exp_w_up", [ne_, 1024, FF])
            wd = din("l1_exp_w_down", [ne_, FF, 1024])
            wg = [wg[min(e, ne_ - 1)] for e in range(8)]
            wu = [wu[min(e, ne_ - 1)] for e in range(8)]
            wd = [wd[min(e, ne_ - 1)] for e in range(8)]
            d["wg"] = wg
            d["wu"] = wu
            d["wd"] = wd
        W.append(d)
    out_d = nc.dram_tensor("out", [T_LAT, 1024], F32, kind="ExternalOutput").ap()
    xs = nc.dram_tensor("xs", [8, 128, T_ALL], F32, kind="Internal").ap()
    xs_v = xs.rearrange("k p t -> p k t")
    hs = nc.dram_tensor("hs", [8, 128, T_ALL], BF16, kind="Internal").ap()
    hs_v = hs.rearrange("k p t -> p k t")
    NSLOT = 8 * T_LAT
    Gd = nc.dram_tensor("Gd", [NSLOT, 1024], BF16, kind="Internal").ap()
    Yd = nc.dram_tensor("Yd", [NSLOT, 1024], F32, kind="Internal").ap()
    dbg_d = None
    if dbg is not None:
        dbg_d = nc.dram_tensor("dbg", [128, dbg], F32, kind="ExternalOutput").ap()

    P = Prog(nc)
    P.limit = limit
    P.oplimit = oplimit
    BLOCKS = [(0, 512), (512, 512), (1024, 512), (1536, 512), (2048, 256)]

    with ExitStack() as top:
        uid = [0]

        def sb(st, name, shape, dt):
            uid[0] += 1
            return st.enter_context(nc.sbuf_tensor(f"sb{uid[0]}_{name}", list(shape), dt))

        PS2 = [top.enter_context(nc.psum_tensor(f"ps{i}", [128, 1024], F32)) for i in range(4)]

        def bank(i):
            return PS2[i // 2][:, (i % 2) * 512:(i % 2) * 512 + 512]

        rr = [0]

        def nb():
            i = rr[0] % 8
            rr[0] += 1
            return i

        ident_f = sb(top, "ident_f", [128, 128], F32)
        cbt = sb(top, "cbt", [128, 6, 128], BF16)
        sel96 = sb(top, "sel96", [128, 96], BF16)
        epsc = sb(top, "epsc", [128, 1], F32)
        cols = sb(top, "cols", [128, 83], F32)
        modc = sb(top, "modc", [128, 48, 2], F32)
        A1 = sb(top, "A1", [128, 8, 2], F32)
        A2 = sb(top, "A2", [128, 8, 2], F32)
        gq = sb(top, "gq", [128, 4], F32)
        ident_b = cbt[:, 0, :]
        ones_b = cbt[:, 1, :]
        bonesB = cbt[:, 2, :]
        PA = cbt[:, 3, :]
        PB = cbt[:, 4, :]
        triU = cbt[:, 5, :]

        P.begin()
        P.dma("sp", DMA(ident_f[:], identf_d[:, :]), writes=["ident_f"], key="c0")
        P.dma("pool", DMA(cbt[:], cb_d[:, :, :]), writes=["cbt"], key="c1")
        P.dma("pool", DMA(sel96[0:32, :], sel96_d[:, :]), writes=["sel96"], key="c2")
        P.op("dve", MEMSET(epsc[:], EPS), writes=["epsc"])
        P.end()

        with ExitStack() as st:
            xin = [sb(st, f"xin{i}", [128, 1024], F32) for i in range(2)]
            xblk = sb(st, "xblk", [128, 8, 512], F32)
            P.begin()
            ti = 0
            for (c0, n) in BLOCKS:
                nt = n // 128
                for tt in range(nt):
                    tok0 = c0 + tt * 128
                    src = x_d[tok0:tok0 + 128, :] if tok0 < T_LAT else ctx_d[tok0 - T_LAT:tok0 - T_LAT + 128, :]
                    b_ = ti % 2
                    P.dma("sp", DMA(xin[b_][:], src), writes=[f"xin{b_}"], key=f"xin{b_}")
                    for k in range(8):
                        P.op("pe", TR(bank(k)[:, tt * 128:(tt + 1) * 128], xin[b_][:, k * 128:(k + 1) * 128], ident_f[:]),
                             reads=[f"xin{b_}", "ident_f"], writes=[f"B{k}"])
                    ti += 1
                for k in range(8):
                    eng = "act" if k % 2 == 0 else "dve"
                    fn = ACT(xblk[:, k, 0:n], bank(k)[:, 0:n], AF.Copy) if eng == "act" else CP(xblk[:, k, 0:n], bank(k)[:, 0:n])
                    P.op(eng, fn, reads=[f"B{k}"], writes=[f"xblk{k}"])
                P.dma("sp", DMA(xs_v[:, :, c0:c0 + n], xblk[:, :, 0:n]), reads=[f"xblk{k}" for k in range(8)], key="xo")
            P.end()

        for L in range(2):
            WL = W[L]
            last = (L == 1)
            with ExitStack() as st:
                stage = sb(st, "stage", [128, 128], F32)
                scb = sb(st, "scb", [128, 8, 2], BF16)
                wm = [sb(st, f"wm{i}", [128, 8, 1024], BF16) for i in range(2)]
                wmod_v = WL["w_mod"].rearrange("(k p) n -> p k n", p=128)
                P.begin()
                P.op("dve", MEMSET(stage[:], 0.0), writes=["stage"])
                P.dma("sp", DMA(stage[0:67, :], WL["vecs"][:, :]), reads=["stage"], writes=["stage"], key="st")
                P.dma("sp", DMA(stage[67:83, :], cvec_d[:, :]), writes=["stage"], key="st")
                P.dma("sp", DMAS([(gq[0:96, 0:1], WL["mla_q_gain"][:, :]), (gq[0:96, 1:2], WL["mla_k_gain"][:, :]),
                                  (gq[0:64, 2:3], WL["gqa_q_gain"][:, :]), (gq[64:128, 2:3], WL["gqa_q_gain"][:, :]),
                                  (gq[0:64, 3:4], WL["gqa_k_gain"][:, :]), (gq[64:128, 3:4], WL["gqa_k_gain"][:, :])]),
                      writes=["gq"], key="gq", n=6)
                b0 = nb()
                P.op("pe", TR(bank(b0)[:, 0:128], stage[:, :], ident_f[:, :]), reads=["stage", "ident_f"], writes=[f"B{b0}"])
                P.op("dve", CP(cols[:, :], bank(b0)[:, 0:83]), reads=[f"B{b0}"], writes=["cols"])
                P.op("act", ACT(scb[:, :, 0], cols[:, 67:75], AF.Silu), reads=["cols"], writes=["scb"])
                P.op("act", ACT(scb[:, :, 1], cols[:, 75:83], AF.Silu), reads=["scb", "cols"], writes=["scb"])
                bm = nb()
                psm = bank(bm)[:, 0:96].rearrange("p (m i) -> p m i", i=2)
                for sec in range(6):
                    b_ = sec % 2
                    P.dma("pool", DMAS([(wm[b_][:, k, :], wmod_v[:, k, sec * 1024:(sec + 1) * 1024]) for k in range(8)]),
                          writes=[f"wm{b_}"], key=f"wm{b_}", n=8)
                    for m in range(8):
                        for k in range(8):
                            P.op("pe", MM(psm[:, sec * 8 + m, :], wm[b_][:, k, m * 128:(m + 1) * 128], scb[:, k, :],
                                          start=(k == 0), stop=(k == 7)),
                                 reads=[f"wm{b_}", "scb"], writes=[f"B{bm}"])
                for i in range(2):
                    P.op("dve", TT(modc[:, :, i], psm[:, :, i], cols[:, 0:48], ALU.add), reads=[f"B{bm}", "cols"], writes=["modc"])
                for i in range(2):
                    P.op("dve", STT(A1[:, :, i], modc[:, 8:16, i], 1.0, cols[:, 48:56], ALU.add, ALU.mult),
                         reads=["modc", "cols"], writes=["A1"])
                    P.op("dve", STT(A2[:, :, i], modc[:, 32:40, i], 1.0, cols[:, 56:64], ALU.add, ALU.mult),
                         reads=["modc", "cols"], writes=["A2"])
                P.end()

            def Bcol(sec, k, i):
                return modc[:, sec * 8 + k, i:i + 1]

            def mk_sets(st, ns):
                sets = []
                for i_ in range(ns):
                    sets.append(dict(i=i_, sq=sb(st, f"sq{i_}", [128, 512], BF16), ms=sb(st, f"ms{i_}", [128, 512], F32),
                                     kn=sb(st, f"kn{i_}", [128, 512], BF16), t1=sb(st, f"t1{i_}", [128, 512], BF16),
                                     t2=sb(st, f"t2{i_}", [128, 512], BF16)))
                return sets

            job = [0]

            def next_set(SR):
                S = SR[job[0] % len(SR)]
                job[0] += 1
                return S

            def rstd_from(srcs, rows, n, inv_sqrt_d, ones_ap, sqs, ms):
                bi = nb()
                mst, msk = ms
                for i, (ap, rk) in enumerate(srcs):
                    s_, sk = sqs[i % len(sqs)]
                    P.op("act", ACT(s_[0:rows, 0:n], ap, AF.Square, scale=inv_sqrt_d), reads=rk, writes=[sk])
                    P.op("pe", MM(bank(bi)[0:rows, 0:n], ones_ap, s_[0:rows, 0:n], start=(i == 0), stop=(i == len(srcs) - 1)),
                         reads=[sk, "cbt"], writes=[f"B{bi}"])
                P.op("act", ACT(mst[0:rows, 0:n], bank(bi)[0:rows, 0:n], AF.Ln, bias=epsc[0:rows, 0:1], scale=1.0),
                     reads=[f"B{bi}", "epsc"], writes=[msk])
                P.op("act", ACT(mst[0:rows, 0:n], mst[0:rows, 0:n], AF.Exp, scale=-0.5), reads=[msk], writes=[msk])

            def norm_mod(xj, n, Acol, sec, i, hj, SR, xk="xj", hk="hj"):
                rstd_from([(xj[:, k, 0:n], [xk]) for k in range(8)], 128, n, 1.0 / 32.0, ones_b,
                          [(SR[0]["sq"], "sq0"), (SR[1]["sq"], "sq1")], (SR[2]["ms"], "ms2"))
                for k in range(8):
                    t_, tk = SR[k % 2]["ms"], f"ms{k % 2}"
                    P.op("dve", STT(t_[:, 0:n], xj[:, k, 0:n], Acol[:, k, i:i + 1], SR[2]["ms"][:, 0:n], ALU.mult, ALU.mult),
                         reads=[xk, "ms2", "A1", "A2"], writes=[tk])
                    if k % 2 == 0:
                        P.op("dve", TS(hj[:, k, 0:n], t_[:, 0:n], Bcol(sec, k, i), None, ALU.add),
                             reads=[tk, "modc"], writes=[f"{hk}{k}"])
                    else:
                        P.op("act", ACT(hj[:, k, 0:n], t_[:, 0:n], AF.Identity, bias=Bcol(sec, k, i), scale=1.0),
                             reads=[tk, "modc"], writes=[f"{hk}{k}"])

            with ExitStack() as att:
                tabs = sb(att, "tabs", [128, 4, T_LAT], BF16)
                KaT = sb(att, "KaT", [128, 8, T_ALL], BF16)
                KbT = sb(att, "KbT", [128, 2, T_ALL], BF16)
                Vst = sb(att, "Vst", [128, 18, 1152], BF16)
                cosA, sinA, cosB, sinB = tabs[:, 0, :], tabs[:, 1, :], tabs[:, 2, :], tabs[:, 3, :]

                def head_norm_rope(pre_bank, rows, n, inv_sqrt_d, ones_ap, gcol, perm, cos_t, sin_t, c0, dst, rope,
                                   S, dstkey):
                    si = S["i"]
                    rstdt, knt, t1t, t2t = S["ms"], S["kn"], S["t1"], S["t2"]
                    pre = bank(pre_bank)[0:rows, 0:n]
                    rstd_from([(pre, [f"B{pre_bank}"])], rows, n, inv_sqrt_d, ones_ap, [(S["sq"], f"sq{si}")], (rstdt, f"ms{si}"))
                    if not rope:
                        P.op("dve", STT(dst, pre, gcol, rstdt[0:rows, 0:n], ALU.mult, ALU.mult),
                             reads=[f"B{pre_bank}", f"ms{si}", "gq"], writes=[dstkey])
                        return
                    P.op("dve", STT(knt[0:rows, 0:n], pre, gcol, rstdt[0:rows, 0:n], ALU.mult, ALU.mult),
                         reads=[f"B{pre_bank}", f"ms{si}", "gq"], writes=[f"kn{si}"])
                    br = pre_bank
                    P.op("pe", MM(bank(br)[0:rows, 0:n], perm[0:rows, 0:rows], knt[0:rows, 0:n]),
                         reads=[f"kn{si}", "cbt"], writes=[f"B{br}"])
                    P.op("pool", TT(t1t[0:rows, 0:n], knt[0:rows, 0:n], cos_t[0:rows, c0:c0 + n], ALU.mult),
                         reads=[f"kn{si}", "tabs"], writes=[f"t1{si}"])
                    P.op("dve", TT(t2t[0:rows, 0:n], bank(br)[0:rows, 0:n], sin_t[0:rows, c0:c0 + n], ALU.mult),
                         reads=[f"B{br}", "tabs"], writes=[f"t2{si}"])
                    P.op("dve", TT(dst, t1t[0:rows, 0:n], t2t[0:rows, 0:n], ALU.add),
                         reads=[f"t1{si}", f"t2{si}"], writes=[dstkey])

                def head_job(SRl, pre_mm, pre_reads, rows, n, inv_sqrt_d, ones_ap, gcol, perm, cos_t, sin_t, c0, dst, rope, dstkey):
                    stt = {}

                    def A():
                        S = next_set(SRl)
                        bp = nb()
                        stt["S"], stt["bp"] = S, bp
                        pre_mm(bp)
                        P.op("act", ACT(S["sq"][0:rows, 0:n], bank(bp)[0:rows, 0:n], AF.Square, scale=inv_sqrt_d),
                             reads=[f"B{bp}"], writes=[f"sq{S['i']}"])

                    def B():
                        S, bp = stt["S"], stt["bp"]
                        si = S["i"]
                        pre = bank(bp)[0:rows, 0:n]
                        ms = S["ms"][0:rows, 0:n]
                        bi = nb()
                        P.op("pe", MM(bank(bi)[0:rows, 0:n], ones_ap, S["sq"][0:rows, 0:n]), reads=[f"sq{si}", "cbt"], writes=[f"B{bi}"])
                        P.op("act", ACT(ms, bank(bi)[0:rows, 0:n], AF.Ln, bias=epsc[0:rows, 0:1], scale=1.0),
                             reads=[f"B{bi}", "epsc"], writes=[f"ms{si}"])
                        P.op("act", ACT(ms, ms, AF.Exp, scale=-0.5), reads=[f"ms{si}"], writes=[f"ms{si}"])
                        if not rope:
                            P.op("dve", STT(dst, pre, gcol, ms, ALU.mult, ALU.mult), reads=[f"B{bp}", f"ms{si}", "gq"], writes=[dstkey])
                        else:
                            P.op("dve", STT(S["kn"][0:rows, 0:n], pre, gcol, ms, ALU.mult, ALU.mult),
                                 reads=[f"B{bp}", f"ms{si}", "gq"], writes=[f"kn{si}"])

                    def C():
                        if not rope:
                            return
                        S, bp = stt["S"], stt["bp"]
                        si = S["i"]
                        knt, t1t, t2t = S["kn"], S["t1"], S["t2"]
                        P.op("pe", MM(bank(bp)[0:rows, 0:n], perm[0:rows, 0:rows], knt[0:rows, 0:n]),
                             reads=[f"kn{si}", "cbt"], writes=[f"B{bp}"])
                        P.op("pool", TT(t1t[0:rows, 0:n], knt[0:rows, 0:n], cos_t[0:rows, c0:c0 + n], ALU.mult),
                             reads=[f"kn{si}", "tabs"], writes=[f"t1{si}"])
                        P.op("dve", TT(t2t[0:rows, 0:n], bank(bp)[0:rows, 0:n], sin_t[0:rows, c0:c0 + n], ALU.mult),
                             reads=[f"B{bp}", "tabs"], writes=[f"t2{si}"])
                        P.op("dve", TT(dst, t1t[0:rows, 0:n], t2t[0:rows, 0:n], ALU.add),
                             reads=[f"t1{si}", f"t2{si}"], writes=[dstkey])
                    return [A, B, C]

                def run_pipeline(jobs):
                    ns = 3
                    for t in range(len(jobs) + ns - 1):
                        for s in range(ns):
                            j = t - s
                            if 0 <= j < len(jobs):
                                jobs[j][s]()

                with ExitStack() as st:
                    w_in = sb(st, "w_in", [128, 8, 160], BF16)
                    WKB = sb(st, "WKB", [128, 8, 2, 128], BF16)
                    WVB = sb(st, "WVB", [128, 8, 128], BF16)
                    WN = sb(st, "WN", [128, 8, 96], BF16)
                    WV = sb(st, "WV", [128, 8, 64], BF16)
                    xjs = [sb(st, f"xj{i}", [128, 8, 512], F32) for i in range(2)]
                    hjs = [sb(st, f"hj{i}", [128, 8, 512], BF16) for i in range(2)]
                    ckvn2 = [sb(st, f"ckvn{i}", [128, 512], BF16) for i in range(2)]
                    krope2 = [sb(st, f"krope{i}", [128, 512], BF16) for i in range(2)]
                    SR = mk_sets(st, 4)
                    win_v = WL["w_in"].rearrange("(k p) n -> p k n", p=128)
                    wukv_v = WL["w_ukv"].rearrange("p (h c) -> p h c", c=128)
                    P.begin()
                    P.dma("pool", DMAS([(w_in[:, k, 0:160], win_v[:, k, 256:416]) for k in range(8)]), writes=["w_in"], key="w_in", n=8)
                    P.op("dve", MEMSET(WN[:], 0.0), writes=["WN"])
                    P.dma("pool", DMA(WN[:, :, 0:64], wukv_v[:, :, 0:64]), reads=["WN"], writes=["WN"], key="WN")
                    P.dma("pool", DMA(WV[:, :, :], wukv_v[:, :, 64:128]), writes=["WV"], key="WV")
                    P.dma("pool", DMA(tabs[:], tabs_d[:, :, :]), writes=["tabs"], key="tabs")
                    P.dma("pool", DMAS([(WKB[:, k, g, h_ * 64:(h_ + 1) * 64], win_v[:, k, 928 + g * 64:928 + (g + 1) * 64])
                                        for k in range(8) for g in range(2) for h_ in range(2)]), writes=["WKB"], key="WKB", n=32)
                    P.dma("pool", DMAS([(WVB[:, k, :], win_v[:, k, 1056:1184]) for k in range(8)]), writes=["WVB"], key="WVB", n=8)
                    P.op("dve", MEMSET(Vst[:, :, :].rearrange("p k (a s c) -> p k a s c", s=3, c=64)[:, :, :, 1, :], 1.0), writes=["Vst"])
                    if L == 0:
                        zt = sb(st, "zt", [128, 4, 1024], BF16)
                        P.op("pool", MEMSET(zt[:], 0.0), writes=["zt"])
                    def kv_block(bj_, c0, n):
                        i_mod = 0 if c0 < T_LAT else 1
                        rope = c0 < T_LAT
                        p2 = bj_ % 2
                        xj = xjs[p2]
                        hj = hjs[p2]
                        ckvn = ckvn2[p2]
                        krope = krope2[p2]
                        xk_ = f"xj{p2}"
                        hk_ = f"hj{p2}_"
                        ck_ = f"ckvn{p2}"
                        kr_ = f"krope{p2}"

                        def preA():
                            P.dma("sp", DMA(xj[:, :, 0:n], xs_v[:, :, c0:c0 + n]), writes=[xk_], key=xk_)
                            if L == 0 and bj_ >= 1:
                                for zi in range((bj_ - 1) * 8, bj_ * 8):
                                    P.dma("sp", DMA(Gd[zi * 512:(zi + 1) * 512, :].rearrange("(s p) d -> p s d", p=128), zt[:, :, :]),
                                          reads=["zt"], key="zf")
                            norm_mod(xj, n, A1, 0, i_mod, hj, SR, xk=xk_, hk=hk_)
                            P.dma("sp", DMA(hs_v[:, :, c0:c0 + n], hj[:, :, 0:n]), reads=[f"{hk_}{k}" for k in range(8)], key="hso")

                        def preB():
                            bc = nb()
                            for k in range(8):
                                P.op("pe", MM(bank(bc)[:, 0:n], w_in[:, k, 0:128], hj[:, k, 0:n], start=(k == 0), stop=(k == 7)),
                                     reads=["w_in", f"{hk_}{k}"], writes=[f"B{bc}"])
                            S_ = next_set(SR)
                            rstd_from([(bank(bc)[:, 0:n], [f"B{bc}"])], 128, n, 128 ** -0.5, ones_b, [(S_["sq"], f"sq{S_['i']}")], (S_["ms"], f"ms{S_['i']}"))
                            P.op("dve", STT(ckvn[:, 0:n], bank(bc)[:, 0:n], cols[:, 66:67], S_["ms"][:, 0:n], ALU.mult, ALU.mult),
                                 reads=[f"B{bc}", f"ms{S_['i']}", "cols"], writes=[ck_])
                            bk = nb()
                            for k in range(8):
                                P.op("pe", MM(bank(bk)[0:32, 0:n], w_in[:, k, 128:160], hj[:, k, 0:n], start=(k == 0), stop=(k == 7)),
                                     reads=["w_in", f"{hk_}{k}"], writes=[f"B{bk}"])
                            P.op("act", ACT(krope[0:32, 0:n], bank(bk)[0:32, 0:n], AF.Copy), reads=[f"B{bk}"], writes=[kr_])

                        def main():
                            jobs = []
                            for h in range(8):
                                def pre_mla(bp, h=h):
                                    P.op("pe", MM(bank(bp)[0:96, 0:n], WN[:, h, :], ckvn[:, 0:n], start=True, stop=False),
                                         reads=["WN", ck_], writes=[f"B{bp}"])
                                    P.op("pe", MM(bank(bp)[0:96, 0:n], sel96[0:32, :], krope[0:32, 0:n], start=False, stop=True),
                                         reads=["sel96", kr_], writes=[f"B{bp}"])
                                jobs.append(head_job(SR, pre_mla, None, 96, n, 96 ** -0.5, ones_b[0:96, 0:96], gq[0:96, 1:2], PA, cosA, sinA, c0,
                                                     KaT[0:96, h, c0:c0 + n], rope, "KaT"))
                            for g in range(2):
                                def pre_gqa(bp, g=g):
                                    for k in range(8):
                                        P.op("pe", MM(bank(bp)[:, 0:n], WKB[:, k, g, :], hj[:, k, 0:n], start=(k == 0), stop=(k == 7)),
                                             reads=["WKB", f"{hk_}{k}"], writes=[f"B{bp}"])
                                jobs.append(head_job(SR, pre_gqa, None, 128, n, 0.125, bonesB, gq[:, 3:4], PB, cosB, sinB, c0,
                                                     KbT[:, g, c0:c0 + n], rope, "KbT"))
                            run_pipeline(jobs)

                        def vals():
                            for tt in range(n // 128):
                                kc = (c0 + tt * 128) // 128
                                bv = nb()
                                P.op("pe", MM(bank(bv)[:, 0:512], ckvn[:, tt * 128:(tt + 1) * 128], WV[:, :, :].rearrange("p h c -> p (h c)"),
                                              start=True, stop=True), reads=[ck_, "WV"], writes=[f"B{bv}"])
                                src = bank(bv)[:, 0:512].rearrange("p (a s c) -> p a s c", s=2, c=64)
                                dst = Vst[:, kc, 0:768].rearrange("p (a s c) -> p a s c", s=3, c=64)[:, :, 0:3:2, :]
                                P.op("act", ACT(dst, src, AF.Copy), reads=[f"B{bv}"], writes=["Vst"])
                                bw = nb()
                                for k in range(8):
                                    P.op("pe", MM(bank(bw)[:, 0:128], hj[:, k, tt * 128:(tt + 1) * 128], WVB[:, k, :], start=(k == 0), stop=(k == 7)),
                                         reads=["WVB", f"{hk_}{k}"], writes=[f"B{bw}"])
                                srcb = bank(bw)[:, 0:128].rearrange("p (g c) -> p g c", c=64)
                                for s_ in (0, 2):
                                    dstb = Vst[:, kc, 768:1152].rearrange("p (g s c) -> p g s c", s=3, c=64)[:, :, s_, :]
                                    P.op("dve", CP(dstb, srcb), reads=[f"B{bw}"], writes=["Vst"])
                        return preA, preB, main, vals

                    kvb = [kv_block(bj_, c0, n) for bj_, (c0, n) in enumerate(BLOCKS)]
                    kvb[0][0]()
                    kvb[0][1]()
                    for bj_ in range(len(BLOCKS)):
                        if bj_ + 1 < len(BLOCKS):
                            kvb[bj_ + 1][0]()
                        kvb[bj_][2]()
                        if bj_ + 1 < len(BLOCKS):
                            kvb[bj_ + 1][1]()
                        kvb[bj_][3]()
                    P.end()

                with ExitStack() as st:
                    wq = sb(st, "wq", [128, 8, 768], BF16)
                    wuq = sb(st, "wuq", [128, 2, 768], BF16)
                    wout = sb(st, "wout", [128, 8, 1024], BF16)
                    xj = sb(st, "xj", [128, 8, 512], F32)
                    hjs2 = [sb(st, f"hjq{i}", [128, 8, 512], BF16) for i in range(2)]
                    QaT = sb(st, "QaT", [128, 8, 512], BF16)
                    QbT = sb(st, "QbT", [128, 4, 512], BF16)
                    cqn = sb(st, "cqn", [128, 2, 512], BF16)
                    PT = [sb(st, f"PT{i}", [128, 2, 512], BF16) for i in range(3)]
                    SR = mk_sets(st, 3)
                    rden = [SR[0]["ms"], SR[1]["ms"]]
                    win_v = WL["w_in"].rearrange("(k p) n -> p k n", p=128)
                    wuq_v = WL["w_uq"].rearrange("(k p) n -> p k n", p=128)
                    wout_v = WL["w_out"].rearrange("(k p) n -> p k n", p=128)
                    P.begin()
                    P.dma("pool", DMAS([(wq[:, k, 0:256], win_v[:, k, 0:256]) for k in range(8)]), writes=["wq"], key="wq", n=8)
                    P.dma("pool", DMAS([(wq[:, k, 256:768], win_v[:, k, 416:928]) for k in range(8)]), reads=["wq"], writes=["wq"], key="wq", n=8)
                    P.dma("pool", DMAS([(wuq[:, k, :], wuq_v[:, k, :]) for k in range(2)]), writes=["wuq"], key="wuq", n=2)
                    P.dma("pool", DMAS([(wout[:, k, :], wout_v[:, k, :]) for k in range(8)]), writes=["wout"], key="wout", n=8)
                    qblocks = BLOCKS[:4] if last else BLOCKS

                    def load_h(jb_):
                        c0_, n_ = qblocks[jb_]
                        p_ = jb_ % 2
                        P.dma("sp", DMA(hjs2[p_][:, :, 0:n_], hs_v[:, :, c0_:c0_ + n_]), writes=[f"hj{p_}_{k}" for k in range(8)], key=f"hjl{p_}")


                    def cq_stage(jb_):
                        c0_, n_ = qblocks[jb_]
                        hj_ = hjs2[jb_ % 2]
                        hq_ = f"hj{jb_ % 2}_"
                        bq = [nb(), nb()]
                        for c_ in range(2):
                            for k in range(8):
                                P.op("pe", MM(bank(bq[c_])[:, 0:n_], wq[:, k, c_ * 128:(c_ + 1) * 128], hj_[:, k, 0:n_], start=(k == 0), stop=(k == 7)),
                                     reads=["wq", f"{hq_}{k}"], writes=[f"B{bq[c_]}"])
                        S_ = SR[2]
                        rstd_from([(bank(bq[c_])[:, 0:n_], [f"B{bq[c_]}"]) for c_ in range(2)], 128, n_, 1.0 / 16.0, ones_b,
                                  [(S_["sq"], "sq2"), (S_["kn"], "kn2")], (S_["ms"], "ms2"))
                        for c_ in range(2):
                            P.op("dve", STT(cqn[:, c_, 0:n_], bank(bq[c_])[:, 0:n_], cols[:, 64 + c_:65 + c_], S_["ms"][:, 0:n_], ALU.mult, ALU.mult),
                                 reads=[f"B{bq[c_]}", "ms2", "cols"], writes=["cqn"])

                    load_h(0)
                    cq_stage(0)
                    for jb, (c0, n) in enumerate(qblocks):
                        lat = c0 < T_LAT
                        i_mod = 0 if lat else 1
                        hj = hjs2[jb % 2]
                        hq = f"hj{jb % 2}_"
                        P.dma("sp", DMA(xj[:, :, 0:n], xs_v[:, :, c0:c0 + n]), writes=["xj"], key="xj")
                        jobs = []
                        for h in range(8):
                            def pre_q(bp, h=h, n=n):
                                for c_ in range(2):
                                    P.op("pe", MM(bank(bp)[0:96, 0:n], wuq[:, c_, h * 96:(h + 1) * 96], cqn[:, c_, 0:n], start=(c_ == 0), stop=(c_ == 1)),
                                         reads=["wuq", "cqn"], writes=[f"B{bp}"])
                            jobs.append(head_job(SR, pre_q, None, 96, n, 96 ** -0.5, ones_b[0:96, 0:96], gq[0:96, 0:1], PA, cosA, sinA, c0,
                                                 QaT[0:96, h, 0:n], lat, f"QaT{h}"))
                        for m in range(4):
                            def pre_qb(bp, m=m, n=n):
                                for k in range(8):
                                    P.op("pe", MM(bank(bp)[:, 0:n], wq[:, k, 256 + m * 128:256 + (m + 1) * 128], hj[:, k, 0:n], start=(k == 0), stop=(k == 7)),
                                         reads=["wq", f"{hq}{k}"], writes=[f"B{bp}"])
                            jobs.append(head_job(SR, pre_qb, None, 128, n, 0.125, bonesB, gq[:, 2:3], PB, cosB, sinB, c0,
                                                 QbT[:, m, 0:n], lat, f"QbT{m}"))
                        run_pipeline(jobs)
                        if jb + 1 < len(qblocks):
                            load_h(jb + 1)
                        kchunks = list(range(18)) if lat else [16, 17]
                        items = []
                        for hh in range(16):
                            for pi in range(0, len(kchunks), 2):
                                items.append((hh, kchunks[pi:pi + 2], pi == 0, pi + 2 >= len(kchunks)))

                        def head_ops(hh):
                            if hh < 8:
                                return (lambda kc: KaT[0:96, hh, kc * 128:(kc + 1) * 128], QaT[0:96, hh, 0:n], f"QaT{hh}", "KaT",
                                        96 ** -0.5, (hh // 2) * 192 + (hh % 2) * 64, hh // 2, hh % 2)
                            q = hh - 8
                            kv = q // 4
                            r0 = (q % 2) * 64
                            return (lambda kc: KbT[r0:r0 + 64, kv, kc * 128:(kc + 1) * 128], QbT[r0:r0 + 64, q // 2, 0:n], f"QbT{q // 2}", "KbT",
                                    0.125, 768 + kv * 192 + (q % 2) * 64, 4 + q // 2, q % 2)

                        SB = [(0, 1), (2, 3), (4, 5)]
                        OB = [6, 7]
                        pend = []
                        for it, (hh, kcs, first, lastp) in enumerate(items):
                            kfn, qap, qkey, kkey, scl, voff, ochunk, par = head_ops(hh)
                            sp_ = it % 3
                            ps2 = PS2[sp_]
                            for i_, kc in enumerate(kcs):
                                P.op("pe", MM(ps2[:, i_ * 512:i_ * 512 + n], kfn(kc), qap, start=True, stop=True),
                                     reads=[kkey, qkey], writes=[f"B{SB[sp_][i_]}"])
                            if len(pend) >= 2:
                                pend.pop(0)()
                            ptv = PT[sp_]
                            nk = len(kcs)
                            P.op("act", ACT(ptv[:, 0:nk, 0:n], ps2[:, 0:nk * 512].rearrange("p (a c) -> p a c", c=512)[:, :, 0:n], AF.Exp, scale=scl),
                                 reads=[f"B{SB[sp_][i_]}" for i_ in range(nk)], writes=[f"PT{sp_}"])

                            def mk(hh=hh, kcs=kcs, first=first, lastp=lastp, sp_=sp_, voff=voff, ochunk=ochunk, par=par):
                                def run():
                                    ob = OB[hh % 2]
                                    for i_, kc in enumerate(kcs):
                                        P.op("pe", MM(bank(ob)[:, 0:n], Vst[:, kc, voff:voff + 128], PT[sp_][:, i_, 0:n],
                                                      start=(first and i_ == 0), stop=(lastp and i_ == len(kcs) - 1)),
                                             reads=["Vst", f"PT{sp_}"], writes=[f"B{ob}"])
                                    if lastp:
                                        o0 = par * 64
                                        d0 = 64 - o0
                                        rd = rden[hh % 2]
                                        P.op("dve", RECIP(rd[o0:o0 + 64, 0:n], bank(ob)[d0:d0 + 64, 0:n]), reads=[f"B{ob}"], writes=[f"ms{hh % 2}"])
                                        P.op("dve", TT(hj[o0:o0 + 64, ochunk, 0:n], bank(ob)[o0:o0 + 64, 0:n], rd[o0:o0 + 64, 0:n], ALU.mult),
                                             reads=[f"B{ob}", f"ms{hh % 2}"], writes=[f"{hq}{ochunk}"])
                                return run
                            pend.append(mk())
                        while pend:
                            pend.pop(0)()
                        rr[0] = 0
                        if jb + 1 < len(qblocks):
                            rr[0] = 6
                            cq_stage(jb + 1)
                        for m in range(8):
                            bo = 1 + (m % 5)
                            for c_ in range(8):
                                P.op("pe", MM(bank(bo)[:, 0:n], wout[:, c_, m * 128:(m + 1) * 128], hj[:, c_, 0:n], start=(c_ == 0), stop=(c_ == 7)),
                                     reads=["wout", f"{hq}{c_}"], writes=[f"B{bo}"])
                            P.op("dve", STT(xj[:, m, 0:n], bank(bo)[:, 0:n], Bcol(2, m, i_mod), xj[:, m, 0:n], ALU.mult, ALU.add),
                                 reads=[f"B{bo}", "xj", "modc"], writes=["xj"])
                        P.dma("sp", DMA(xs_v[:, :, c0:c0 + n], xj[:, :, 0:n]), reads=["xj"], key="xo")
                    P.end()

            if last:
                with ExitStack() as ffn:
                    ridx = sb(ffn, "ridx", [128, 2, 16], I32)
                    wts = sb(ffn, "wts", [128, 2, 16], F32)
                    flags = sb(ffn, "flags", [128, 48], I32)
                    P.flags_ap = flags
                    with ExitStack() as st:
                        SR = mk_sets(st, 4)
                        xT = sb(st, "xT", [128, 8, T_LAT], F32)
                        h2T = sb(st, "h2T", [128, 8, T_LAT], BF16)
                        hf2 = [sb(st, f"hf{i}", [128, 8, 512], F32) for i in range(2)]
                        rt = sb(st, "rt", [128, 8, 8], F32)
                        Lg = sb(st, "Lg", [128, 16, 8], F32)
                        L2 = sb(st, "L2", [128, 16, 8], F32)
                        mk1 = sb(st, "mk1", [128, 16, 8], F32)
                        mk2 = sb(st, "mk2", [128, 16, 8], F32)
                        sm = sb(st, "sm", [128, 4, 16], F32)
                        maskb = sb(st, "maskb", [128, 16, 8], BF16)
                        tot_s = sb(st, "tot_s", [128, 16, 8], F32)
                        cum = sb(st, "cum", [128, 17, 8], F32)
                        val = sb(st, "val", [128, 16, 8], F32)
                        tmp = sb(st, "tmp", [128, 16, 8], F32)
                        rf = sb(st, "rf", [128, 2, 16], F32)
                        flagsf = sb(st, "flagsf", [128, 8, 6], F32)
                        ebase = sb(st, "ebase", [128, 16, 8], F32)
                        htok = [sb(st, f"htok{i}", [128, 1024], BF16) for i in range(2)]
                        fblocks = BLOCKS[:4]
                        P.begin()
                        for bi_, (c0, n) in enumerate(fblocks):
                            P.dma("sp", DMA(xT[:, :, c0:c0 + n], xs_v[:, :, c0:c0 + n]), writes=[f"xT{bi_}"], key=f"xT{bi_}")
                        P.dma("sp", DMA(rt[:], WL["router"].rearrange("(k p) e -> p k e", p=128)), writes=["rt"], key="rt")
                        P.dma("sp", DMA(ebase[:].rearrange("p a b -> p (a b)"), ebase_d[:, :]), writes=["ebase"], key="eb")
                        for bi_, (c0, n) in enumerate(fblocks):
                            r3 = bi_ % 2
                            hf = hf2[bi_ % 2]
                            hfk = f"hf{bi_ % 2}_"
                            rstd_from([(xT[:, k, c0:c0 + n], [f"xT{bi_}"]) for k in range(8)], 128, n, 1.0 / 32.0, ones_b,
                                      [(SR[0]["sq"], "sq0"), (SR[1]["sq"], "sq1")], (SR[2 + r3]["ms"], f"ms{2 + r3}"))
                            rst_ = SR[2 + r3]["ms"]
                            for k in range(8):
                                t_, tk = SR[k % 2]["ms"], f"ms{k % 2}"
                                P.op("dve", STT(t_[:, 0:n], xT[:, k, c0:c0 + n], A2[:, k, 0:1], rst_[:, 0:n], ALU.mult, ALU.mult),
                                     reads=[f"xT{bi_}", f"ms{2 + r3}", "A2"], writes=[tk])
                                P.op("act", ACT(hf[:, k, 0:n], t_[:, 0:n], AF.Identity, bias=Bcol(3, k, 0), scale=1.0),
                                     reads=[tk, "modc"], writes=[f"{hfk}{k}"])
                                P.op("dve", CP(h2T[:, k, c0:c0 + n], hf[:, k, 0:n]), reads=[f"{hfk}{k}"], writes=[f"h2T{k}"])
                            for tt in range(n // 128):
                                tg = (c0 // 128) + tt
                                bl = nb()
                                for k in range(8):
                                    P.op("pe", MM(bank(bl)[:, 0:8], hf[:, k, tt * 128:(tt + 1) * 128], rt[:, k, :], start=(k == 0), stop=(k == 7)),
                                         reads=[f"{hfk}{k}", "rt"], writes=[f"B{bl}"])
                                P.op("dve", CP(Lg[:, tg, :], bank(bl)[:, 0:8]), reads=[f"B{bl}"], writes=["Lg"])
                        m1, m2, df, e2 = (sm[:, i, :] for i in range(4))
                        g1, g2 = wts[:, 0, :], wts[:, 1, :]
                        P.op("dve", RMAX(m1, Lg[:, :, :]), reads=["Lg"], writes=["m1"])
                        for e_ in range(8):
                            P.op("dve", TT(mk1[:, :, e_], Lg[:, :, e_], m1, ALU.is_equal), reads=["Lg", "m1"], writes=["mk1"])
                        P.op("dve", STT(L2[:, :, :], mk1[:, :, :], -1e30, Lg[:, :, :], ALU.mult, ALU.add), reads=["mk1", "Lg"], writes=["L2"])
                        P.op("dve", RMAX(m2, L2[:, :, :]), reads=["L2"], writes=["m2"])
                        for e_ in range(8):
                            P.op("dve", TT(mk2[:, :, e_], L2[:, :, e_], m2, ALU.is_equal), reads=["L2", "m2"], writes=["mk2"])
                        P.op("dve", TT(df, m2, m1, ALU.subtract), reads=["m1", "m2"], writes=["df"])
                        P.op("act", ACT(e2, df, AF.Exp), reads=["df"], writes=["e2"])
                        P.op("dve", TS(g1, e2, 1.0, None, ALU.add), reads=["e2"], writes=["wts"])
                        P.op("dve", RECIP(g1, g1), reads=["wts"], writes=["wts"])
                        P.op("dve", TT(g2, e2, g1, ALU.mult), reads=["e2", "wts"], writes=["wts"])
                        P.op("dve", TT(maskb[:, :, :], mk1[:, :, :], mk2[:, :, :], ALU.add), reads=["mk1", "mk2"], writes=["maskb"])
                        mflat = maskb[:, :, :].rearrange("p a b -> p (a b)")
                        bw_, bt_ = nb(), nb()
                        P.op("pe", MM(bank(bw_)[:, 0:128], triU, mflat), reads=["cbt", "maskb"], writes=[f"B{bw_}"])
                        P.op("pe", MM(bank(bt_)[:, 0:128], ones_b, mflat), reads=["cbt", "maskb"], writes=[f"B{bt_}"])
                        P.op("dve", CP(tot_s[:, :, :].rearrange("p a b -> p (a b)"), bank(bt_)[:, 0:128]), reads=[f"B{bt_}"], writes=["tot_s"])
                        P.op("dve", MEMSET(cum[:, 0, :], 0.0), writes=["cum"])
                        for tg in range(16):
                            P.op("dve", TT(cum[:, tg + 1, :], cum[:, tg, :], tot_s[:, tg, :], ALU.add), reads=["cum", "tot_s"], writes=["cum"])
                        P.op("dve", TT(val[:, :, :].rearrange("p a b -> p (a b)"), bank(bw_)[:, 0:128],
                                       cum[:, 0:16, :].rearrange("p a b -> p (a b)"), ALU.add), reads=[f"B{bw_}", "cum"], writes=["val"])
                        P.op("dve", TT(val[:, :, :], val[:, :, :], ebase[:, :, :], ALU.add), reads=["val", "ebase"], writes=["val"])
                        for j, mk_ in enumerate((mk1, mk2)):
                            P.op("dve", TT(tmp[:, :, :], mk_[:, :, :], val[:, :, :], ALU.mult), reads=["val", f"mk{j + 1}"], writes=["tmp"])
                            P.op("dve", RSUM(rf[:, j, :], tmp[:, :, :]), reads=["tmp"], writes=["rf"])
                        P.op("dve", CP(ridx[:, :, :], rf[:, :, :]), reads=["rf"], writes=["ridx"])
                        for b_ in range(6):
                            P.op("dve", TS(flagsf[:, :, b_], cum[:, 16, :], 512.0 + 256.0 * b_, None, ALU.is_gt), reads=["cum"], writes=["flagsf"])
                        P.op("dve", CP(flags[:, :], flagsf[:, :, :].rearrange("p a b -> p (a b)")), reads=["flagsf"], writes=["flags"])
                        for tg in range(16):
                            pz = PS2[tg % 2]
                            bk0 = 2 * (tg % 2)
                            for k in range(8):
                                P.op("pe", MM(pz[:, k * 128:(k + 1) * 128], h2T[:, k, tg * 128:(tg + 1) * 128], ident_b),
                                     reads=[f"h2T{k}", "cbt"], writes=[f"B{bk0 + k // 4}"])
                            ht = htok[tg % 2]
                            P.op("act", ACT(ht[:, 0:512], pz[:, 0:512], AF.Copy), reads=[f"B{bk0}"], writes=[f"htok{tg % 2}a"])
                            P.op("dve", CP(ht[:, 512:1024], pz[:, 512:1024]), reads=[f"B{bk0 + 1}"], writes=[f"htok{tg % 2}b"])
                            for j in range(2):
                                P.dma("pool", ISCAT(Gd[:, :], ridx[:, j, tg:tg + 1], ht[:, :], P.bc),
                                      reads=[f"htok{tg % 2}a", f"htok{tg % 2}b", "ridx"], key=f"sc{tg % 2}")
                        P.end()

                    GS = 4
                    groups = []
                    f0 = 0
                    while f0 < NFC:
                        gs = min(GS, NFC - f0)
                        groups.append((f0, gs))
                        f0 += gs
                    NRING = 4
                    with ExitStack() as st:
                        wgt = [sb(st, f"wgt{i}", [128, 8, GS * 128], BF16) for i in range(2)]
                        wut = [sb(st, f"wut{i}", [128, 8, GS * 128], BF16) for i in range(2)]
                        wdt = [sb(st, f"wdt{i}", [128, GS, 1024], BF16) for i in range(2)]
                        sgt = [sb(st, f"sgt{i}", [128, 512], F32) for i in range(2)]
                        actt = [sb(st, f"actt{i}", [128, GS, 512], BF16) for i in range(2)]
                        hT0 = [sb(st, f"hT0_{i}", [128, 8, 512], BF16) for i in range(2)]
                        hTx = [sb(st, f"hTx{i}", [128, 8, 256], BF16) for i in range(6)]
                        ya0 = sb(st, "ya0", [128, 4, 1024], F32)
                        yax = sb(st, "yax", [128, 12, 1024], F32)

                        def geom(blk):
                            return (0, 512) if blk == 0 else (512 + 256 * (blk - 1), 256)

                        def yslice(blk, s4, half):
                            if blk == 0:
                                return ya0[:, s4, half * 512:(half + 1) * 512]
                            return yax[:, (blk - 1) * 2 + s4, half * 512:(half + 1) * 512]
                        gtok = [sb(st, f"gtok{i}", [128, 1024], BF16) for i in range(NRING)]
                        P.begin()
                        ring = [0]
                        evq = [0]

                        def hbuf(e_, blk):
                            if blk == 0:
                                return hT0[e_ % 2], f"hT0_{e_ % 2}"
                            return hTx[blk - 1], f"hTx{blk - 1}"

                        def prep(e_, blk, cond):
                            hT, hkey = hbuf(e_, blk)
                            s0, W = geom(blk)
                            nst = W // 128
                            bufs = []
                            for s4 in range(nst):
                                r = ring[0] % NRING
                                ring[0] += 1
                                row0 = e_ * T_LAT + s0 + s4 * 128
                                P.dma("sp", DMA(gtok[r][:, :], Gd[row0:row0 + 128, :]), writes=[f"gt{r}"], key=f"gl{r}")
                                bufs.append(r)
                            for k in range(8):
                                bi = k % 4
                                for s4 in range(nst):
                                    P.op("pe", MM(bank(bi)[:, s4 * 128:(s4 + 1) * 128], gtok[bufs[s4]][:, k * 128:(k + 1) * 128], ident_b),
                                         reads=[f"gt{bufs[s4]}", "cbt"], writes=[f"B{bi}"], cond=cond)
                                evq[0] += 1
                                if evq[0] % 2 == 0:
                                    P.op("act", ACT(hT[:, k, 0:W], bank(bi)[:, 0:W], AF.Copy), reads=[f"B{bi}"], writes=[f"{hkey}_{k}"], cond=cond)
                                else:
                                    P.op("dve", CP(hT[:, k, 0:W], bank(bi)[:, 0:W]), reads=[f"B{bi}"], writes=[f"{hkey}_{k}"], cond=cond)

                        gi = 0
                        ai = 0
                        cq = [0]

                        def block_compute(e_, gidx, gs, b_, blk, cond):
                            nonlocal ai
                            hT, hkey = hbuf(e_, blk)
                            s0, W = geom(blk)
                            nst = W // 128
                            a_ = ai % 2
                            ai += 1
                            at = actt[a_]
                            for f in range(gs):
                                pg = (f % 2) * 2
                                pu = pg + 1
                                for k in range(8):
                                    P.op("pe", MM(bank(pg)[:, 0:W], wgt[b_][:, k, f * 128:(f + 1) * 128], hT[:, k, 0:W], start=(k == 0), stop=(k == 7)),
                                         reads=[f"wgt{b_}", f"{hkey}_{k}"], writes=[f"B{pg}"], cond=cond)
                                for k in range(8):
                                    P.op("pe", MM(bank(pu)[:, 0:W], wut[b_][:, k, f * 128:(f + 1) * 128], hT[:, k, 0:W], start=(k == 0), stop=(k == 7)),
                                         reads=[f"wut{b_}", f"{hkey}_{k}"], writes=[f"B{pu}"], cond=cond)
                                sg = sgt[f % 2]
                                P.op("act", ACT(sg[:, 0:W], bank(pg)[:, 0:W], AF.Silu), reads=[f"B{pg}"], writes=[f"sg{f % 2}"], cond=cond)
                                P.op("dve", TT(at[:, f, 0:W], sg[:, 0:W], bank(pu)[:, 0:W], ALU.mult),
                                     reads=[f"sg{f % 2}", f"B{pu}"], writes=[f"act{a_}_{f}"], cond=cond)
                            for half in range(2):
                                for s4 in range(nst):
                                    bd = 4 + (half * nst + s4) % 4
                                    for f in range(gs):
                                        P.op("pe", MM(bank(bd)[:, 0:512], at[:, f, s4 * 128:(s4 + 1) * 128], wdt[b_][:, f, half * 512:(half + 1) * 512],
                                                      start=(f == 0), stop=(f == gs - 1)),
                                             reads=[f"wdt{b_}", f"act{a_}_{f}"], writes=[f"B{bd}"], cond=cond)
                                    ya = yslice(blk, s4, half)
                                    yk = f"yacc{blk}_{s4}_{half}"
                                    if gidx == 0:
                                        cq[0] += 1
                                        if cq[0] % 2 == 0:
                                            P.op("act", ACT(ya, bank(bd)[:, 0:512], AF.Copy), reads=[f"B{bd}"], writes=[yk], cond=cond)
                                        else:
                                            P.op("dve", CP(ya, bank(bd)[:, 0:512]), reads=[f"B{bd}"], writes=[yk], cond=cond)
                                    else:
                                        P.op("dve", TT(ya, bank(bd)[:, 0:512], ya, ALU.add), reads=[f"B{bd}", yk], writes=[yk], cond=cond)

                        prep(0, 0, None)
                        for e_ in range(8):
                            wg_v = WL["wg"][e_].rearrange("(k p) f -> p k f", p=128)
                            wu_v = WL["wu"][e_].rearrange("(k p) f -> p k f", p=128)
                            wd_v = WL["wd"][e_].rearrange("(f p) d -> p f d", p=128)
                            for gidx, (f0, gs) in enumerate(groups):
                                b_ = gi % 2
                                gi += 1
                                P.dma("pool", DMAS([(wgt[b_][:, k, 0:gs * 128], wg_v[:, k, f0 * 128:(f0 + gs) * 128]) for k in range(8)]),
                                      writes=[f"wgt{b_}"], key=f"wg{b_}", n=8)
                                P.dma("pool", DMAS([(wut[b_][:, k, 0:gs * 128], wu_v[:, k, f0 * 128:(f0 + gs) * 128]) for k in range(8)]),
                                      writes=[f"wut{b_}"], key=f"wu{b_}", n=8)
                                P.dma("pool", DMAS([(wdt[b_][:, f, :], wd_v[:, f0 + f, :]) for f in range(gs)]),
                                      writes=[f"wdt{b_}"], key=f"wd{b_}", n=gs)
                                for blk in range(7):
                                    cond = None if blk == 0 else (e_, gidx, blk)
                                    if gidx == 0 and blk > 0:
                                        prep(e_, blk, cond)
                                    block_compute(e_, gidx, gs, b_, blk, cond)
                                if gidx == 3 and e_ < 7:
                                    prep(e_ + 1, 0, None)
                            row0 = e_ * T_LAT
                            P.dma("sp", DMA(Yd[row0:row0 + 512, :].rearrange("(s p) d -> p s d", p=128), ya0[:, :, :]),
                                  reads=[f"yacc0_{s4}_{half}" for s4 in range(4) for half in range(2)], key="yw0")
                            P.dma("sp", DMA(Yd[row0 + 512:row0 + T_LAT, :].rearrange("(s p) d -> p s d", p=128), yax[:, :, :]),
                                  reads=[f"yacc{blk}_{s4}_{half}" for blk in range(1, 7) for s4 in range(2) for half in range(2)], key="ywx")
                        P.end()

                    with ExitStack() as st:
                        xo = [sb(st, f"xo{i}", [128, 8, 512], F32) for i in range(2)]
                        yg = [[sb(st, f"yg{i}_{j}", [128, 1024], F32) for j in range(2)] for i in range(3)]
                        ttl = [sb(st, f"ttl{i}", [128, 1024], F32) for i in range(2)]
                        ot = [sb(st, f"ot{i}", [128, 1024], F32) for i in range(2)]
                        g2row = sb(st, "g2row", [128, 1024], F32)
                        dg = [sb(st, f"dg{i}", [128, 128], F32) for i in range(2)]
                        onesf = sb(st, "onesf", [128, 128], F32)
                        P.begin()
                        P.op("dve", MEMSET(onesf[:], 1.0), writes=["onesf"])
                        for m in range(8):
                            d_ = dg[m % 2]
                            P.op("dve", TS(d_[:, :], ident_f[:, :], Bcol(5, m, 0), None, ALU.mult), reads=["ident_f", "modc"], writes=[f"dg{m % 2}"])
                            bq_ = 6 + (m % 2)
                            P.op("pe", MM(bank(bq_)[:, 0:128], onesf[:, :], d_[:, :]), reads=["onesf", f"dg{m % 2}"], writes=[f"B{bq_}"])
                            P.op("act", ACT(g2row[:, m * 128:(m + 1) * 128], bank(bq_)[:, 0:128], AF.Copy), reads=[f"B{bq_}"], writes=["g2row"])
                        for tg in range(16):
                            b4 = tg // 4
                            xb = xo[b4 % 2]
                            if tg % 4 == 0:
                                P.dma("sp", DMA(xb[:, :, :], xs_v[:, :, b4 * 512:(b4 + 1) * 512]), writes=[f"xo{b4 % 2}"], key=f"xo{b4 % 2}")
                            pz = PS2[tg % 2]
                            bk0 = 2 * (tg % 2)
                            for k in range(8):
                                P.op("pe", TR(pz[:, k * 128:(k + 1) * 128], xb[:, k, (tg % 4) * 128:(tg % 4 + 1) * 128], ident_f[:, :]),
                                     reads=[f"xo{b4 % 2}", "ident_f"], writes=[f"B{bk0 + k // 4}"])
                            y0, y1 = yg[tg % 3]
                            for j in range(2):
                                P.dma("pool", IGATH(yg[tg % 3][j][:, :], Yd[:, :], ridx[:, j, tg:tg + 1], P.bc),
                                      reads=["ridx"], writes=[f"yg{tg % 3}_{j}"], key=f"yg{tg % 3}_{j}")
                            t_ = ttl[tg % 2]
                            tk = f"ttl{tg % 2}"
                            P.op("act", ACT(t_[:, :], y1[:, :], AF.Identity, scale=wts[:, 1, tg:tg + 1]), reads=[f"yg{tg % 3}_1", "wts"], writes=[tk])
                            P.op("dve", STT(t_[:, :], y0[:, :], wts[:, 0, tg:tg + 1], t_[:, :], ALU.mult, ALU.add),
                                 reads=[f"yg{tg % 3}_0", "wts", tk], writes=[tk])
                            P.op("dve", TT(t_[:, :], t_[:, :], g2row[:, :], ALU.mult), reads=[tk, "g2row"], writes=[tk])
                            o_ = ot[tg % 2]
                            P.op("dve", TT(o_[:, 0:512], t_[:, 0:512], pz[:, 0:512], ALU.add), reads=[tk, f"B{bk0}"], writes=[f"ot{tg % 2}a"])
                            P.op("dve", TT(o_[:, 512:1024], t_[:, 512:1024], pz[:, 512:1024], ALU.add), reads=[tk, f"B{bk0 + 1}"], writes=[f"ot{tg % 2}b"])
                            P.dma("sp", DMA(out_d[tg * 128:(tg + 1) * 128, :], o_[:, :]), reads=[f"ot{tg % 2}a", f"ot{tg % 2}b"], key=f"out{tg % 2}")
                        P.end()
                continue

            with ExitStack() as ffn:
                ntok = T_LAT if last else T_ALL
                fblocks = BLOCKS[:4] if last else BLOCKS
                xT = sb(ffn, "xT", [128, 8, ntok], F32)
                h2T = sb(ffn, "h2T", [128, 8, ntok], BF16)
                if last:
                    gT = sb(ffn, "gT", [128, T_LAT], F32)
                with ExitStack() as st:
                    SR = mk_sets(ffn, 4)
                    if last:
                        hf = sb(st, "hf", [128, 8, 512], F32)
                        rt = sb(st, "rt", [128, 8, 8], F32)
                        Lg = sb(st, "Lg", [128, 16, 8], F32)
                        L2 = sb(st, "L2", [128, 16, 8], F32)
                        mk1 = sb(st, "mk1", [128, 16, 8], F32)
                        mk2 = sb(st, "mk2", [128, 16, 8], F32)
                        gts = sb(st, "gts", [128, 16, 8], F32)
                        sm = sb(st, "sm", [128, 6, 16], F32)
                    P.begin()
                    for bi_, (c0, n) in enumerate(fblocks):
                        P.dma("sp", DMA(xT[:, :, c0:c0 + n], xs_v[:, :, c0:c0 + n]), writes=[f"xT{bi_}"], key=f"xT{bi_}")
                    if last:
                        P.dma("sp", DMA(rt[:], WL["router"].rearrange("(k p) e -> p k e", p=128)), writes=["rt"], key="rt")

                    norm_fns = []
                    for bi_, (c0, n) in enumerate(fblocks):
                        def norm_blk(bi_=bi_, c0=c0, n=n):
                            i_mod = 0 if c0 < T_LAT else 1

                            r3 = bi_ % 2
                            rstd_from([(xT[:, k, c0:c0 + n], [f"xT{bi_}"]) for k in range(8)], 128, n, 1.0 / 32.0, ones_b,
                                      [(SR[0]["sq"], "sq0"), (SR[1]["sq"], "sq1")], (SR[2 + r3]["ms"], f"ms{2 + r3}"))
                            rst_ = SR[2 + r3]["ms"]
                            for k in range(8):
                                t_, tk = SR[k % 2]["ms"], f"ms{k % 2}"
                                P.op("dve", STT(t_[:, 0:n], xT[:, k, c0:c0 + n], A2[:, k, i_mod:i_mod + 1], rst_[:, 0:n], ALU.mult, ALU.mult),
                                     reads=[f"xT{bi_}", f"ms{2 + r3}", "A2"], writes=[tk])
                                if not last:
                                    if k % 2 == 0:
                                        P.op("dve", TS(h2T[:, k, c0:c0 + n], t_[:, 0:n], Bcol(3, k, i_mod), None, ALU.add),
                                             reads=[tk, "modc"], writes=[f"h2T{k}_{bi_}"])
                                    else:
                                        P.op("act", ACT(h2T[:, k, c0:c0 + n], t_[:, 0:n], AF.Identity, bias=Bcol(3, k, i_mod), scale=1.0),
                                             reads=[tk, "modc"], writes=[f"h2T{k}_{bi_}"])
                                else:
                                    P.op("act", ACT(hf[:, k, 0:n], t_[:, 0:n], AF.Identity, bias=Bcol(3, k, i_mod), scale=1.0),
                                         reads=[tk, "modc"], writes=[f"hf{k}"])
                                    P.op("pool", CP(h2T[:, k, c0:c0 + n], hf[:, k, 0:n]), reads=[f"hf{k}"], writes=[f"h2T{k}"])
                            if last:
                                for tt in range(n // 128):
                                    tg = (c0 // 128) + tt
                                    bl = nb()
                                    for k in range(8):
                                        P.op("pe", MM(bank(bl)[:, 0:8], hf[:, k, tt * 128:(tt + 1) * 128], rt[:, k, :], start=(k == 0), stop=(k == 7)),
                                             reads=[f"hf{k}", "rt"], writes=[f"B{bl}"])
                                    P.op("dve", CP(Lg[:, tg, :], bank(bl)[:, 0:8]), reads=[f"B{bl}"], writes=["Lg"])
                        norm_fns.append(norm_blk)
                    norm_fns[0]()
                    if last:
                        m1, m2, df, e2, g1, g2 = (sm[:, i, :] for i in range(6))
                        P.op("dve", RMAX(m1, Lg[:, :, :]), reads=["Lg"], writes=["m1"])
                        for e_ in range(8):
                            P.op("dve", TT(mk1[:, :, e_], Lg[:, :, e_], m1, ALU.is_equal), reads=["Lg", "m1"], writes=["mk1"])
                        P.op("dve", STT(L2[:, :, :], mk1[:, :, :], -1e30, Lg[:, :, :], ALU.mult, ALU.add), reads=["mk1", "Lg"], writes=["L2"])
                        P.op("dve", RMAX(m2, L2[:, :, :]), reads=["L2"], writes=["m2"])
                        for e_ in range(8):
                            P.op("dve", TT(mk2[:, :, e_], L2[:, :, e_], m2, ALU.is_equal), reads=["L2", "m2"], writes=["mk2"])
                        P.op("dve", TT(df, m2, m1, ALU.subtract), reads=["m1", "m2"], writes=["df"])
                        P.op("act", ACT(e2, df, AF.Exp), reads=["df"], writes=["e2"])
                        P.op("dve", TS(g1, e2, 1.0, None, ALU.add), reads=["e2"], writes=["g1"])
                        P.op("dve", RECIP(g1, g1), reads=["g1"], writes=["g1"])
                        P.op("dve", TT(g2, e2, g1, ALU.mult), reads=["e2", "g1"], writes=["g2"])
                        for e_ in range(8):
                            P.op("dve", TT(gts[:, :, e_], mk1[:, :, e_], g1, ALU.mult), reads=["mk1", "g1"], writes=["gts"])
                            P.op("dve", TT(mk2[:, :, e_], mk2[:, :, e_], g2, ALU.mult), reads=["mk2", "g2"], writes=["mk2"])
                        P.op("dve", TT(gts[:, :, :], gts[:, :, :], mk2[:, :, :], ALU.add), reads=["gts", "mk2"], writes=["gts"])
                        for q4 in range(4):
                            bg = nb()
                            for tt in range(4):
                                tg = q4 * 4 + tt
                                P.op("pe", TR(bank(bg)[0:8, tt * 128:(tt + 1) * 128], gts[:, tg, :], ident_f[:, :]),
                                     reads=["gts", "ident_f"], writes=[f"B{bg}"])
                            P.op("dve", CP(gT[0:8, q4 * 512:(q4 + 1) * 512], bank(bg)[0:8, 0:512]), reads=[f"B{bg}"], writes=["gT"])

                GS = 4
                groups = []
                f0 = 0
                while f0 < NFC:
                    gs = min(GS, NFC - f0)
                    groups.append((f0, gs))
                    f0 += gs
                with ExitStack() as st:
                    wgt = [sb(st, f"wgt{i}", [128, 8, GS * 128], BF16) for i in range(2)]
                    wut = [sb(st, f"wut{i}", [128, 8, GS * 128], BF16) for i in range(2)]
                    wdt = [sb(st, f"wdt{i}", [128, GS, 1024], BF16) for i in range(2)]
                    sgt = [sb(st, f"sgt{i}", [128, 512], F32) for i in range(2)]
                    actt = [sb(st, f"actt{i}", [128, GS, 512], BF16) for i in range(2)]
                    if last:
                        Ge = [sb(st, f"Ge{i}", [128, T_LAT], BF16) for i in range(2)]
                        selE = sb(st, "selE", [128, 8, 128], F32)
                    nexp = 8 if last else 1
                    if last:
                        P.dma("sp", DMA(selE[0:8, :, :], selE_d[:, :, :]), writes=["selE"], key="selE")
                    gi = 0
                    ai = 0
                    rr[0] = 0
                    for e_ in range(nexp):
                        wg_v = WL["wg"][e_].rearrange("(k p) f -> p k f", p=128)
                        wu_v = WL["wu"][e_].rearrange("(k p) f -> p k f", p=128)
                        wd_v = WL["wd"][e_].rearrange("(f p) d -> p f d", p=128)
                        if last:
                            ge = Ge[e_ % 2]
                            for q4 in range(4):
                                bg = 6 + (q4 % 2)
                                P.op("pe", MM(bank(bg)[:, 0:512], selE[0:8, e_, :], gT[0:8, q4 * 512:(q4 + 1) * 512]),
                                     reads=["selE", "gT"], writes=[f"B{bg}"])
                                P.op("act", ACT(ge[:, q4 * 512:(q4 + 1) * 512], bank(bg)[:, 0:512], AF.Copy), reads=[f"B{bg}"], writes=[f"Ge{e_ % 2}"])
                        for (f0, gs) in groups:
                            b_ = gi % 2
                            gi += 1
                            P.dma("pool", DMAS([(wgt[b_][:, k, 0:gs * 128], wg_v[:, k, f0 * 128:(f0 + gs) * 128]) for k in range(8)]),
                                  writes=[f"wgt{b_}"], key=f"wg{b_}", n=8)
                            P.dma("pool", DMAS([(wut[b_][:, k, 0:gs * 128], wu_v[:, k, f0 * 128:(f0 + gs) * 128]) for k in range(8)]),
                                  writes=[f"wut{b_}"], key=f"wu{b_}", n=8)
                            P.dma("pool", DMAS([(wdt[b_][:, f, :], wd_v[:, f0 + f, :]) for f in range(gs)]),
                                  writes=[f"wdt{b_}"], key=f"wd{b_}", n=gs)
                            for bidx, (c0, n) in enumerate(fblocks):
                                if e_ == 0 and f0 == 0 and bidx + 1 < len(fblocks):
                                    norm_fns[bidx + 1]()
                                i_mod = 0 if c0 < T_LAT else 1
                                a_ = ai % 2
                                ai += 1
                                at = actt[a_]
                                for f in range(gs):
                                    pg = (f % 2) * 2
                                    pu = pg + 1
                                    for k in range(8):
                                        P.op("pe", MM(bank(pg)[:, 0:n], wgt[b_][:, k, f * 128:(f + 1) * 128], h2T[:, k, c0:c0 + n], start=(k == 0), stop=(k == 7)),
                                             reads=[f"wgt{b_}", f"h2T{k}_{bidx}"], writes=[f"B{pg}"])
                                    for k in range(8):
                                        P.op("pe", MM(bank(pu)[:, 0:n], wut[b_][:, k, f * 128:(f + 1) * 128], h2T[:, k, c0:c0 + n], start=(k == 0), stop=(k == 7)),
                                             reads=[f"wut{b_}", f"h2T{k}_{bidx}"], writes=[f"B{pu}"])
                                    sg = sgt[f % 2]
                                    P.op("act", ACT(sg[:, 0:n], bank(pg)[:, 0:n], AF.Silu), reads=[f"B{pg}"], writes=[f"sg{f % 2}"])
                                    if last:
                                        P.op("dve", TT(sg[:, 0:n], sg[:, 0:n], Ge[e_ % 2][:, c0:c0 + n], ALU.mult),
                                             reads=[f"sg{f % 2}", f"Ge{e_ % 2}"], writes=[f"sg{f % 2}"])
                                    P.op("dve", TT(at[:, f, 0:n], sg[:, 0:n], bank(pu)[:, 0:n], ALU.mult),
                                         reads=[f"sg{f % 2}", f"B{pu}"], writes=[f"act{a_}_{f}"])
                                for m in range(8):
                                    bd = 4 + (m % 4)
                                    for f in range(gs):
                                        P.op("pe", MM(bank(bd)[:, 0:n], wdt[b_][:, f, m * 128:(m + 1) * 128], at[:, f, 0:n], start=(f == 0), stop=(f == gs - 1)),
                                             reads=[f"wdt{b_}", f"act{a_}_{f}"], writes=[f"B{bd}"])
                                    P.op("dve", STT(xT[:, m, c0:c0 + n], bank(bd)[:, 0:n], Bcol(5, m, i_mod), xT[:, m, c0:c0 + n], ALU.mult, ALU.add),
                                         reads=[f"B{bd}", "modc", f"xT{m}_{c0}"], writes=[f"xT{m}_{c0}"])
                                    if (not last) and e_ == nexp - 1 and f0 + gs == NFC:
                                        P.dma("sp", DMA(xs_v[:, m, c0:c0 + n], xT[:, m, c0:c0 + n]), reads=[f"xT{m}_{c0}"], key="xo")
                    P.end()

                if not last:
                    pass
                else:
                    with ExitStack() as st:
                        ot = [sb(st, f"ot{i}", [128, 1024], F32) for i in range(2)]
                        P.begin()
                        for tt in range(16):
                            o_ = ot[tt % 2]
                            pz = PS2[tt % 2]
                            for k in range(8):
                                P.op("pe", TR(pz[:, k * 128:(k + 1) * 128], xT[:, k, tt * 128:(tt + 1) * 128], ident_f[:, :]),
                                     reads=["ident_f"], writes=[f"PZ{tt % 2}"])
                            P.op("act", ACT(o_[:, 0:512], pz[:, 0:512], AF.Copy), reads=[f"PZ{tt % 2}"], writes=[f"ot{tt % 2}a"])
                            P.op("dve", CP(o_[:, 512:1024], pz[:, 512:1024]), reads=[f"PZ{tt % 2}"], writes=[f"ot{tt % 2}b"])
                            P.dma("sp", DMA(out_d[tt * 128:(tt + 1) * 128, :], o_[:, :]), reads=[f"ot{tt % 2}a", f"ot{tt % 2}b"], key=f"out{tt % 2}")
                        P.end()
    return nc


_CACHE = {}


def _prep_inputs(inputs):
    f = lambda a: np.ascontiguousarray(np.asarray(a, dtype=np.float32))
    consts = make_consts()
    shared = dict(consts)
    for L in range(2):
        p = f"l{L}_"
        shared[p + "w_mod"] = f(inputs[p + "w_mod"])
        vecs = np.concatenate([f(inputs[p + "b_mod"]).reshape(48, 128), f(inputs[p + "norm_attn"]).reshape(8, 128),
                               f(inputs[p + "norm_ffn"]).reshape(8, 128), f(inputs[p + "q_lat_norm"]).reshape(2, 128),
                               f(inputs[p + "kv_lat_norm"]).reshape(1, 128)], axis=0)
        shared[p + "vecs"] = np.ascontiguousarray(vecs)
        for nm in ("w_in", "w_uq", "w_ukv", "w_out"):
            shared[p + nm] = f(inputs[p + nm])
        for nm in ("mla_q_gain", "mla_k_gain", "gqa_q_gain", "gqa_k_gain"):
            shared[p + nm] = f(inputs[p + nm]).reshape(-1, 1)
    for nm in ("l0_ffn_w_gate", "l0_ffn_w_up", "l0_ffn_w_down", "l1_router", "l1_exp_w_gate", "l1_exp_w_up", "l1_exp_w_down"):
        shared[nm] = f(inputs[nm])
    x = f(inputs["x"])
    ctx = f(inputs["ctx"])
    c = f(inputs["c"])
    cc = f(inputs["c_ctx"]).reshape(8, 128)
    in_maps = []
    for b in range(8):
        m = dict(shared)
        m["x"] = x[b]
        m["ctx"] = ctx[b]
        m["cvec"] = np.ascontiguousarray(np.concatenate([c[b].reshape(8, 128), cc], axis=0))
        in_maps.append(m)
    return in_maps


def kernel(**inputs):
    if "nc" not in _CACHE:
        _CACHE["nc"] = build_program()
    nc = _CACHE["nc"]
    in_maps = _prep_inputs(inputs)
    res = run_bass_kernel_spmd(nc, in_maps, core_ids=list(range(8)))
    out = np.stack([np.asarray(res.results[b]["out"], dtype=np.float32) for b in range(8)], axis=0)
    return out
```
